# Optimizing a Trainium2 kernel written in Bass

```python
import math
import jax, jax.numpy as jnp
from jax import lax
import numpy as np

D_MODEL = 1024
BATCH = 8
SEQ = 8192
DEPTH = 1

EPS = 1e-6
POOL_WINDOWS = (2, 4, 8, 16)
POOL_GROUPS = 4
POOL_WIDTH = D_MODEL // 2
POOL_GROUP_DIM = POOL_WIDTH // POOL_GROUPS
DN_HEADS = 8
DN_HEAD_DIM = 128
DN_WIDTH = DN_HEADS * DN_HEAD_DIM
CONV_WIDTH = 4
CHUNK = 64
PEER_HEADS = 8
N_KEYS = 128
N_EXPERTS = N_KEYS * N_KEYS
PEER_TOPK = 16
PEER_HALF = 128
PEER_QUERY_DIM = 2 * PEER_HALF
PEER_BLOCK = 64
SPLIT_POINTS = (
    POOL_WIDTH,
    POOL_WIDTH + 3 * DN_WIDTH,
    POOL_WIDTH + 4 * DN_WIDTH,
    POOL_WIDTH + 4 * DN_WIDTH + DN_HEADS,
    POOL_WIDTH + 4 * DN_WIDTH + 2 * DN_HEADS,
    POOL_WIDTH + 4 * DN_WIDTH + 2 * DN_HEADS + D_MODEL,
)
IN_COLS = POOL_WIDTH + 4 * DN_WIDTH + 2 * DN_HEADS + 2 * D_MODEL

kernel_name = "hybrid_pool_deltanet_peer_block"


def rmsnorm(x, w):
    xf = x.astype(jnp.float32)
    y = xf * lax.rsqrt(jnp.mean(xf * xf, axis=-1, keepdims=True) + EPS)
    return (y * w.astype(jnp.float32)).astype(x.dtype)


def l2norm(x):
    return x * lax.rsqrt(jnp.sum(x * x, axis=-1, keepdims=True) + EPS)


def pool_mixer(xa, pool_w, pool_scale):
    b, s, _ = xa.shape
    xf = xa.astype(jnp.float32)
    csum = jnp.cumsum(xf, axis=1)
    count = jnp.arange(1, s + 1, dtype=jnp.float32)[None, :, None]
    groups = []
    for g, win in enumerate(POOL_WINDOWS):
        sl = slice(g * POOL_GROUP_DIM, (g + 1) * POOL_GROUP_DIM)
        c = csum[..., sl]
        c_lag = jnp.pad(c, ((0, 0), (win, 0), (0, 0)))[:, :s]
        mean = (c - c_lag) / jnp.minimum(count, float(win))
        groups.append(mean - xf[..., sl])
    pooled = jnp.stack(groups, axis=2)
    y = jnp.einsum("bsgc,gcd->bsgd", pooled, pool_w.astype(jnp.float32))
    y = y.reshape(b, s, POOL_WIDTH) * pool_scale.astype(jnp.float32)
    return y.astype(xa.dtype)


def causal_dwconv(x, w):
    s = x.shape[1]
    k = w.shape[0]
    xp = jnp.pad(x, ((0, 0), (k - 1, 0), (0, 0)))
    y = xp[:, 0:s] * w[0]
    for j in range(1, k):
        y = y + xp[:, j:j + s] * w[j]
    return y


def gated_delta_rule(q, k, v, g, beta):
    b, h, s, dk = q.shape
    dv = v.shape[-1]
    nc = s // CHUNK
    q = q * (dk ** -0.5)
    k_beta = k * beta[..., None]
    v_beta = v * beta[..., None]

    def chunks(t):
        return t.reshape(b, h, nc, CHUNK, t.shape[-1])

    q, k, k_beta, v_beta = chunks(q), chunks(k), chunks(k_beta), chunks(v_beta)
    gc = jnp.cumsum(g.reshape(b, h, nc, CHUNK), axis=-1)
    tril = jnp.tril(jnp.ones((CHUNK, CHUNK), dtype=bool))
    strict = jnp.tril(jnp.ones((CHUNK, CHUNK), dtype=bool), -1)
    diff = gc[..., :, None] - gc[..., None, :]
    decay = jnp.where(tril, jnp.exp(jnp.where(tril, diff, 0.0)), 0.0)
    lower = jnp.where(strict, jnp.einsum("bhnid,bhnjd->bhnij", k_beta, k) * decay, 0.0)
    eye = jnp.eye(CHUNK, dtype=jnp.float32)
    t_inv = lax.linalg.triangular_solve(eye + lower, jnp.broadcast_to(eye, lower.shape),
                                        left_side=True, lower=True, unit_diagonal=True)
    u = jnp.einsum("bhnij,bhnjd->bhnid", t_inv, v_beta)
    w = jnp.einsum("bhnij,bhnjd->bhnid", t_inv, k_beta * jnp.exp(gc)[..., None])
    attn = jnp.where(tril, jnp.einsum("bhnid,bhnjd->bhnij", q, k) * decay, 0.0)

    def step(state, inp):
        q_i, k_i, u_i, w_i, a_i, g_i = inp
        v_new = u_i - jnp.einsum("bhcd,bhde->bhce", w_i, state)
        o = (jnp.einsum("bhcd,bhde->bhce", q_i * jnp.exp(g_i)[..., None], state)
             + jnp.einsum("bhij,bhje->bhie", a_i, v_new))
        g_last = g_i[..., -1]
        k_dec = k_i * jnp.exp(g_last[..., None] - g_i)[..., None]
        state = state * jnp.exp(g_last)[..., None, None] + jnp.einsum("bhcd,bhce->bhde", k_dec, v_new)
        return state, o

    xs = tuple(jnp.moveaxis(t, 2, 0) for t in (q, k, u, w, attn, gc))
    state0 = jnp.zeros((b, h, dk, dv), dtype=jnp.float32)
    _, o = lax.scan(step, state0, xs)
    return jnp.moveaxis(o, 0, 2).reshape(b, h, s, dv)


def deltanet_branch(qkv, z, beta_logit, a_logit, conv_w, a_log, dt_bias, dn_norm_w):
    b, s, _ = qkv.shape
    out_dtype = qkv.dtype
    f32 = jnp.float32
    qkv = jax.nn.silu(causal_dwconv(qkv.astype(f32), conv_w.astype(f32)))
    q, k, v = jnp.split(qkv, 3, axis=-1)

    def heads(t):
        return t.reshape(b, s, DN_HEADS, DN_HEAD_DIM).transpose(0, 2, 1, 3)

    q = l2norm(heads(q))
    k = l2norm(heads(k))
    v = heads(v)
    beta = jax.nn.sigmoid(beta_logit.astype(f32)).transpose(0, 2, 1)
    g = (-jnp.exp(a_log.astype(f32))
         * jax.nn.softplus(a_logit.astype(f32) + dt_bias.astype(f32))).transpose(0, 2, 1)
    o = gated_delta_rule(q, k, v, g, beta).transpose(0, 2, 1, 3)
    zf = z.astype(f32).reshape(b, s, DN_HEADS, DN_HEAD_DIM)
    o = rmsnorm(o, dn_norm_w) * jax.nn.silu(zf)
    return o.reshape(b, s, DN_WIDTH).astype(out_dtype)


def peer_ffn(x, w_query, keys_1, keys_2, expert_down, expert_up):
    b, s, d = x.shape
    f32 = jnp.float32
    q = (x @ w_query).astype(f32).reshape(b, s, PEER_HEADS, 2, PEER_HALF)
    s1 = jnp.einsum("bshc,hkc->bshk", q[..., 0, :], keys_1.astype(f32))
    s2 = jnp.einsum("bshc,hkc->bshk", q[..., 1, :], keys_2.astype(f32))
    v1, i1 = lax.top_k(s1, PEER_TOPK)
    v2, i2 = lax.top_k(s2, PEER_TOPK)
    cand_score = (v1[..., :, None] + v2[..., None, :]).reshape(b, s, PEER_HEADS, PEER_TOPK * PEER_TOPK)
    cand_idx = (i1[..., :, None] * N_KEYS + i2[..., None, :]).reshape(b, s, PEER_HEADS, PEER_TOPK * PEER_TOPK)
    top_score, pos = lax.top_k(cand_score, PEER_TOPK)
    experts = jnp.take_along_axis(cand_idx, pos, axis=-1)
    gates = jax.nn.softmax(top_score, axis=-1)

    nb = s // PEER_BLOCK
    xb = x.reshape(b, nb, PEER_BLOCK, d).transpose(1, 0, 2, 3)
    eb = experts.reshape(b, nb, PEER_BLOCK, PEER_HEADS, PEER_TOPK).transpose(1, 0, 2, 3, 4)
    gb = gates.reshape(b, nb, PEER_BLOCK, PEER_HEADS, PEER_TOPK).transpose(1, 0, 2, 3, 4)

    def block(args):
        xi, ei, gi = args
        u = jnp.take(expert_down, ei, axis=0)
        act = jax.nn.gelu(jnp.einsum("btd,bthkd->bthk", xi, u).astype(f32), approximate=False)
        vv = jnp.take(expert_up, ei, axis=0)
        return jnp.einsum("bthk,bthkd->btd", (gi * act).astype(x.dtype), vv)

    y = lax.map(block, (xb, eb, gb))
    return y.transpose(1, 0, 2, 3).reshape(b, s, d)


def setup_inputs(seed: int = 0) -> dict:
    key = jax.random.key(seed)
    ks = jax.random.split(key, 20)
    f32 = jnp.float32

    def nrm(k, shape, scale):
        return scale * jax.random.normal(k, shape, f32)

    def gain(k, shape):
        return 1.0 + 0.05 * jax.random.normal(k, shape, f32)

    dt = jnp.exp(jax.random.uniform(ks[7], (DEPTH, DN_HEADS), f32, math.log(1e-3), math.log(1e-1)))
    dt_bias = dt + jnp.log(-jnp.expm1(-dt))
    a_log = jnp.log(jax.random.uniform(ks[8], (DEPTH, DN_HEADS), f32, 1.0, 16.0))
    return {
        "x": jax.random.normal(ks[0], (BATCH, SEQ, D_MODEL), f32),
        "mix_norm_w": gain(ks[1], (DEPTH, D_MODEL)),
        "w_in": nrm(ks[2], (DEPTH, D_MODEL, IN_COLS), D_MODEL ** -0.5),
        "pool_w": nrm(ks[3], (DEPTH, POOL_GROUPS, POOL_GROUP_DIM, POOL_GROUP_DIM), POOL_GROUP_DIM ** -0.5),
        "pool_scale": 1.0 + 0.1 * jax.random.normal(ks[4], (DEPTH, POOL_WIDTH), f32),
        "conv_w": nrm(ks[5], (DEPTH, CONV_WIDTH, 3 * DN_WIDTH), CONV_WIDTH ** -0.5),
        "a_log": a_log,
        "dt_bias": dt_bias,
        "dn_norm_w": gain(ks[6], (DEPTH, DN_HEAD_DIM)),
        "w_pool_up": nrm(ks[9], (DEPTH, POOL_WIDTH, D_MODEL), POOL_WIDTH ** -0.5),
        "w_dn_up": nrm(ks[10], (DEPTH, DN_WIDTH, D_MODEL), DN_WIDTH ** -0.5),
        "w_mix_out": nrm(ks[11], (DEPTH, D_MODEL, D_MODEL), D_MODEL ** -0.5),
        "ffn_norm_w": gain(ks[12], (DEPTH, D_MODEL)),
        "peer_w_query": nrm(ks[13], (DEPTH, D_MODEL, PEER_HEADS * PEER_QUERY_DIM), D_MODEL ** -0.5),
        "peer_keys_1": nrm(ks[14], (DEPTH, PEER_HEADS, N_KEYS, PEER_HALF), PEER_HALF ** -0.5),
        "peer_keys_2": nrm(ks[15], (DEPTH, PEER_HEADS, N_KEYS, PEER_HALF), PEER_HALF ** -0.5),
        "peer_down": nrm(ks[16], (DEPTH, N_EXPERTS, D_MODEL), D_MODEL ** -0.5),
        "peer_up": nrm(ks[17], (DEPTH, N_EXPERTS, D_MODEL), PEER_HEADS ** -0.5),
        "final_norm_w": gain(ks[18], (D_MODEL,)),
    }


def reference(x, mix_norm_w, w_in, pool_w, pool_scale, conv_w, a_log, dt_bias, dn_norm_w,
              w_pool_up, w_dn_up, w_mix_out, ffn_norm_w, peer_w_query, peer_keys_1,
              peer_keys_2, peer_down, peer_up, final_norm_w):
    h = x
    for l in range(DEPTH):
        xn = rmsnorm(h, mix_norm_w[l])
        proj = xn @ w_in[l]
        xa, qkv, z, beta_logit, a_logit, gate_a, gate_b = jnp.split(proj, SPLIT_POINTS, axis=-1)
        y_a = pool_mixer(xa, pool_w[l], pool_scale[l]) @ w_pool_up[l]
        y_b = deltanet_branch(qkv, z, beta_logit, a_logit, conv_w[l], a_log[l], dt_bias[l],
                              dn_norm_w[l]) @ w_dn_up[l]
        merged = jax.nn.sigmoid(gate_a) * y_a + jax.nn.sigmoid(gate_b) * y_b
        h = h + merged @ w_mix_out[l]
        h = h + peer_ffn(rmsnorm(h, ffn_norm_w[l]), peer_w_query[l], peer_keys_1[l],
                         peer_keys_2[l], peer_down[l], peer_up[l])
    return rmsnorm(h, final_norm_w)
```

```python
from contextlib import ExitStack
import numpy as np
import concourse.bass as bass
import concourse.mybir as mybir
from concourse.bass_utils import run_bass_kernel_spmd

F32 = mybir.dt.float32
BF16 = mybir.dt.bfloat16
AF = mybir.ActivationFunctionType
ALU = mybir.AluOpType

NEG = -30000.0
EPS = 1e-6
IN_COLS = 6672
NE = 16384


class Buf:
    def __init__(self):
        self.last_w = None
        self.readers = []


class Tn:
    def __init__(self, t, b=None, psum=False):
        self.t = t
        self.b = b if b is not None else Buf()
        self.psum = psum


class Sched:
    CE = ("pe", "act", "dve", "pool")

    def __init__(self, nc, ctx, ndma=8, nsets=2):
        self.nc = nc
        self.eng = {}
        self.sets = [{} for _ in range(nsets)]
        for nm, obj in (("pe", nc.tensor), ("act", nc.scalar), ("dve", nc.vector),
                        ("pool", nc.gpsimd), ("sp", nc.sync)):
            for k in range(nsets):
                if nm != "sp":
                    self.sets[k][nm] = ctx.enter_context(nc.semaphore(f"s{k}_" + nm))
            self.eng[nm] = dict(name=nm, obj=obj, sem=self.sets[0].get(nm), count=0, waited={})
        self.epoch = 0
        self.dq = {}
        for q in ("sp", "pool"):
            sems = [ctx.enter_context(nc.semaphore(f"d_{q}{i}")) for i in range(ndma)]
            self.dq[q] = dict(sems=sems, n=0)
        self.ndma = ndma
        self.ninst = 0

    def _wait(self, e, tok):
        sem, val, ep = tok
        if ep is not None and ep < self.epoch:
            return
        if e["name"] == "pe" and sem is e["sem"]:
            return
        key = id(sem)
        w = e["waited"]
        if w.get(key, 0) >= val:
            return
        e["obj"].wait_ge(sem, val)
        w[key] = val
        self.ninst += 1

    def _deps(self, e, reads, writes):
        for b in reads:
            if b.last_w is not None:
                self._wait(e, b.last_w)
        for b in writes:
            if b.last_w is not None:
                self._wait(e, b.last_w)
            for r in b.readers:
                self._wait(e, r)

    def _commit(self, tok, reads, writes):
        for b in reads:
            b.readers = [r for r in b.readers if r[0] is not tok[0]] + [tok]
        for b in writes:
            b.last_w = tok
            b.readers = []

    def op(self, en, fn, reads=(), writes=()):
        e = self.eng[en]
        rb = [x.b for x in reads]
        wb = [x.b for x in writes] + [x.b for x in reads if x.psum]
        self._deps(e, rb, wb)
        inst = fn(e["obj"])
        e["count"] += 1
        inst.then_inc(e["sem"], 1)
        tok = (e["sem"], e["count"], self.epoch)
        self._commit(tok, rb, wb)
        self.ninst += 1
        return tok

    def dma(self, q, out, in_, reads=(), writes=()):
        e = self.eng[q]
        d = self.dq[q]
        n = d["n"]
        sem = d["sems"][n % self.ndma]
        if n >= self.ndma:
            self._wait(e, (sem, 16 * (n // self.ndma), None))
        rb = [x.b for x in reads]
        wb = [x.b for x in writes]
        self._deps(e, rb, wb)
        e["obj"].dma_start(out=out, in_=in_).then_inc(sem, 16)
        d["n"] = n + 1
        tok = (sem, 16 * (n // self.ndma + 1), None)
        self._commit(tok, rb, wb)
        self.ninst += 1
        return tok

    def _sync_all(self):
        for a in self.CE + ("sp",):
            for b in self.CE:
                if a != b and self.eng[b]["count"] > 0:
                    self._wait(self.eng[a], (self.eng[b]["sem"], self.eng[b]["count"], self.epoch))

    def barrier(self, switch=False):
        self._sync_all()
        if not switch:
            return
        self.epoch += 1
        new = self.sets[self.epoch]
        for nm, e in self.eng.items():
            e["sem"] = new.get(nm)
            e["count"] = 0

    def finish(self):
        for q, d in self.dq.items():
            e = self.eng[q]
            for i, sem in enumerate(d["sems"]):
                cnt = (d["n"] - i + self.ndma - 1) // self.ndma
                if cnt > 0:
                    e["obj"].wait_ge(sem, 16 * cnt)


def build(S_TOK, dbg=False, stage=99, sw_every=2):
    NST = S_TOK // 256
    nc = bass.Bass("TRN2", target_bir_lowering=False)

    def din(name, shape):
        return nc.dram_tensor(name, list(shape), F32, kind="ExternalInput").ap()

    x_d = din("x", [S_TOK, 1024])
    win_d = din("w_in_p", [1024, IN_COLS])
    mnw_d = din("mnw", [128, 8])
    fnw_d = din("fnw", [128, 8])
    pw_d = din("pool_w_p", [128, 4, 128])
    psc_d = din("pool_scale_p", [128, 4])
    cw_d = din("conv_w_p", [128, 24, 4])
    alog_d = din("a_log_p", [128, 8])
    dtb_d = din("dt_bias_p", [128, 8])
    dnw_d = din("dn_norm_p", [128, 1])
    wpu_d = din("w_pool_up", [512, 1024])
    wdu_d = din("w_dn_up", [1024, 1024])
    wo_d = din("w_mix_out", [1024, 1024])
    wq_d = din("peer_w_query", [1024, 2048])
    kt_d = din("keys_t", [128, 16, 128])
    dnT_d = din("peer_down_t", [1024, NE])
    up_d = din("peer_up", [NE, 1024])
    fnl_d = din("final_w_p", [128, 1024])
    ident_d = din("c_ident", [128, 128])
    tri_d = din("c_tri", [128, 128])
    negu_d = din("c_negu", [128, 128])
    negls_d = din("c_negls", [128, 128])
    ones_d = din("c_ones", [128, 128])
    pfix_d = din("c_poolfix", [128, 4, 16])
    y_d = nc.dram_tensor("y", [S_TOK, 1024], F32, kind="ExternalOutput").ap()
    if dbg:
        h1_d = nc.dram_tensor("h1dbg", [S_TOK, 1024], F32, kind="ExternalOutput").ap()

    win_s = Tn(nc.dram_tensor("win_s", [128, 8, IN_COLS], BF16).ap())
    wq_s = Tn(nc.dram_tensor("wq_s", [128, 8, 2048], BF16).ap())
    dn_s = Tn(nc.dram_tensor("dn_s", [128, 8, NE], BF16).ap())
    up_s = Tn(nc.dram_tensor("up_s", [128, 128, 1024], BF16).ap())

    with ExitStack() as ctx:
        NEP = (NST + sw_every - 1) // sw_every
        S = Sched(nc, ctx, nsets=NEP + 1)

        uid = [0]

        def un_(name):
            uid[0] += 1
            return f"t{uid[0]}_{name}"

        def sb(cx, name, shape, dt=F32):
            return Tn(cx.enter_context(nc.sbuf_tensor(un_(name), list(shape), dt)))

        def ps(cx, name, shape, dt=F32):
            return Tn(cx.enter_context(nc.psum_tensor(un_(name), list(shape), dt)), psum=True)

        def mm(o, o_ap, l, l_ap, r, r_ap, start=True, stop=True):
            S.op("pe", lambda e: e.matmul(o_ap, lhsT=l_ap, rhs=r_ap, start=start, stop=stop),
                 reads=[l, r], writes=[o])

        def tr(o, o_ap, i, i_ap, idn):
            S.op("pe", lambda e: e.transpose(out=o_ap, in_=i_ap, identity=idn.t[:]),
                 reads=[i, idn], writes=[o])

        def act(o, o_ap, i, i_ap, func, reads=(), wr=(), **kw):
            S.op("act", lambda e: e.activation(out=o_ap, in_=i_ap, func=func, **kw),
                 reads=[i] + list(reads), writes=[o] + list(wr))

        def ts(en, o, o_ap, i, i_ap, s1, s2, op0, op1=None, reads=()):
            if op1 is None:
                S.op(en, lambda e: e.tensor_scalar(out=o_ap, in0=i_ap, scalar1=s1, scalar2=None, op0=op0),
                     reads=[i] + list(reads), writes=[o])
            else:
                S.op(en, lambda e: e.tensor_scalar(out=o_ap, in0=i_ap, scalar1=s1, scalar2=s2, op0=op0, op1=op1),
                     reads=[i] + list(reads), writes=[o])

        def tt(en, o, o_ap, a, a_ap, b, b_ap, op):
            S.op(en, lambda e: e.tensor_tensor(out=o_ap, in0=a_ap, in1=b_ap, op=op),
                 reads=[a, b], writes=[o])

        def stt(en, o, o_ap, a, a_ap, sc, b, b_ap, op0, op1, reads=()):
            S.op(en, lambda e: e.scalar_tensor_tensor(out=o_ap, in0=a_ap, scalar=sc, in1=b_ap, op0=op0, op1=op1),
                 reads=[a, b] + list(reads), writes=[o])

        def cp(en, o, o_ap, i, i_ap):
            if en == "act":
                S.op("act", lambda e: e.copy(out=o_ap, in_=i_ap), reads=[i], writes=[o])
            else:
                S.op(en, lambda e: e.tensor_copy(out=o_ap, in_=i_ap), reads=[i], writes=[o])

        def rsqrt_col(o, i, scale, eps, reads=()):
            ts("dve", o, o.t[:], i, i.t[:], scale, eps, ALU.mult, ALU.add)
            act(o, o.t[:], o, o.t[:], AF.Sqrt)
            S.op("dve", lambda e: e.reciprocal(out=o.t[:], in_=o.t[:]), reads=[o], writes=[o])

        ident_f = sb(ctx, "ident_f", [128, 128])
        ident_b = sb(ctx, "ident_b", [128, 128], BF16)
        tri = sb(ctx, "tri", [128, 128])
        negu = sb(ctx, "negu", [128, 128])
        negls = sb(ctx, "negls", [128, 128])
        ones = sb(ctx, "ones", [128, 128])
        pfix = sb(ctx, "pfix", [128, 4, 16])
        mnw = sb(ctx, "mnw", [128, 8])
        fnw = sb(ctx, "fnw", [128, 8])
        psc = sb(ctx, "psc", [128, 4])
        cw = sb(ctx, "cw", [128, 24, 4])
        nA = sb(ctx, "nA", [128, 8])
        dtb = sb(ctx, "dtb", [128, 8])
        dnw = sb(ctx, "dnw", [128, 1])
        fnl = sb(ctx, "fnl", [128, 1024])
        wpu = sb(ctx, "wpu", [128, 4, 1024], BF16)
        wdu = sb(ctx, "wdu", [128, 8, 1024], BF16)
        wo = sb(ctx, "wo", [128, 8, 1024], BF16)
        pw = sb(ctx, "pw", [128, 4, 128], BF16)
        wsm = sb(ctx, "wsm", [128, 8, 16], BF16)
        KT = sb(ctx, "KT", [128, 16, 128], BF16)
        wpool = [sb(ctx, f"wpool{i}", [128, 4096], BF16) for i in range(5)]
        wp_n = [0]
        Sst = [sb(ctx, f"Sst{h}", [128, 128]) for h in range(8)]
        ccar = [sb(ctx, f"ccar{b}", [128, 3]) for b in range(24)]
        xab = [sb(ctx, f"xab{g}", [128, 272]) for g in range(4)]
        xh = [sb(ctx, f"xh{i}", [128, 1024]) for i in range(2)]
        yo_p = [sb(ctx, f"yo{i}", [128, 1024]) for i in range(2)]

        def wload(src, src_ap, view):
            t = wpool[wp_n[0] % len(wpool)]
            wp_n[0] += 1
            S.dma("sp", view(t.t), src_ap, reads=[src], writes=[t])
            return t

        v8 = lambda t: t[:].rearrange("p (c n) -> p c n", c=8)
        v4 = lambda t: t[:].rearrange("p (c n) -> p c n", c=4)

        dummy = Tn(None)
        for (t, d) in ((ident_f, ident_d), (tri, tri_d), (negu, negu_d), (negls, negls_d), (ones, ones_d),
                       (pfix, pfix_d), (mnw, mnw_d), (fnw, fnw_d), (psc, psc_d), (cw, cw_d), (nA, alog_d),
                       (dtb, dtb_d), (dnw, dnw_d), (fnl, fnl_d)):
            S.dma("sp", t.t[:], d, writes=[t])
        cp("dve", ident_b, ident_b.t[:], ident_f, ident_f.t[:])
        act(nA, nA.t[:], nA, nA.t[:], AF.Exp)
        ts("dve", nA, nA.t[:], nA, nA.t[:], -1.0, None, ALU.mult)
        for h in range(8):
            S.op("dve", lambda e: e.memset(Sst[h].t[:], 0.0), writes=[Sst[h]])
        for b in range(24):
            S.op("pool", lambda e: e.memset(ccar[b].t[:], 0.0), writes=[ccar[b]])
        for g in range(4):
            S.op("pool", lambda e: e.memset(xab[g].t[:], 0.0), writes=[xab[g]])

        with ExitStack() as pc:
            stg = [sb(pc, f"stg{i}", [128, 4096]) for i in range(2)]
            sn = [0]

            def prep(src_ap, shape3, dst, dst_ap, scale_t=None, scale_ap=None):
                a, b = shape3
                st_ = stg[sn[0] % 2]
                sn[0] += 1
                sv = st_.t[:, 0:a * b].rearrange("p (a b) -> p a b", a=a)
                S.dma("sp", sv, src_ap, writes=[st_])
                ob = wpool[wp_n[0] % len(wpool)]
                wp_n[0] += 1
                ov = ob.t[:, 0:a * b].rearrange("p (a b) -> p a b", a=a)
                en = "dve" if sn[0] % 2 == 0 else "pool"
                if scale_t is None:
                    cp("act" if sn[0] % 2 == 0 else "dve", ob, ov, st_, sv)
                else:
                    tt(en, ob, ov, st_, sv, scale_t, scale_ap, ALU.mult)
                if dst is None:
                    return ob, ov
                S.dma("pool", dst_ap, ov, reads=[ob], writes=[dst])
                return ob, ov

            for (wt, d, a, b, rs) in ((wpu, wpu_d, 4, 1024, "(g p) n -> p g n"),
                                      (wo, wo_d, 8, 1024, "(c p) n -> p c n")):
                for half in range(a // 4):
                    ob, ov = prep(d.rearrange(rs, p=128)[:, half * 4:(half + 1) * 4, :], (4, 1024), None, None)
                    cp("dve", wt, wt.t[:, half * 4:(half + 1) * 4, :], ob, ov)
            for half in range(2):
                ob, ov = prep(wdu_d.rearrange("(h p) n -> p h n", p=128)[:, half * 4:(half + 1) * 4, :], (4, 1024),
                              None, None, dnw, dnw.t[:, 0:1].unsqueeze(2).to_broadcast([128, 4, 1024]))
                cp("dve", wdu, wdu.t[:, half * 4:(half + 1) * 4, :], ob, ov)
            ob, ov = prep(pw_d, (4, 128), None, None)
            cp("dve", pw, pw.t[:], ob, ov)
            ob, ov = prep(kt_d, (16, 128), None, None)
            cp("dve", KT, KT.t[:], ob, ov)
            winv = win_d.rearrange("(c p) n -> p c n", p=128)
            for blk in range(14):
                c0 = blk * 512
                n = min(512, IN_COLS - c0)
                ob, ov = prep(winv[:, :, c0:c0 + n], (8, n), win_s, win_s.t[:, :, c0:c0 + n],
                              mnw, mnw.t[:].unsqueeze(2).to_broadcast([128, 8, n]))
                if blk == 13:
                    cp("dve", wsm, wsm.t[:], ob, ov)
            wqv = wq_d.rearrange("(c p) n -> p c n", p=128)
            for blk in range(4):
                prep(wqv[:, :, blk * 512:(blk + 1) * 512], (8, 512), wq_s, wq_s.t[:, :, blk * 512:(blk + 1) * 512],
                     fnw, fnw.t[:].unsqueeze(2).to_broadcast([128, 8, 512]))
            dnv = dnT_d.rearrange("(c p) e -> p c e", p=128)
            upv = up_d.rearrange("(ch p) d -> p ch d", p=128)
            for blk in range(32 if stage >= 2 else 0):
                prep(dnv[:, :, blk * 512:(blk + 1) * 512], (8, 512), dn_s, dn_s.t[:, :, blk * 512:(blk + 1) * 512],
                     fnw, fnw.t[:].unsqueeze(2).to_broadcast([128, 8, 512]))
                prep(upv[:, blk * 4:(blk + 1) * 4, :], (4, 1024), up_s, up_s.t[:, blk * 4:(blk + 1) * 4, :])
        S.barrier(switch=True)

        def mixer(st):
            t0 = st * 256
            with ExitStack() as mx:
                pTb = ps(mx, "pTb", [128, 1024], BF16)
                big = [ps(mx, f"big{k}", [128, 512]) for k in range(4)]
                bn = [0]
                qbank = [mx.enter_context(nc.psum_tensor(un_(f"qb{k}"), [128, 512], F32)) for k in range(3)]
                qsl = [Tn(qbank[k], psum=True) for k in range(3)]
                qn = [0]

                def nbig():
                    b = big[bn[0] % 4]
                    bn[0] += 1
                    return b

                def nq():
                    k = qn[0] % 12
                    qn[0] += 1
                    s = qsl[k % 3]
                    qq = k // 3
                    return s, s.t[:, qq * 128:(qq + 1) * 128]

                sq = sb(mx, "sq", [128, 1024])
                xsb = sb(mx, "xsb", [128, 1024], BF16)
                xT = sb(mx, "xT", [128, 8, 256], BF16)
                ss = sb(mx, "ss", [128, 1])
                rstd = sb(mx, "rstd", [128, 1])
                raw = [sb(mx, f"raw{k}", [128, 259]) for k in range(3)]
                cacc = sb(mx, "cacc", [128, 256])
                qkvT = [sb(mx, f"qkvT{k}", [128, 256]) for k in range(3)]
                szT = sb(mx, "szT", [128, 256])
                sqn = sb(mx, "sqn", [128, 256])
                rn = sb(mx, "rn", [128, 256])
                lg = [sb(mx, f"lg{i}", [128, 16]) for i in range(2)]
                beta = [sb(mx, f"beta{i}", [128, 8]) for i in range(2)]
                nbeta = [sb(mx, f"nbeta{i}", [128, 8]) for i in range(2)]
                gg = [sb(mx, f"gg{i}", [128, 8]) for i in range(2)]
                gc = [sb(mx, f"gc{i}", [128, 8]) for i in range(2)]
                egl = [sb(mx, f"egl{i}", [128, 8]) for i in range(2)]
                kds = [sb(mx, f"kds{i}", [128, 8]) for i in range(2)]
                bgs = [sb(mx, f"bgs{i}", [128, 8]) for i in range(2)]
                kbg = sb(mx, "kbg", [128, 128])
                kdec = sb(mx, "kdec", [128, 128])
                vb = sb(mx, "vb", [128, 128])
                trig = sb(mx, "trig", [128, 128])
                Am = sb(mx, "Am", [128, 128])
                EU = sb(mx, "EU", [128, 128])
                DLs = sb(mx, "DLs", [128, 128])
                egrow = sb(mx, "egrow", [128, 128])
                Mb = [sb(mx, f"Mb{k}", [128, 128]) for k in range(2)]
                MTb = [sb(mx, f"MTb{k}", [128, 128]) for k in range(2)]
                PTb = [sb(mx, f"PTb{k}", [128, 128]) for k in range(2)]
                attnT = sb(mx, "attnT", [128, 128])
                qgT = sb(mx, "qgT", [128, 128])
                nwT = sb(mx, "nwT", [128, 128])
                vn = sb(mx, "vn", [128, 128])
                oss = sb(mx, "oss", [128, 1])
                orst = sb(mx, "orst", [128, 1])
                osq = sb(mx, "osq", [128, 128])
                onb = sb(mx, "onb", [128, 128], BF16)
                onT = sb(mx, "onT", [128, 8, 256], BF16)
                pa = [sb(mx, f"pa{k}", [128, 272]) for k in range(2)]
                pooledb = sb(mx, "pooledb", [128, 256], BF16)
                paT = sb(mx, "paT", [128, 4, 256], BF16)
                sga = sb(mx, "sga", [128, 256])
                m1 = sb(mx, "m1", [128, 256])
                sgb = sb(mx, "sgb", [128, 256])
                m2 = sb(mx, "m2", [128, 256])
                mT = sb(mx, "mT", [128, 8, 256], BF16)

                for i in range(2):
                    S.dma("sp", xh[i].t[:], x_d[t0 + i * 128:t0 + (i + 1) * 128, :], writes=[xh[i]])
                    act(sq, sq.t[:], xh[i], xh[i].t[:], AF.Square, wr=[ss], accum_out=ss.t[:])
                    rsqrt_col(rstd, ss, 1.0 / 1024, EPS)
                    ts("dve", xsb, xsb.t[:], xh[i], xh[i].t[:], rstd.t[:, 0:1], None, ALU.mult, reads=[rstd])
                    for c in range(8):
                        tr(pTb, pTb.t[:, c * 128:(c + 1) * 128], xsb, xsb.t[:, c * 128:(c + 1) * 128], ident_b)
                    cp("act", xT, xT.t[:, :, i * 128:(i + 1) * 128], pTb,
                       pTb.t[:].rearrange("p (c n) -> p c n", c=8))

                for i in range(2):
                    s_, s_ap = nq()
                    for c in range(8):
                        mm(s_, s_ap[:, 0:16], xT, xT.t[:, c, i * 128:(i + 1) * 128], wsm, wsm.t[:, c, :],
                           start=(c == 0), stop=(c == 7))
                    cp("dve", lg[i], lg[i].t[:], s_, s_ap[:, 0:16])
                    act(beta[i], beta[i].t[:], lg[i], lg[i].t[:, 0:8], AF.Sigmoid)
                    ts("dve", nbeta[i], nbeta[i].t[:], beta[i], beta[i].t[:], -1.0, None, ALU.mult)
                    tt("dve", gg[i], gg[i].t[:], lg[i], lg[i].t[:, 8:16], dtb, dtb.t[:], ALU.add)
                    act(gg[i], gg[i].t[:], gg[i], gg[i].t[:], AF.Exp)
                    act(gg[i], gg[i].t[:], gg[i], gg[i].t[:], AF.Ln, bias=1.0)
                    tt("dve", gg[i], gg[i].t[:], gg[i], gg[i].t[:], nA, nA.t[:], ALU.mult)
                    s_, s_ap = nq()
                    mm(s_, s_ap[:, 0:8], tri, tri.t[:], gg[i], gg[i].t[:])
                    cp("dve", gc[i], gc[i].t[:], s_, s_ap[:, 0:8])
                    s_, s_ap = nq()
                    mm(s_, s_ap[:, 0:8], ones, ones.t[:], gg[i], gg[i].t[:])
                    tt("dve", kds[i], kds[i].t[:], s_, s_ap[:, 0:8], gc[i], gc[i].t[:], ALU.subtract)
                    act(kds[i], kds[i].t[:], kds[i], kds[i].t[:], AF.Exp)
                    act(egl[i], egl[i].t[:], s_, s_ap[:, 0:8], AF.Exp)
                    act(bgs[i], bgs[i].t[:], gc[i], gc[i].t[:], AF.Exp)
                    tt("dve", bgs[i], bgs[i].t[:], bgs[i], bgs[i].t[:], beta[i], beta[i].t[:], ALU.mult)

                for h in range(8):
                    wb = wload(win_s, win_s.t[:, :, h * 512:(h + 1) * 512], v8)
                    wv = v8(wb.t)
                    for k in range(4):
                        pb = nbig()
                        for c in range(8):
                            mm(pb, pb.t[:, 0:256], wb, wv[:, c, k * 128:(k + 1) * 128], xT, xT.t[:, c, :],
                               start=(c == 0), stop=(c == 7))
                        if k == 3:
                            act(szT, szT.t[:], pb, pb.t[:, 0:256], AF.Silu)
                            continue
                        blk = k * 8 + h
                        r = raw[k]
                        cp("pool", r, r.t[:, 0:3], ccar[blk], ccar[blk].t[:])
                        cp("act", r, r.t[:, 3:259], pb, pb.t[:, 0:256])
                        cp("pool", ccar[blk], ccar[blk].t[:], r, r.t[:, 256:259])
                        ts("dve", cacc, cacc.t[:], r, r.t[:, 0:256], cw.t[:, blk, 0:1], None, ALU.mult, reads=[cw])
                        for j in range(1, 4):
                            stt("dve", cacc, cacc.t[:], r, r.t[:, j:j + 256], cw.t[:, blk, j:j + 1], cacc, cacc.t[:],
                                ALU.mult, ALU.add, reads=[cw])
                        act(qkvT[k], qkvT[k].t[:], cacc, cacc.t[:], AF.Silu)
                        if k < 2:
                            tt("dve", sqn, sqn.t[:], qkvT[k], qkvT[k].t[:], qkvT[k], qkvT[k].t[:], ALU.mult)
                            pn = nbig()
                            mm(pn, pn.t[:, 0:256], ones, ones.t[:], sqn, sqn.t[:])
                            ts("dve", rn, rn.t[:], pn, pn.t[:, 0:256], EPS, None, ALU.add)
                            act(rn, rn.t[:], rn, rn.t[:], AF.Sqrt)
                            S.op("dve", lambda e: e.reciprocal(out=rn.t[:], in_=rn.t[:]), reads=[rn], writes=[rn])
                            sc = (128.0 ** -0.5) if k == 0 else 1.0
                            stt("dve", qkvT[k], qkvT[k].t[:], qkvT[k], qkvT[k].t[:], sc, rn, rn.t[:],
                                ALU.mult, ALU.mult)
                    qT_, kT_, vT_ = qkvT
                    for i in range(2):
                        sl = slice(i * 128, (i + 1) * 128)
                        hh = slice(h, h + 1)
                        k_s, k_ap = nq()
                        tr(k_s, k_ap, kT_, kT_.t[:, sl], ident_f)
                        v_s, v_ap = nq()
                        tr(v_s, v_ap, vT_, vT_.t[:, sl], ident_f)
                        g_s, g_ap = nq()
                        mm(g_s, g_ap, kT_, kT_.t[:, sl], kT_, kT_.t[:, sl])
                        a_s, a_ap = nq()
                        mm(a_s, a_ap, kT_, kT_.t[:, sl], qT_, qT_.t[:, sl])
                        ts("dve", kbg, kbg.t[:], k_s, k_ap, bgs[i].t[:, hh], None, ALU.mult, reads=[bgs[i]])
                        act(kdec, kdec.t[:], k_s, k_ap, AF.Copy, reads=[kds[i]], scale=kds[i].t[:, hh])
                        act(vb, vb.t[:], v_s, v_ap, AF.Copy, reads=[beta[i]], scale=beta[i].t[:, hh])
                        ts("pool", trig, trig.t[:], tri, tri.t[:], gg[i].t[:, hh], None, ALU.mult, reads=[gg[i]])
                        r_s, r_ap = nq()
                        mm(r_s, r_ap, ones, ones.t[:], trig, trig.t[:])
                        ts("dve", Am, Am.t[:], r_s, r_ap, gc[i].t[:, hh], None, ALU.subtract, reads=[gc[i]])
                        act(egrow, egrow.t[:], r_s, r_ap, AF.Exp)
                        tt("pool", EU, EU.t[:], Am, Am.t[:], negu, negu.t[:], ALU.add)
                        act(EU, EU.t[:], EU, EU.t[:], AF.Exp)
                        tt("pool", DLs, DLs.t[:], negls, negls.t[:], Am, Am.t[:], ALU.subtract)
                        act(DLs, DLs.t[:], DLs, DLs.t[:], AF.Exp)
                        M, MT, PT = Mb[0], MTb[0], PTb[0]
                        stt("dve", M, M.t[:], g_s, g_ap, nbeta[i].t[:, hh], DLs, DLs.t[:], ALU.mult, ALU.mult,
                            reads=[nbeta[i]])
                        tt("dve", attnT, attnT.t[:], a_s, a_ap, EU, EU.t[:], ALU.mult)
                        tt("pool", qgT, qgT.t[:], qT_, qT_.t[:, sl], egrow, egrow.t[:], ALU.mult)
                        t_s, t_ap = nq()
                        tr(t_s, t_ap, M, M.t[:], ident_f)
                        cp("act", MT, MT.t[:], t_s, t_ap)
                        tt("dve", PT, PT.t[:], t_s, t_ap, ident_f, ident_f.t[:], ALU.add)
                        for kk in range(6):
                            Mn, MTn, PTn = Mb[(kk + 1) % 2], MTb[(kk + 1) % 2], PTb[(kk + 1) % 2]
                            s1, s1ap = nq()
                            mm(s1, s1ap, MT, MT.t[:], M, M.t[:])
                            cp("act", Mn, Mn.t[:], s1, s1ap)
                            if kk < 5:
                                s2, s2ap = nq()
                                mm(s2, s2ap, M, M.t[:], MT, MT.t[:])
                                cp("dve", MTn, MTn.t[:], s2, s2ap)
                            s3, s3ap = nq()
                            mm(s3, s3ap, Mn, Mn.t[:], PT, PT.t[:])
                            tt("dve", PTn, PTn.t[:], s3, s3ap, PT, PT.t[:], ALU.add)
                            M, MT, PT = Mn, MTn, PTn
                        w_s, w_ap = nq()
                        mm(w_s, w_ap, kbg, kbg.t[:], PT, PT.t[:])
                        S.op("act", lambda e: e.mul(out=nwT.t[:], in_=w_ap, mul=-1.0), reads=[w_s], writes=[nwT])
                        n_s, n_ap = nq()
                        mm(n_s, n_ap, PT, PT.t[:], vb, vb.t[:], start=True, stop=False)
                        mm(n_s, n_ap, nwT, nwT.t[:], Sst[h], Sst[h].t[:], start=False, stop=True)
                        cp("dve", vn, vn.t[:], n_s, n_ap)
                        o_s, o_ap = nq()
                        mm(o_s, o_ap, qgT, qgT.t[:], Sst[h], Sst[h].t[:], start=True, stop=False)
                        mm(o_s, o_ap, attnT, attnT.t[:], vn, vn.t[:], start=False, stop=True)
                        u_s, u_ap = nq()
                        mm(u_s, u_ap, kdec, kdec.t[:], vn, vn.t[:])
                        stt("dve", Sst[h], Sst[h].t[:], Sst[h], Sst[h].t[:], egl[i].t[:, hh], u_s, u_ap,
                            ALU.mult, ALU.add, reads=[egl[i]])
                        act(osq, osq.t[:], o_s, o_ap, AF.Square, wr=[oss], accum_out=oss.t[:])
                        rsqrt_col(orst, oss, 1.0 / 128, EPS)
                        act(onb, onb.t[:], o_s, o_ap, AF.Copy, reads=[orst], scale=orst.t[:, 0:1])
                        pslot = pTb.t[:, (h % 8) * 128:(h % 8 + 1) * 128]
                        tr(pTb, pslot, onb, onb.t[:], ident_b)
                        tt("dve", onT, onT.t[:, h, sl], pTb, pslot, szT, szT.t[:, sl], ALU.mult)

                wb = wload(win_s, win_s.t[:, :, 4096:4608], v8)
                wv = v8(wb.t)
                for g in range(4):
                    win_ = 2 << g
                    pb = nbig()
                    for c in range(8):
                        mm(pb, pb.t[:, 0:256], wb, wv[:, c, g * 128:(g + 1) * 128], xT, xT.t[:, c, :],
                           start=(c == 0), stop=(c == 7))
                    xg = xab[g]
                    cp("pool", xg, xg.t[:, 0:16], xg, xg.t[:, 256:272])
                    cp("act", xg, xg.t[:, 16:272], pb, pb.t[:, 0:256])
                    src = xg
                    sh = 1
                    for stp in range(g + 1):
                        dst = pa[stp % 2]
                        lo = 2 * sh - 1
                        tt("dve", dst, dst.t[:, lo:272], src, src.t[:, lo:272], src, src.t[:, lo - sh:272 - sh], ALU.add)
                        src = dst
                        sh *= 2
                    if st == 0:
                        tt("dve", src, src.t[:, 16:32], src, src.t[:, 16:32], pfix, pfix.t[:, g, :], ALU.mult)
                    stt("dve", pooledb, pooledb.t[:], src, src.t[:, 16:272], 1.0 / win_, xg, xg.t[:, 16:272],
                        ALU.mult, ALU.subtract)
                    pb2 = nbig()
                    mm(pb2, pb2.t[:, 0:256], pw, pw.t[:, g, :], pooledb, pooledb.t[:])
                    act(paT, paT.t[:, g, :], pb2, pb2.t[:, 0:256], AF.Copy, reads=[psc], scale=psc.t[:, g:g + 1])

                wga = [wload(win_s, win_s.t[:, :, 4608 + k * 512:4608 + (k + 1) * 512], v8) for k in range(2)]
                wgb = [wload(win_s, win_s.t[:, :, 5632 + k * 512:5632 + (k + 1) * 512], v8) for k in range(2)]
                for j in range(8):
                    js = slice(j * 128, (j + 1) * 128)
                    jw = slice((j % 4) * 128, (j % 4 + 1) * 128)
                    pA, pB, pC, pD = big[0], big[1], big[2], big[3]
                    for g in range(4):
                        mm(pA, pA.t[:, 0:256], wpu, wpu.t[:, g, js], paT, paT.t[:, g, :], start=(g == 0), stop=(g == 3))
                    wa = wga[j // 4]
                    for c in range(8):
                        mm(pB, pB.t[:, 0:256], wa, v8(wa.t)[:, c, jw], xT, xT.t[:, c, :], start=(c == 0), stop=(c == 7))
                    for hd in range(8):
                        mm(pC, pC.t[:, 0:256], wdu, wdu.t[:, hd, js], onT, onT.t[:, hd, :], start=(hd == 0), stop=(hd == 7))
                    wg = wgb[j // 4]
                    for c in range(8):
                        mm(pD, pD.t[:, 0:256], wg, v8(wg.t)[:, c, jw], xT, xT.t[:, c, :], start=(c == 0), stop=(c == 7))
                    act(sga, sga.t[:], pB, pB.t[:, 0:256], AF.Sigmoid)
                    tt("dve", m1, m1.t[:], pA, pA.t[:, 0:256], sga, sga.t[:], ALU.mult)
                    act(sgb, sgb.t[:], pD, pD.t[:, 0:256], AF.Sigmoid)
                    tt("dve", m2, m2.t[:], pC, pC.t[:, 0:256], sgb, sgb.t[:], ALU.mult)
                    tt("pool", mT, mT.t[:, j, :], m1, m1.t[:], m2, m2.t[:], ALU.add)

                for i in range(2):
                    for n in range(2):
                        pb = nbig()
                        for k in range(8):
                            mm(pb, pb.t[:], mT, mT.t[:, k, i * 128:(i + 1) * 128], wo, wo.t[:, k, n * 512:(n + 1) * 512],
                               start=(k == 0), stop=(k == 7))
                        tt("dve", xh[i], xh[i].t[:, n * 512:(n + 1) * 512], pb, pb.t[:],
                           xh[i], xh[i].t[:, n * 512:(n + 1) * 512], ALU.add)
                    if dbg:
                        S.dma("pool", h1_d[t0 + i * 128:t0 + (i + 1) * 128, :], xh[i].t[:], reads=[xh[i]], writes=[Tn(None)])

        def peer(st):
            t0 = st * 256
            with ExitStack() as px:
                pY = [[ps(px, f"pY{i}{n}", [128, 512]) for n in range(2)] for i in range(2)]
                pU = [ps(px, f"pU{k}", [128, 512]) for k in range(2)]
                pTb = ps(px, "pTbp", [128, 1024], BF16)
                pM = ps(px, "pM", [128, 512])
                sq = sb(px, "psq", [128, 1024])
                hsb = sb(px, "hsb", [128, 1024], BF16)
                xn2T = sb(px, "xn2T", [128, 8, 256], BF16)
                ss = sb(px, "pss", [128, 1])
                rstd = sb(px, "prstd", [128, 1])
                qT = sb(px, "pqT", [128, 16, 256], BF16)
                sc = [sb(px, f"sc{i}", [128, 2048]) for i in range(2)]
                v16 = [sb(px, f"v16{k}", [128, 16]) for k in range(2)]
                tmp128 = sb(px, "tmp128", [128, 128])
                cand = sb(px, "cand", [128, 256])
                cand2 = sb(px, "cand2", [128, 256])
                c16 = sb(px, "c16", [128, 16])
                ex16 = sb(px, "ex16", [128, 16])
                negm = sb(px, "negm", [128, 1])
                zz = sb(px, "zz", [128, 1])
                taum = [sb(px, f"taum{i}", [128, 8]) for i in range(2)]
                ttau = [sb(px, f"ttau{i}", [128, 8]) for i in range(2)]
                cand3 = sb(px, "cand3", [128, 256])
                c24 = sb(px, "c24", [128, 8])
                tsum = sb(px, "tsum", [128, 1])
                E1n = [sb(px, f"E1n{i}", [128, 8, 128]) for i in range(2)]
                E2 = [sb(px, f"E2{i}", [128, 8, 128]) for i in range(2)]
                gmb = [sb(px, f"gmb{k}", [128, 512], BF16) for k in range(3)]
                ncc = [sb(px, f"ncc{i}", [128, 8]) for i in range(2)]
                Ag = [sb(px, f"Ag{k}", [128, 512]) for k in range(2)]
                Tt = [sb(px, f"Tt{k}", [128, 512]) for k in range(4)]
                GA = sb(px, "GA", [128, 512], BF16)
                GAT = [sb(px, f"GAT{k}", [128, 4, 128], BF16) for k in range(2)]
                hf = sb(px, "hf", [128, 1024])

                for i in range(2):
                    act(sq, sq.t[:], xh[i], xh[i].t[:], AF.Square, wr=[ss], accum_out=ss.t[:])
                    rsqrt_col(rstd, ss, 1.0 / 1024, EPS)
                    ts("dve", hsb, hsb.t[:], xh[i], xh[i].t[:], rstd.t[:, 0:1], None, ALU.mult, reads=[rstd])
                    for c in range(8):
                        tr(pTb, pTb.t[:, c * 128:(c + 1) * 128], hsb, hsb.t[:, c * 128:(c + 1) * 128], ident_b)
                    cp("act", xn2T, xn2T.t[:, :, i * 128:(i + 1) * 128], pTb,
                       pTb.t[:].rearrange("p (c n) -> p c n", c=8))
                for blk in range(4):
                    wb = wload(wq_s, wq_s.t[:, :, blk * 512:(blk + 1) * 512], v8)
                    wv = v8(wb.t)
                    for jj in range(4):
                        jb = blk * 4 + jj
                        for c in range(8):
                            mm(pM, pM.t[:, 0:256], wb, wv[:, c, jj * 128:(jj + 1) * 128], xn2T, xn2T.t[:, c, :],
                               start=(c == 0), stop=(c == 7))
                        cp("act" if jb % 2 == 0 else "dve", qT, qT.t[:, jb, :], pM, pM.t[:, 0:256])
                for i in range(2):
                    for b4 in range(4):
                        for jj in range(4):
                            jb = b4 * 4 + jj
                            mm(pM, pM.t[:, jj * 128:(jj + 1) * 128], qT, qT.t[:, jb, i * 128:(i + 1) * 128],
                               KT, KT.t[:, jb, :])
                        cp("act", sc[i], sc[i].t[:, b4 * 512:(b4 + 1) * 512], pM, pM.t[:])
                for i in range(2):
                    for h in range(8):
                        for half in range(2):
                            sv = sc[i].t[:, (h * 2 + half) * 128:(h * 2 + half + 1) * 128]
                            vv = v16[half]
                            S.op("dve", lambda e: e.max(out=vv.t[:, 0:8], in_=sv), reads=[sc[i]], writes=[vv])
                            S.op("dve", lambda e: e.match_replace(out=tmp128.t[:], in_to_replace=vv.t[:, 0:8],
                                                                  in_values=sv, imm_value=-1e30),
                                 reads=[sc[i], vv], writes=[tmp128])
                            S.op("dve", lambda e: e.max(out=vv.t[:, 8:16], in_=tmp128.t[:]), reads=[tmp128], writes=[vv])
                        tt("dve", cand, cand.t[:].rearrange("p (a b) -> p a b", a=16),
                           v16[0], v16[0].t[:].unsqueeze(2).to_broadcast([128, 16, 16]),
                           v16[1], v16[1].t[:].unsqueeze(1).to_broadcast([128, 16, 16]), ALU.add)
                        S.op("dve", lambda e: e.max(out=c16.t[:, 0:8], in_=cand.t[:]), reads=[cand], writes=[c16])
                        S.op("dve", lambda e: e.match_replace(out=cand2.t[:], in_to_replace=c16.t[:, 0:8],
                                                              in_values=cand.t[:], imm_value=-1e30),
                             reads=[cand, c16], writes=[cand2])
                        S.op("dve", lambda e: e.max(out=c16.t[:, 8:16], in_=cand2.t[:]), reads=[cand2], writes=[c16])
                        ts("dve", negm, negm.t[:], c16, c16.t[:, 0:1], -1.0, None, ALU.mult)
                        act(ex16, ex16.t[:], c16, c16.t[:], AF.Exp, reads=[negm], wr=[zz], bias=negm.t[:, 0:1],
                            accum_out=zz.t[:])
                        act(zz, zz.t[:], zz, zz.t[:], AF.Ln)
                        tt("dve", ncc[i], ncc[i].t[:, h:h + 1], negm, negm.t[:], zz, zz.t[:], ALU.subtract)
                        S.op("dve", lambda e: e.match_replace(out=cand3.t[:], in_to_replace=c16.t[:, 8:16],
                                                              in_values=cand2.t[:], imm_value=-1e30),
                             reads=[cand2, c16], writes=[cand3])
                        S.op("dve", lambda e: e.max(out=c24.t[:], in_=cand3.t[:]), reads=[cand3], writes=[c24])
                        tt("dve", tsum, tsum.t[:], c16, c16.t[:, 15:16], c24, c24.t[:, 0:1], ALU.add)
                        ts("dve", taum[i], taum[i].t[:, h:h + 1], tsum, tsum.t[:], 0.5, None, ALU.mult)
                    tt("dve", ttau[i], ttau[i].t[:], taum[i], taum[i].t[:], ncc[i], ncc[i].t[:], ALU.add)
                    act(ttau[i], ttau[i].t[:], ttau[i], ttau[i].t[:], AF.Exp)
                    for h in range(8):
                        act(E1n[i], E1n[i].t[:, h, :], sc[i], sc[i].t[:, (h * 2) * 128:(h * 2 + 1) * 128], AF.Exp,
                            reads=[ncc[i]], bias=ncc[i].t[:, h:h + 1])
                    act(E2[i], E2[i].t[:], sc[i],
                        sc[i].t[:].rearrange("p (h t k) -> p h t k", h=8, t=2)[:, :, 1, :], AF.Exp)
                un = 0
                tn = 0
                gn = 0
                pAcc = pM
                for eb in range(32):
                    dnb = wload(dn_s, dn_s.t[:, :, eb * 512:(eb + 1) * 512], v8)
                    upb = wload(up_s, up_s.t[:, eb * 4:(eb + 1) * 4, :], v4)
                    dv = v8(dnb.t)
                    uv = v4(upb.t)
                    for i in range(2):
                        pu = pU[un % 2]
                        ag = Ag[un % 2]
                        gat = GAT[un % 2]
                        un += 1
                        for c in range(8):
                            mm(pu, pu.t[:], xn2T, xn2T.t[:, c, i * 128:(i + 1) * 128], dnb, dv[:, c, :],
                               start=(c == 0), stop=(c == 7))
                        act(ag, ag.t[:], pu, pu.t[:], AF.Gelu)
                        for h in range(8):
                            Tb = Tt[tn % 4]
                            tn += 1
                            if h < 5:
                                for q in range(4):
                                    act(Tb, Tb.t[:, q * 128:(q + 1) * 128], E2[i], E2[i].t[:, h, :], AF.Copy,
                                        reads=[E1n[i]], scale=E1n[i].t[:, h, eb * 4 + q:eb * 4 + q + 1])
                            else:
                                tt("pool" if h == 5 else "dve", Tb, Tb.t[:].rearrange("p (a b) -> p a b", a=4),
                                   E1n[i], E1n[i].t[:, h, eb * 4:eb * 4 + 4].unsqueeze(2).to_broadcast([128, 4, 128]),
                                   E2[i], E2[i].t[:, h, :].unsqueeze(1).to_broadcast([128, 4, 128]), ALU.mult)
                            gm = gmb[gn % 3]
                            gn += 1
                            stt("dve", gm, gm.t[:], Tb, Tb.t[:], ttau[i].t[:, h:h + 1], Tb, Tb.t[:],
                                ALU.is_ge, ALU.mult, reads=[ttau[i]])
                            mm(pAcc, pAcc.t[:], ident_b, ident_b.t[:], gm, gm.t[:], start=(h == 0), stop=(h == 7))
                        tt("dve", GA, GA.t[:], pAcc, pAcc.t[:], ag, ag.t[:], ALU.mult)
                        half = (un % 2) * 512
                        for q in range(4):
                            tr(pTb, pTb.t[:, half + q * 128:half + (q + 1) * 128], GA, GA.t[:, q * 128:(q + 1) * 128], ident_b)
                        cp("act", gat, gat.t[:], pTb, pTb.t[:, half:half + 512].rearrange("p (a b) -> p a b", a=4))
                        for q in range(4):
                            for n in range(2):
                                mm(pY[i][n], pY[i][n].t[:], gat, gat.t[:, q, :], upb, uv[:, q, n * 512:(n + 1) * 512],
                                   start=(eb == 0 and q == 0), stop=(eb == 31 and q == 3))
                for i in range(2):
                    for n in range(2):
                        tt("dve", hf, hf.t[:, n * 512:(n + 1) * 512], pY[i][n], pY[i][n].t[:],
                           xh[i], xh[i].t[:, n * 512:(n + 1) * 512], ALU.add)
                    act(sq, sq.t[:], hf, hf.t[:], AF.Square, wr=[ss], accum_out=ss.t[:])
                    rsqrt_col(rstd, ss, 1.0 / 1024, EPS)
                    yo = yo_p[i]
                    stt("dve", yo, yo.t[:], hf, hf.t[:], rstd.t[:, 0:1], fnl, fnl.t[:], ALU.mult, ALU.mult, reads=[rstd])
                    S.dma("pool", y_d[t0 + i * 128:t0 + (i + 1) * 128, :], yo.t[:], reads=[yo], writes=[Tn(None)])

        for st in range(NST):
            if stage >= 1:
                mixer(st)
                S.barrier()
            if stage >= 2:
                peer(st)
                S.barrier(switch=((st + 1) % sw_every == 0 and st + 1 < NST))
        S.finish()
        print("ninst", S.ninst, {k: v["count"] for k, v in S.eng.items()})
    return nc


def host_inputs(inp):
    f = lambda a: np.ascontiguousarray(np.asarray(a, dtype=np.float32))
    w_in = np.asarray(inp["w_in"])[0]
    cols = []
    for h in range(8):
        for base in (512, 1536, 2560, 3584):
            cols.append(np.arange(base + h * 128, base + (h + 1) * 128))
    cols.append(np.arange(0, 512))
    cols.append(np.arange(4624, 6672))
    cols.append(np.arange(4608, 4624))
    cols = np.concatenate(cols)
    assert cols.shape[0] == IN_COLS
    r = np.arange(128)
    d = {}
    d["w_in_p"] = f(w_in[:, cols])
    d["mnw"] = f(np.asarray(inp["mix_norm_w"])[0].reshape(8, 128).T)
    d["fnw"] = f(np.asarray(inp["ffn_norm_w"])[0].reshape(8, 128).T)
    d["pool_w_p"] = f(np.asarray(inp["pool_w"])[0].transpose(1, 0, 2))
    d["pool_scale_p"] = f(np.asarray(inp["pool_scale"])[0].reshape(4, 128).T)
    d["conv_w_p"] = f(np.asarray(inp["conv_w"])[0].T.reshape(24, 128, 4).transpose(1, 0, 2))
    d["a_log_p"] = f(np.broadcast_to(np.asarray(inp["a_log"])[0][None, :], (128, 8)))
    d["dt_bias_p"] = f(np.broadcast_to(np.asarray(inp["dt_bias"])[0][None, :], (128, 8)))
    d["dn_norm_p"] = f(np.asarray(inp["dn_norm_w"])[0].reshape(128, 1))
    d["w_pool_up"] = f(np.asarray(inp["w_pool_up"])[0])
    d["w_dn_up"] = f(np.asarray(inp["w_dn_up"])[0])
    d["w_mix_out"] = f(np.asarray(inp["w_mix_out"])[0])
    d["peer_w_query"] = f(np.asarray(inp["peer_w_query"])[0])
    k1 = np.asarray(inp["peer_keys_1"])[0]
    k2 = np.asarray(inp["peer_keys_2"])[0]
    kt = np.stack([k1, k2], axis=1).reshape(16, 128, 128)
    d["keys_t"] = f(kt.transpose(2, 0, 1))
    d["peer_down_t"] = f(np.asarray(inp["peer_down"])[0].T)
    d["peer_up"] = f(np.asarray(inp["peer_up"])[0])
    d["final_w_p"] = f(np.broadcast_to(np.asarray(inp["final_norm_w"])[None, :], (128, 1024)))
    d["c_ident"] = f(np.eye(128))
    d["c_tri"] = f(r[:, None] <= r[None, :])
    d["c_negu"] = f(np.where(r[None, :] >= r[:, None], 0.0, NEG))
    d["c_negls"] = f(np.where(r[:, None] > r[None, :], 0.0, NEG))
    d["c_ones"] = f(np.ones((128, 128)))
    pf = np.ones((4, 16), np.float32)
    for g in range(4):
        w = 2 << g
        for t in range(16):
            pf[g, t] = w / min(t + 1, w)
    d["c_poolfix"] = f(np.broadcast_to(pf[None], (128, 4, 16)))
    return d


_NC_CACHE = {}


def kernel(**inputs):
    x = np.asarray(inputs["x"], dtype=np.float32)
    B, S_TOK, _ = x.shape
    if S_TOK not in _NC_CACHE:
        _NC_CACHE[S_TOK] = build(S_TOK)
    nc = _NC_CACHE[S_TOK]
    shared = host_inputs(inputs)
    in_maps = []
    for b in range(B):
        m = dict(shared)
        m["x"] = np.ascontiguousarray(x[b])
        in_maps.append(m)
    res = run_bass_kernel_spmd(nc, in_maps, core_ids=list(range(B)))
    return np.stack([np.asarray(r["y"]) for r in res.results], axis=0).astype(np.float32)
```

```python
from contextlib import ExitStack
import numpy as np
import concourse.bass as bass
import concourse.mybir as mybir
from concourse.bass_utils import run_bass_kernel_spmd

F32 = mybir.dt.float32
BF16 = mybir.dt.bfloat16
AF = mybir.ActivationFunctionType
ALU = mybir.AluOpType

NEG = -30000.0
EPS = 1e-6
IN_COLS = 6672
NE = 16384


class Buf:
    def __init__(self):
        self.last_w = None
        self.readers = []


class Tn:
    def __init__(self, t, b=None, psum=False):
        self.t = t
        self.b = b if b is not None else Buf()
        self.psum = psum


class Sched:
    CE = ("pe", "act", "dve", "pool")

    def __init__(self, nc, ctx, ndma=8, nsets=2):
        self.nc = nc
        self.eng = {}
        self.sets = [{} for _ in range(nsets)]
        for nm, obj in (("pe", nc.tensor), ("act", nc.scalar), ("dve", nc.vector),
                        ("pool", nc.gpsimd), ("sp", nc.sync)):
            for k in range(nsets):
                if nm != "sp":
                    self.sets[k][nm] = ctx.enter_context(nc.semaphore(f"s{k}_" + nm))
            self.eng[nm] = dict(name=nm, obj=obj, sem=self.sets[0].get(nm), count=0, waited={})
        self.epoch = 0
        self.dq = {}
        for q in ("sp", "pool"):
            sems = [ctx.enter_context(nc.semaphore(f"d_{q}{i}")) for i in range(ndma)]
            self.dq[q] = dict(sems=sems, n=0)
        self.ndma = ndma
        self.ninst = 0

    def _wait(self, e, tok):
        sem, val, ep = tok
        if ep is not None and ep < self.epoch:
            return
        if e["name"] == "pe" and sem is e["sem"]:
            return
        key = id(sem)
        w = e["waited"]
        if w.get(key, 0) >= val:
            return
        e["obj"].wait_ge(sem, val)
        w[key] = val
        self.ninst += 1

    def _deps(self, e, reads, writes):
        for b in reads:
            if b.last_w is not None:
                self._wait(e, b.last_w)
        for b in writes:
            if b.last_w is not None:
                self._wait(e, b.last_w)
            for r in b.readers:
                self._wait(e, r)

    def _commit(self, tok, reads, writes):
        for b in reads:
            b.readers = [r for r in b.readers if r[0] is not tok[0]] + [tok]
        for b in writes:
            b.last_w = tok
            b.readers = []

    def op(self, en, fn, reads=(), writes=()):
        e = self.eng[en]
        rb = [x.b for x in reads]
        wb = [x.b for x in writes] + [x.b for x in reads if x.psum]
        self._deps(e, rb, wb)
        inst = fn(e["obj"])
        e["count"] += 1
        inst.then_inc(e["sem"], 1)
        tok = (e["sem"], e["count"], self.epoch)
        self._commit(tok, rb, wb)
        self.ninst += 1
        return tok

    def dma(self, q, out, in_, reads=(), writes=()):
        e = self.eng[q]
        d = self.dq[q]
        n = d["n"]
        sem = d["sems"][n % self.ndma]
        if n >= self.ndma:
            self._wait(e, (sem, 16 * (n // self.ndma), None))
        rb = [x.b for x in reads]
        wb = [x.b for x in writes]
        self._deps(e, rb, wb)
        e["obj"].dma_start(out=out, in_=in_).then_inc(sem, 16)
        d["n"] = n + 1
        tok = (sem, 16 * (n // self.ndma + 1), None)
        self._commit(tok, rb, wb)
        self.ninst += 1
        return tok

    def _sync_all(self):
        for a in self.CE + ("sp",):
            for b in self.CE:
                if a != b and self.eng[b]["count"] > 0:
                    self._wait(self.eng[a], (self.eng[b]["sem"], self.eng[b]["count"], self.epoch))

    def barrier(self, switch=False):
        self._sync_all()
        if not switch:
            return
        self.epoch += 1
        new = self.sets[self.epoch]
        for nm, e in self.eng.items():
            e["sem"] = new.get(nm)
            e["count"] = 0

    def finish(self):
        for q, d in self.dq.items():
            e = self.eng[q]
            for i, sem in enumerate(d["sems"]):
                cnt = (d["n"] - i + self.ndma - 1) // self.ndma
                if cnt > 0:
                    e["obj"].wait_ge(sem, 16 * cnt)


def build(S_TOK, dbg=False, stage=99, sw_every=2):
    NST = S_TOK // 256
    nc = bass.Bass("TRN2", target_bir_lowering=False)

    def din(name, shape):
        return nc.dram_tensor(name, list(shape), F32, kind="ExternalInput").ap()

    x_d = din("x", [S_TOK, 1024])
    win_d = din("w_in_p", [1024, IN_COLS])
    mnw_d = din("mnw", [128, 8])
    fnw_d = din("fnw", [128, 8])
    pw_d = din("pool_w_p", [128, 4, 128])
    psc_d = din("pool_scale_p", [128, 4])
    cw_d = din("conv_w_p", [128, 24, 4])
    alog_d = din("a_log_p", [128, 8])
    dtb_d = din("dt_bias_p", [128, 8])
    dnw_d = din("dn_norm_p", [128, 1])
    wpu_d = din("w_pool_up", [512, 1024])
    wdu_d = din("w_dn_up", [1024, 1024])
    wo_d = din("w_mix_out", [1024, 1024])
    wq_d = din("peer_w_query", [1024, 2048])
    kt_d = din("keys_t", [128, 16, 128])
    dnT_d = din("peer_down_t", [1024, NE])
    up_d = din("peer_up", [NE, 1024])
    fnl_d = din("final_w_p", [128, 1024])
    ident_d = din("c_ident", [128, 128])
    tri_d = din("c_tri", [128, 128])
    negu_d = din("c_negu", [128, 128])
    negls_d = din("c_negls", [128, 128])
    ones_d = din("c_ones", [128, 128])
    pfix_d = din("c_poolfix", [128, 4, 16])
    y_d = nc.dram_tensor("y", [S_TOK, 1024], F32, kind="ExternalOutput").ap()
    if dbg:
        h1_d = nc.dram_tensor("h1dbg", [S_TOK, 1024], F32, kind="ExternalOutput").ap()

    win_s = Tn(nc.dram_tensor("win_s", [128, 8, IN_COLS], BF16).ap())
    wq_s = Tn(nc.dram_tensor("wq_s", [128, 8, 2048], BF16).ap())
    dn_s = Tn(nc.dram_tensor("dn_s", [128, 8, NE], BF16).ap())
    up_s = Tn(nc.dram_tensor("up_s", [128, 128, 1024], BF16).ap())

    with ExitStack() as ctx:
        NEP = (NST + sw_every - 1) // sw_every
        S = Sched(nc, ctx, nsets=NEP + 1)

        uid = [0]

        def un_(name):
            uid[0] += 1
            return f"t{uid[0]}_{name}"

        def sb(cx, name, shape, dt=F32):
            return Tn(cx.enter_context(nc.sbuf_tensor(un_(name), list(shape), dt)))

        def ps(cx, name, shape, dt=F32):
            return Tn(cx.enter_context(nc.psum_tensor(un_(name), list(shape), dt)), psum=True)

        def mm(o, o_ap, l, l_ap, r, r_ap, start=True, stop=True):
            S.op("pe", lambda e: e.matmul(o_ap, lhsT=l_ap, rhs=r_ap, start=start, stop=stop),
                 reads=[l, r], writes=[o])

        def tr(o, o_ap, i, i_ap, idn):
            S.op("pe", lambda e: e.transpose(out=o_ap, in_=i_ap, identity=idn.t[:]),
                 reads=[i, idn], writes=[o])

        def act(o, o_ap, i, i_ap, func, reads=(), wr=(), **kw):
            S.op("act", lambda e: e.activation(out=o_ap, in_=i_ap, func=func, **kw),
                 reads=[i] + list(reads), writes=[o] + list(wr))

        def ts(en, o, o_ap, i, i_ap, s1, s2, op0, op1=None, reads=()):
            if op1 is None:
                S.op(en, lambda e: e.tensor_scalar(out=o_ap, in0=i_ap, scalar1=s1, scalar2=None, op0=op0),
                     reads=[i] + list(reads), writes=[o])
            else:
                S.op(en, lambda e: e.tensor_scalar(out=o_ap, in0=i_ap, scalar1=s1, scalar2=s2, op0=op0, op1=op1),
                     reads=[i] + list(reads), writes=[o])

        def tt(en, o, o_ap, a, a_ap, b, b_ap, op):
            S.op(en, lambda e: e.tensor_tensor(out=o_ap, in0=a_ap, in1=b_ap, op=op),
                 reads=[a, b], writes=[o])

        def stt(en, o, o_ap, a, a_ap, sc, b, b_ap, op0, op1, reads=()):
            S.op(en, lambda e: e.scalar_tensor_tensor(out=o_ap, in0=a_ap, scalar=sc, in1=b_ap, op0=op0, op1=op1),
                 reads=[a, b] + list(reads), writes=[o])

        def cp(en, o, o_ap, i, i_ap):
            if en == "act":
                S.op("act", lambda e: e.copy(out=o_ap, in_=i_ap), reads=[i], writes=[o])
            else:
                S.op(en, lambda e: e.tensor_copy(out=o_ap, in_=i_ap), reads=[i], writes=[o])

        def rsqrt_col(o, i, scale, eps, reads=()):
            ts("dve", o, o.t[:], i, i.t[:], scale, eps, ALU.mult, ALU.add)
            act(o, o.t[:], o, o.t[:], AF.Sqrt)
            S.op("dve", lambda e: e.reciprocal(out=o.t[:], in_=o.t[:]), reads=[o], writes=[o])

        ident_f = sb(ctx, "ident_f", [128, 128])
        ident_b = sb(ctx, "ident_b", [128, 128], BF16)
        tri = sb(ctx, "tri", [128, 128])
        negu = sb(ctx, "negu", [128, 128])
        negls = sb(ctx, "negls", [128, 128])
        ones = sb(ctx, "ones", [128, 128])
        pfix = sb(ctx, "pfix", [128, 4, 16])
        mnw = sb(ctx, "mnw", [128, 8])
        fnw = sb(ctx, "fnw", [128, 8])
        psc = sb(ctx, "psc", [128, 4])
        cw = sb(ctx, "cw", [128, 24, 4])
        nA = sb(ctx, "nA", [128, 8])
        dtb = sb(ctx, "dtb", [128, 8])
        dnw = sb(ctx, "dnw", [128, 1])
        fnl = sb(ctx, "fnl", [128, 1024])
        wpu = sb(ctx, "wpu", [128, 4, 1024], BF16)
        wdu = sb(ctx, "wdu", [128, 8, 1024], BF16)
        wo = sb(ctx, "wo", [128, 8, 1024], BF16)
        pw = sb(ctx, "pw", [128, 4, 128], BF16)
        wsm = sb(ctx, "wsm", [128, 8, 16], BF16)
        KT = sb(ctx, "KT", [128, 16, 128], BF16)
        wpool = [sb(ctx, f"wpool{i}", [128, 4096], BF16) for i in range(5)]
        wp_n = [0]
        Sst = [sb(ctx, f"Sst{h}", [128, 128]) for h in range(8)]
        ccar = [sb(ctx, f"ccar{b}", [128, 3]) for b in range(24)]
        xab = [sb(ctx, f"xab{g}", [128, 272]) for g in range(4)]
        xh = [sb(ctx, f"xh{i}", [128, 1024]) for i in range(2)]
        yo_p = [sb(ctx, f"yo{i}", [128, 1024]) for i in range(2)]

        def wload(src, src_ap, view):
            t = wpool[wp_n[0] % len(wpool)]
            wp_n[0] += 1
            S.dma("sp", view(t.t), src_ap, reads=[src], writes=[t])
            return t

        v8 = lambda t: t[:].rearrange("p (c n) -> p c n", c=8)
        v4 = lambda t: t[:].rearrange("p (c n) -> p c n", c=4)

        dummy = Tn(None)
        for (t, d) in ((ident_f, ident_d), (tri, tri_d), (negu, negu_d), (negls, negls_d), (ones, ones_d),
                       (pfix, pfix_d), (mnw, mnw_d), (fnw, fnw_d), (psc, psc_d), (cw, cw_d), (nA, alog_d),
                       (dtb, dtb_d), (dnw, dnw_d), (fnl, fnl_d)):
            S.dma("sp", t.t[:], d, writes=[t])
        cp("dve", ident_b, ident_b.t[:], ident_f, ident_f.t[:])
        act(nA, nA.t[:], nA, nA.t[:], AF.Exp)
        ts("dve", nA, nA.t[:], nA, nA.t[:], -1.0, None, ALU.mult)
        for h in range(8):
            S.op("dve", lambda e: e.memset(Sst[h].t[:], 0.0), writes=[Sst[h]])
        for b in range(24):
            S.op("pool", lambda e: e.memset(ccar[b].t[:], 0.0), writes=[ccar[b]])
        for g in range(4):
            S.op("pool", lambda e: e.memset(xab[g].t[:], 0.0), writes=[xab[g]])

        with ExitStack() as pc:
            stg = [sb(pc, f"stg{i}", [128, 4096]) for i in range(2)]
            sn = [0]

            def prep(src_ap, shape3, dst, dst_ap, scale_t=None, scale_ap=None):
                a, b = shape3
                st_ = stg[sn[0] % 2]
                sn[0] += 1
                sv = st_.t[:, 0:a * b].rearrange("p (a b) -> p a b", a=a)
                S.dma("sp", sv, src_ap, writes=[st_])
                ob = wpool[wp_n[0] % len(wpool)]
                wp_n[0] += 1
                ov = ob.t[:, 0:a * b].rearrange("p (a b) -> p a b", a=a)
                en = "dve" if sn[0] % 2 == 0 else "pool"
                if scale_t is None:
                    cp("act" if sn[0] % 2 == 0 else "dve", ob, ov, st_, sv)
                else:
                    tt(en, ob, ov, st_, sv, scale_t, scale_ap, ALU.mult)
                if dst is None:
                    return ob, ov
                S.dma("pool", dst_ap, ov, reads=[ob], writes=[dst])
                return ob, ov

            for (wt, d, a, b, rs) in ((wpu, wpu_d, 4, 1024, "(g p) n -> p g n"),
                                      (wo, wo_d, 8, 1024, "(c p) n -> p c n")):
                for half in range(a // 4):
                    ob, ov = prep(d.rearrange(rs, p=128)[:, half * 4:(half + 1) * 4, :], (4, 1024), None, None)
                    cp("dve", wt, wt.t[:, half * 4:(half + 1) * 4, :], ob, ov)
            for half in range(2):
                ob, ov = prep(wdu_d.rearrange("(h p) n -> p h n", p=128)[:, half * 4:(half + 1) * 4, :], (4, 1024),
                              None, None, dnw, dnw.t[:, 0:1].unsqueeze(2).to_broadcast([128, 4, 1024]))
                cp("dve", wdu, wdu.t[:, half * 4:(half + 1) * 4, :], ob, ov)
            ob, ov = prep(pw_d, (4, 128), None, None)
            cp("dve", pw, pw.t[:], ob, ov)
            ob, ov = prep(kt_d, (16, 128), None, None)
            cp("dve", KT, KT.t[:], ob, ov)
            winv = win_d.rearrange("(c p) n -> p c n", p=128)
            for blk in range(14):
                c0 = blk * 512
                n = min(512, IN_COLS - c0)
                ob, ov = prep(winv[:, :, c0:c0 + n], (8, n), win_s, win_s.t[:, :, c0:c0 + n],
                              mnw, mnw.t[:].unsqueeze(2).to_broadcast([128, 8, n]))
                if blk == 13:
                    cp("dve", wsm, wsm.t[:], ob, ov)
            wqv = wq_d.rearrange("(c p) n -> p c n", p=128)
            for blk in range(4):
                prep(wqv[:, :, blk * 512:(blk + 1) * 512], (8, 512), wq_s, wq_s.t[:, :, blk * 512:(blk + 1) * 512],
                     fnw, fnw.t[:].unsqueeze(2).to_broadcast([128, 8, 512]))
            dnv = dnT_d.rearrange("(c p) e -> p c e", p=128)
            upv = up_d.rearrange("(ch p) d -> p ch d", p=128)
            for blk in range(32 if stage >= 2 else 0):
                prep(dnv[:, :, blk * 512:(blk + 1) * 512], (8, 512), dn_s, dn_s.t[:, :, blk * 512:(blk + 1) * 512],
                     fnw, fnw.t[:].unsqueeze(2).to_broadcast([128, 8, 512]))
                prep(upv[:, blk * 4:(blk + 1) * 4, :], (4, 1024), up_s, up_s.t[:, blk * 4:(blk + 1) * 4, :])
        S.barrier(switch=True)

        def mixer(st):
            t0 = st * 256
            with ExitStack() as mx:
                pTb = ps(mx, "pTb", [128, 1024], BF16)
                big = [ps(mx, f"big{k}", [128, 512]) for k in range(4)]
                bn = [0]
                qbank = [mx.enter_context(nc.psum_tensor(un_(f"qb{k}"), [128, 512], F32)) for k in range(3)]
                qsl = [Tn(qbank[k], psum=True) for k in range(3)]
                qn = [0]

                def nbig():
                    b = big[bn[0] % 4]
                    bn[0] += 1
                    return b

                def nq():
                    k = qn[0] % 12
                    qn[0] += 1
                    s = qsl[k % 3]
                    qq = k // 3
                    return s, s.t[:, qq * 128:(qq + 1) * 128]

                sq = sb(mx, "sq", [128, 1024])
                xsb = sb(mx, "xsb", [128, 1024], BF16)
                xT = sb(mx, "xT", [128, 8, 256], BF16)
                ss = sb(mx, "ss", [128, 1])
                rstd = sb(mx, "rstd", [128, 1])
                raw = [sb(mx, f"raw{k}", [128, 259]) for k in range(3)]
                cacc = sb(mx, "cacc", [128, 256])
                qkvT = [sb(mx, f"qkvT{k}", [128, 256]) for k in range(3)]
                szT = sb(mx, "szT", [128, 256])
                sqn = sb(mx, "sqn", [128, 256])
                rn = sb(mx, "rn", [128, 256])
                lg = [sb(mx, f"lg{i}", [128, 16]) for i in range(2)]
                beta = [sb(mx, f"beta{i}", [128, 8]) for i in range(2)]
                nbeta = [sb(mx, f"nbeta{i}", [128, 8]) for i in range(2)]
                gg = [sb(mx, f"gg{i}", [128, 8]) for i in range(2)]
                gc = [sb(mx, f"gc{i}", [128, 8]) for i in range(2)]
                egl = [sb(mx, f"egl{i}", [128, 8]) for i in range(2)]
                kds = [sb(mx, f"kds{i}", [128, 8]) for i in range(2)]
                bgs = [sb(mx, f"bgs{i}", [128, 8]) for i in range(2)]
                kbg = sb(mx, "kbg", [128, 128])
                kdec = sb(mx, "kdec", [128, 128])
                vb = sb(mx, "vb", [128, 128])
                trig = sb(mx, "trig", [128, 128])
                Am = sb(mx, "Am", [128, 128])
                EU = sb(mx, "EU", [128, 128])
                DLs = sb(mx, "DLs", [128, 128])
                egrow = sb(mx, "egrow", [128, 128])
                Mb = [sb(mx, f"Mb{k}", [128, 128]) for k in range(2)]
                MTb = [sb(mx, f"MTb{k}", [128, 128]) for k in range(2)]
                PTb = [sb(mx, f"PTb{k}", [128, 128]) for k in range(2)]
                attnT = sb(mx, "attnT", [128, 128])
                qgT = sb(mx, "qgT", [128, 128])
                nwT = sb(mx, "nwT", [128, 128])
                vn = sb(mx, "vn", [128, 128])
                oss = sb(mx, "oss", [128, 1])
                orst = sb(mx, "orst", [128, 1])
                osq = sb(mx, "osq", [128, 128])
                onb = sb(mx, "onb", [128, 128], BF16)
                onT = sb(mx, "onT", [128, 8, 256], BF16)
                pa = [sb(mx, f"pa{k}", [128, 272]) for k in range(2)]
                pooledb = sb(mx, "pooledb", [128, 256], BF16)
                paT = sb(mx, "paT", [128, 4, 256], BF16)
                sga = sb(mx, "sga", [128, 256])
                m1 = sb(mx, "m1", [128, 256])
                sgb = sb(mx, "sgb", [128, 256])
                m2 = sb(mx, "m2", [128, 256])
                mT = sb(mx, "mT", [128, 8, 256], BF16)

                for i in range(2):
                    S.dma("sp", xh[i].t[:], x_d[t0 + i * 128:t0 + (i + 1) * 128, :], writes=[xh[i]])
                    act(sq, sq.t[:], xh[i], xh[i].t[:], AF.Square, wr=[ss], accum_out=ss.t[:])
                    rsqrt_col(rstd, ss, 1.0 / 1024, EPS)
                    ts("dve", xsb, xsb.t[:], xh[i], xh[i].t[:], rstd.t[:, 0:1], None, ALU.mult, reads=[rstd])
                    for c in range(8):
                        tr(pTb, pTb.t[:, c * 128:(c + 1) * 128], xsb, xsb.t[:, c * 128:(c + 1) * 128], ident_b)
                    cp("act", xT, xT.t[:, :, i * 128:(i + 1) * 128], pTb,
                       pTb.t[:].rearrange("p (c n) -> p c n", c=8))

                for i in range(2):
                    s_, s_ap = nq()
                    for c in range(8):
                        mm(s_, s_ap[:, 0:16], xT, xT.t[:, c, i * 128:(i + 1) * 128], wsm, wsm.t[:, c, :],
                           start=(c == 0), stop=(c == 7))
                    cp("dve", lg[i], lg[i].t[:], s_, s_ap[:, 0:16])
                    act(beta[i], beta[i].t[:], lg[i], lg[i].t[:, 0:8], AF.Sigmoid)
                    ts("dve", nbeta[i], nbeta[i].t[:], beta[i], beta[i].t[:], -1.0, None, ALU.mult)
                    tt("dve", gg[i], gg[i].t[:], lg[i], lg[i].t[:, 8:16], dtb, dtb.t[:], ALU.add)
                    act(gg[i], gg[i].t[:], gg[i], gg[i].t[:], AF.Exp)
                    act(gg[i], gg[i].t[:], gg[i], gg[i].t[:], AF.Ln, bias=1.0)
                    tt("dve", gg[i], gg[i].t[:], gg[i], gg[i].t[:], nA, nA.t[:], ALU.mult)
                    s_, s_ap = nq()
                    mm(s_, s_ap[:, 0:8], tri, tri.t[:], gg[i], gg[i].t[:])
                    cp("dve", gc[i], gc[i].t[:], s_, s_ap[:, 0:8])
                    s_, s_ap = nq()
                    mm(s_, s_ap[:, 0:8], ones, ones.t[:], gg[i], gg[i].t[:])
                    tt("dve", kds[i], kds[i].t[:], s_, s_ap[:, 0:8], gc[i], gc[i].t[:], ALU.subtract)
                    act(kds[i], kds[i].t[:], kds[i], kds[i].t[:], AF.Exp)
                    act(egl[i], egl[i].t[:], s_, s_ap[:, 0:8], AF.Exp)
                    act(bgs[i], bgs[i].t[:], gc[i], gc[i].t[:], AF.Exp)
                    tt("dve", bgs[i], bgs[i].t[:], bgs[i], bgs[i].t[:], beta[i], beta[i].t[:], ALU.mult)

                for h in range(8):
                    wb = wload(win_s, win_s.t[:, :, h * 512:(h + 1) * 512], v8)
                    wv = v8(wb.t)
                    for k in range(4):
                        pb = nbig()
                        for c in range(8):
                            mm(pb, pb.t[:, 0:256], wb, wv[:, c, k * 128:(k + 1) * 128], xT, xT.t[:, c, :],
                               start=(c == 0), stop=(c == 7))
                        if k == 3:
                            act(szT, szT.t[:], pb, pb.t[:, 0:256], AF.Silu)
                            continue
                        blk = k * 8 + h
                        r = raw[k]
                        cp("pool", r, r.t[:, 0:3], ccar[blk], ccar[blk].t[:])
                        cp("act", r, r.t[:, 3:259], pb, pb.t[:, 0:256])
                        cp("pool", ccar[blk], ccar[blk].t[:], r, r.t[:, 256:259])
                        ts("dve", cacc, cacc.t[:], r, r.t[:, 0:256], cw.t[:, blk, 0:1], None, ALU.mult, reads=[cw])
                        for j in range(1, 4):
                            stt("dve", cacc, cacc.t[:], r, r.t[:, j:j + 256], cw.t[:, blk, j:j + 1], cacc, cacc.t[:],
                                ALU.mult, ALU.add, reads=[cw])
                        act(qkvT[k], qkvT[k].t[:], cacc, cacc.t[:], AF.Silu)
                        if k < 2:
                            tt("dve", sqn, sqn.t[:], qkvT[k], qkvT[k].t[:], qkvT[k], qkvT[k].t[:], ALU.mult)
                            pn = nbig()
                            mm(pn, pn.t[:, 0:256], ones, ones.t[:], sqn, sqn.t[:])
                            ts("dve", rn, rn.t[:], pn, pn.t[:, 0:256], EPS, None, ALU.add)
                            act(rn, rn.t[:], rn, rn.t[:], AF.Sqrt)
                            S.op("dve", lambda e: e.reciprocal(out=rn.t[:], in_=rn.t[:]), reads=[rn], writes=[rn])
                            sc = (128.0 ** -0.5) if k == 0 else 1.0
                            stt("dve", qkvT[k], qkvT[k].t[:], qkvT[k], qkvT[k].t[:], sc, rn, rn.t[:],
                                ALU.mult, ALU.mult)
                    qT_, kT_, vT_ = qkvT
                    for i in range(2):
                        sl = slice(i * 128, (i + 1) * 128)
                        hh = slice(h, h + 1)
                        k_s, k_ap = nq()
                        tr(k_s, k_ap, kT_, kT_.t[:, sl], ident_f)
                        v_s, v_ap = nq()
                        tr(v_s, v_ap, vT_, vT_.t[:, sl], ident_f)
                        g_s, g_ap = nq()
                        mm(g_s, g_ap, kT_, kT_.t[:, sl], kT_, kT_.t[:, sl])
                        a_s, a_ap = nq()
                        mm(a_s, a_ap, kT_, kT_.t[:, sl], qT_, qT_.t[:, sl])
                        ts("dve", kbg, kbg.t[:], k_s, k_ap, bgs[i].t[:, hh], None, ALU.mult, reads=[bgs[i]])
                        act(kdec, kdec.t[:], k_s, k_ap, AF.Copy, reads=[kds[i]], scale=kds[i].t[:, hh])
                        act(vb, vb.t[:], v_s, v_ap, AF.Copy, reads=[beta[i]], scale=beta[i].t[:, hh])
                        ts("pool", trig, trig.t[:], tri, tri.t[:], gg[i].t[:, hh], None, ALU.mult, reads=[gg[i]])
                        r_s, r_ap = nq()
                        mm(r_s, r_ap, ones, ones.t[:], trig, trig.t[:])
                        ts("dve", Am, Am.t[:], r_s, r_ap, gc[i].t[:, hh], None, ALU.subtract, reads=[gc[i]])
                        act(egrow, egrow.t[:], r_s, r_ap, AF.Exp)
                        tt("pool", EU, EU.t[:], Am, Am.t[:], negu, negu.t[:], ALU.add)
                        act(EU, EU.t[:], EU, EU.t[:], AF.Exp)
                        tt("pool", DLs, DLs.t[:], negls, negls.t[:], Am, Am.t[:], ALU.subtract)
                        act(DLs, DLs.t[:], DLs, DLs.t[:], AF.Exp)
                        M, MT, PT = Mb[0], MTb[0], PTb[0]
                        stt("dve", M, M.t[:], g_s, g_ap, nbeta[i].t[:, hh], DLs, DLs.t[:], ALU.mult, ALU.mult,
                            reads=[nbeta[i]])
                        tt("dve", attnT, attnT.t[:], a_s, a_ap, EU, EU.t[:], ALU.mult)
                        tt("pool", qgT, qgT.t[:], qT_, qT_.t[:, sl], egrow, egrow.t[:], ALU.mult)
                        t_s, t_ap = nq()
                        tr(t_s, t_ap, M, M.t[:], ident_f)
                        cp("act", MT, MT.t[:], t_s, t_ap)
                        tt("dve", PT, PT.t[:], t_s, t_ap, ident_f, ident_f.t[:], ALU.add)
                        for kk in range(6):
                            Mn, MTn, PTn = Mb[(kk + 1) % 2], MTb[(kk + 1) % 2], PTb[(kk + 1) % 2]
                            s1, s1ap = nq()
                            mm(s1, s1ap, MT, MT.t[:], M, M.t[:])
                            cp("act", Mn, Mn.t[:], s1, s1ap)
                            if kk < 5:
                                s2, s2ap = nq()
                                mm(s2, s2ap, M, M.t[:], MT, MT.t[:])
                                cp("dve", MTn, MTn.t[:], s2, s2ap)
                            s3, s3ap = nq()
                            mm(s3, s3ap, Mn, Mn.t[:], PT, PT.t[:])
                            tt("dve", PTn, PTn.t[:], s3, s3ap, PT, PT.t[:], ALU.add)
                            M, MT, PT = Mn, MTn, PTn
                        w_s, w_ap = nq()
                        mm(w_s, w_ap, kbg, kbg.t[:], PT, PT.t[:])
                        S.op("act", lambda e: e.mul(out=nwT.t[:], in_=w_ap, mul=-1.0), reads=[w_s], writes=[nwT])
                        n_s, n_ap = nq()
                        mm(n_s, n_ap, PT, PT.t[:], vb, vb.t[:], start=True, stop=False)
                        mm(n_s, n_ap, nwT, nwT.t[:], Sst[h], Sst[h].t[:], start=False, stop=True)
                        cp("dve", vn, vn.t[:], n_s, n_ap)
                        o_s, o_ap = nq()
                        mm(o_s, o_ap, qgT, qgT.t[:], Sst[h], Sst[h].t[:], start=True, stop=False)
                        mm(o_s, o_ap, attnT, attnT.t[:], vn, vn.t[:], start=False, stop=True)
                        u_s, u_ap = nq()
                        mm(u_s, u_ap, kdec, kdec.t[:], vn, vn.t[:])
                        stt("dve", Sst[h], Sst[h].t[:], Sst[h], Sst[h].t[:], egl[i].t[:, hh], u_s, u_ap,
                            ALU.mult, ALU.add, reads=[egl[i]])
                        act(osq, osq.t[:], o_s, o_ap, AF.Square, wr=[oss], accum_out=oss.t[:])
                        rsqrt_col(orst, oss, 1.0 / 128, EPS)
                        act(onb, onb.t[:], o_s, o_ap, AF.Copy, reads=[orst], scale=orst.t[:, 0:1])
                        pslot = pTb.t[:, (h % 8) * 128:(h % 8 + 1) * 128]
                        tr(pTb, pslot, onb, onb.t[:], ident_b)
                        tt("dve", onT, onT.t[:, h, sl], pTb, pslot, szT, szT.t[:, sl], ALU.mult)

                wb = wload(win_s, win_s.t[:, :, 4096:4608], v8)
                wv = v8(wb.t)
                for g in range(4):
                    win_ = 2 << g
                    pb = nbig()
                    for c in range(8):
                        mm(pb, pb.t[:, 0:256], wb, wv[:, c, g * 128:(g + 1) * 128], xT, xT.t[:, c, :],
                           start=(c == 0), stop=(c == 7))
                    xg = xab[g]
                    cp("pool", xg, xg.t[:, 0:16], xg, xg.t[:, 256:272])
                    cp("act", xg, xg.t[:, 16:272], pb, pb.t[:, 0:256])
                    src = xg
                    sh = 1
                    for stp in range(g + 1):
                        dst = pa[stp % 2]
                        lo = 2 * sh - 1
                        tt("dve", dst, dst.t[:, lo:272], src, src.t[:, lo:272], src, src.t[:, lo - sh:272 - sh], ALU.add)
                        src = dst
                        sh *= 2
                    if st == 0:
                        tt("dve", src, src.t[:, 16:32], src, src.t[:, 16:32], pfix, pfix.t[:, g, :], ALU.mult)
                    stt("dve", pooledb, pooledb.t[:], src, src.t[:, 16:272], 1.0 / win_, xg, xg.t[:, 16:272],
                        ALU.mult, ALU.subtract)
                    pb2 = nbig()
                    mm(pb2, pb2.t[:, 0:256], pw, pw.t[:, g, :], pooledb, pooledb.t[:])
                    act(paT, paT.t[:, g, :], pb2, pb2.t[:, 0:256], AF.Copy, reads=[psc], scale=psc.t[:, g:g + 1])

                wga = [wload(win_s, win_s.t[:, :, 4608 + k * 512:4608 + (k + 1) * 512], v8) for k in range(2)]
                wgb = [wload(win_s, win_s.t[:, :, 5632 + k * 512:5632 + (k + 1) * 512], v8) for k in range(2)]
                for j in range(8):
                    js = slice(j * 128, (j + 1) * 128)
                    jw = slice((j % 4) * 128, (j % 4 + 1) * 128)
                    pA, pB, pC, pD = big[0], big[1], big[2], big[3]
                    for g in range(4):
                        mm(pA, pA.t[:, 0:256], wpu, wpu.t[:, g, js], paT, paT.t[:, g, :], start=(g == 0), stop=(g == 3))
                    wa = wga[j // 4]
                    for c in range(8):
                        mm(pB, pB.t[:, 0:256], wa, v8(wa.t)[:, c, jw], xT, xT.t[:, c, :], start=(c == 0), stop=(c == 7))
                    for hd in range(8):
                        mm(pC, pC.t[:, 0:256], wdu, wdu.t[:, hd, js], onT, onT.t[:, hd, :], start=(hd == 0), stop=(hd == 7))
                    wg = wgb[j // 4]
                    for c in range(8):
                        mm(pD, pD.t[:, 0:256], wg, v8(wg.t)[:, c, jw], xT, xT.t[:, c, :], start=(c == 0), stop=(c == 7))
                    act(sga, sga.t[:], pB, pB.t[:, 0:256], AF.Sigmoid)
                    tt("dve", m1, m1.t[:], pA, pA.t[:, 0:256], sga, sga.t[:], ALU.mult)
                    act(sgb, sgb.t[:], pD, pD.t[:, 0:256], AF.Sigmoid)
                    tt("dve", m2, m2.t[:], pC, pC.t[:, 0:256], sgb, sgb.t[:], ALU.mult)
                    tt("pool", mT, mT.t[:, j, :], m1, m1.t[:], m2, m2.t[:], ALU.add)

                for i in range(2):
                    for n in range(2):
                        pb = nbig()
                        for k in range(8):
                            mm(pb, pb.t[:], mT, mT.t[:, k, i * 128:(i + 1) * 128], wo, wo.t[:, k, n * 512:(n + 1) * 512],
                               start=(k == 0), stop=(k == 7))
                        tt("dve", xh[i], xh[i].t[:, n * 512:(n + 1) * 512], pb, pb.t[:],
                           xh[i], xh[i].t[:, n * 512:(n + 1) * 512], ALU.add)
                    if dbg:
                        S.dma("pool", h1_d[t0 + i * 128:t0 + (i + 1) * 128, :], xh[i].t[:], reads=[xh[i]], writes=[Tn(None)])

        def peer(st):
            t0 = st * 256
            with ExitStack() as px:
                pY = [[ps(px, f"pY{i}{n}", [128, 512]) for n in range(2)] for i in range(2)]
                pU = [ps(px, f"pU{k}", [128, 512]) for k in range(2)]
                pTb = ps(px, "pTbp", [128, 1024], BF16)
                pM = ps(px, "pM", [128, 512])
                sq = sb(px, "psq", [128, 1024])
                hsb = sb(px, "hsb", [128, 1024], BF16)
                xn2T = sb(px, "xn2T", [128, 8, 256], BF16)
                ss = sb(px, "pss", [128, 1])
                rstd = sb(px, "prstd", [128, 1])
                qT = sb(px, "pqT", [128, 16, 256], BF16)
                sc = [sb(px, f"sc{i}", [128, 2048]) for i in range(2)]
                v16 = [sb(px, f"v16{k}", [128, 16]) for k in range(2)]
                tmp128 = sb(px, "tmp128", [128, 128])
                cand = sb(px, "cand", [128, 256])
                cand2 = sb(px, "cand2", [128, 256])
                c16 = sb(px, "c16", [128, 16])
                ex16 = sb(px, "ex16", [128, 16])
                negm = sb(px, "negm", [128, 1])
                zz = sb(px, "zz", [128, 1])
                taum = [sb(px, f"taum{i}", [128, 8]) for i in range(2)]
                ttau = [sb(px, f"ttau{i}", [128, 8]) for i in range(2)]
                cand3 = sb(px, "cand3", [128, 256])
                c24 = sb(px, "c24", [128, 8])
                tsum = sb(px, "tsum", [128, 1])
                E1n = [sb(px, f"E1n{i}", [128, 8, 128]) for i in range(2)]
                E2 = [sb(px, f"E2{i}", [128, 8, 128]) for i in range(2)]
                gmb = [sb(px, f"gmb{k}", [128, 512], BF16) for k in range(3)]
                ncc = [sb(px, f"ncc{i}", [128, 8]) for i in range(2)]
                Ag = [sb(px, f"Ag{k}", [128, 512]) for k in range(2)]
                Tt = [sb(px, f"Tt{k}", [128, 512]) for k in range(4)]
                GA = sb(px, "GA", [128, 512], BF16)
                GAT = [sb(px, f"GAT{k}", [128, 4, 128], BF16) for k in range(2)]
                hf = sb(px, "hf", [128, 1024])

                for i in range(2):
                    act(sq, sq.t[:], xh[i], xh[i].t[:], AF.Square, wr=[ss], accum_out=ss.t[:])
                    rsqrt_col(rstd, ss, 1.0 / 1024, EPS)
                    ts("dve", hsb, hsb.t[:], xh[i], xh[i].t[:], rstd.t[:, 0:1], None, ALU.mult, reads=[rstd])
                    for c in range(8):
                        tr(pTb, pTb.t[:, c * 128:(c + 1) * 128], hsb, hsb.t[:, c * 128:(c + 1) * 128], ident_b)
                    cp("act", xn2T, xn2T.t[:, :, i * 128:(i + 1) * 128], pTb,
                       pTb.t[:].rearrange("p (c n) -> p c n", c=8))
                for blk in range(4):
                    wb = wload(wq_s, wq_s.t[:, :, blk * 512:(blk + 1) * 512], v8)
                    wv = v8(wb.t)
                    for jj in range(4):
                        jb = blk * 4 + jj
                        for c in range(8):
                            mm(pM, pM.t[:, 0:256], wb, wv[:, c, jj * 128:(jj + 1) * 128], xn2T, xn2T.t[:, c, :],
                               start=(c == 0), stop=(c == 7))
                        cp("act" if jb % 2 == 0 else "dve", qT, qT.t[:, jb, :], pM, pM.t[:, 0:256])
                for i in range(2):
                    for b4 in range(4):
                        for jj in range(4):
                            jb = b4 * 4 + jj
                            mm(pM, pM.t[:, jj * 128:(jj + 1) * 128], qT, qT.t[:, jb, i * 128:(i + 1) * 128],
                               KT, KT.t[:, jb, :])
                        cp("act", sc[i], sc[i].t[:, b4 * 512:(b4 + 1) * 512], pM, pM.t[:])
                for i in range(2):
                    for h in range(8):
                        for half in range(2):
                            sv = sc[i].t[:, (h * 2 + half) * 128:(h * 2 + half + 1) * 128]
                            vv = v16[half]
                            S.op("dve", lambda e: e.max(out=vv.t[:, 0:8], in_=sv), reads=[sc[i]], writes=[vv])
                            S.op("dve", lambda e: e.match_replace(out=tmp128.t[:], in_to_replace=vv.t[:, 0:8],
                                                                  in_values=sv, imm_value=-1e30),
                                 reads=[sc[i], vv], writes=[tmp128])
                            S.op("dve", lambda e: e.max(out=vv.t[:, 8:16], in_=tmp128.t[:]), reads=[tmp128], writes=[vv])
                        tt("dve", cand, cand.t[:].rearrange("p (a b) -> p a b", a=16),
                           v16[0], v16[0].t[:].unsqueeze(2).to_broadcast([128, 16, 16]),
                           v16[1], v16[1].t[:].unsqueeze(1).to_broadcast([128, 16, 16]), ALU.add)
                        S.op("dve", lambda e: e.max(out=c16.t[:, 0:8], in_=cand.t[:]), reads=[cand], writes=[c16])
                        S.op("dve", lambda e: e.match_replace(out=cand2.t[:], in_to_replace=c16.t[:, 0:8],
                                                              in_values=cand.t[:], imm_value=-1e30),
                             reads=[cand, c16], writes=[cand2])
                        S.op("dve", lambda e: e.max(out=c16.t[:, 8:16], in_=cand2.t[:]), reads=[cand2], writes=[c16])
                        ts("dve", negm, negm.t[:], c16, c16.t[:, 0:1], -1.0, None, ALU.mult)
                        act(ex16, ex16.t[:], c16, c16.t[:], AF.Exp, reads=[negm], wr=[zz], bias=negm.t[:, 0:1],
                            accum_out=zz.t[:])
                        act(zz, zz.t[:], zz, zz.t[:], AF.Ln)
                        tt("dve", ncc[i], ncc[i].t[:, h:h + 1], negm, negm.t[:], zz, zz.t[:], ALU.subtract)
                        S.op("dve", lambda e: e.match_replace(out=cand3.t[:], in_to_replace=c16.t[:, 8:16],
                                                              in_values=cand2.t[:], imm_value=-1e30),
                             reads=[cand2, c16], writes=[cand3])
                        S.op("dve", lambda e: e.max(out=c24.t[:], in_=cand3.t[:]), reads=[cand3], writes=[c24])
                        tt("dve", tsum, tsum.t[:], c16, c16.t[:, 15:16], c24, c24.t[:, 0:1], ALU.add)
                        ts("dve", taum[i], taum[i].t[:, h:h + 1], tsum, tsum.t[:], 0.5, None, ALU.mult)
                    tt("dve", ttau[i], ttau[i].t[:], taum[i], taum[i].t[:], ncc[i], ncc[i].t[:], ALU.add)
                    act(ttau[i], ttau[i].t[:], ttau[i], ttau[i].t[:], AF.Exp)
                    for h in range(8):
                        act(E1n[i], E1n[i].t[:, h, :], sc[i], sc[i].t[:, (h * 2) * 128:(h * 2 + 1) * 128], AF.Exp,
                            reads=[ncc[i]], bias=ncc[i].t[:, h:h + 1])
                    act(E2[i], E2[i].t[:], sc[i],
                        sc[i].t[:].rearrange("p (h t k) -> p h t k", h=8, t=2)[:, :, 1, :], AF.Exp)
                pu = pU[0]
                pAccs = [pM, pU[1]]
                wts = {}
                cnt = dict(tn=0, gn=0)

                def stA(n):
                    eb, i = divmod(n, 2)
                    if i == 0:
                        wts[eb] = (wload(dn_s, dn_s.t[:, :, eb * 512:(eb + 1) * 512], v8),
                                   wload(up_s, up_s.t[:, eb * 4:(eb + 1) * 4, :], v4))
                    dnb = wts[eb][0]
                    dv = v8(dnb.t)
                    ag = Ag[n % 2]
                    for c in range(8):
                        mm(pu, pu.t[:], xn2T, xn2T.t[:, c, i * 128:(i + 1) * 128], dnb, dv[:, c, :],
                           start=(c == 0), stop=(c == 7))
                    act(ag, ag.t[:], pu, pu.t[:], AF.Gelu)

                def stB(n):
                    eb, i = divmod(n, 2)
                    pAcc = pAccs[n % 2]
                    for h in range(8):
                        Tb = Tt[cnt["tn"] % 4]
                        cnt["tn"] += 1
                        if h < 5:
                            for q in range(4):
                                act(Tb, Tb.t[:, q * 128:(q + 1) * 128], E2[i], E2[i].t[:, h, :], AF.Copy,
                                    reads=[E1n[i]], scale=E1n[i].t[:, h, eb * 4 + q:eb * 4 + q + 1])
                        else:
                            tt("pool" if h == 5 else "dve", Tb, Tb.t[:].rearrange("p (a b) -> p a b", a=4),
                               E1n[i], E1n[i].t[:, h, eb * 4:eb * 4 + 4].unsqueeze(2).to_broadcast([128, 4, 128]),
                               E2[i], E2[i].t[:, h, :].unsqueeze(1).to_broadcast([128, 4, 128]), ALU.mult)
                        gm = gmb[cnt["gn"] % 3]
                        cnt["gn"] += 1
                        stt("dve", gm, gm.t[:], Tb, Tb.t[:], ttau[i].t[:, h:h + 1], Tb, Tb.t[:],
                            ALU.is_ge, ALU.mult, reads=[ttau[i]])
                        mm(pAcc, pAcc.t[:], ident_b, ident_b.t[:], gm, gm.t[:], start=(h == 0), stop=(h == 7))

                def stC(n):
                    eb, i = divmod(n, 2)
                    pAcc = pAccs[n % 2]
                    ag = Ag[n % 2]
                    gat = GAT[n % 2]
                    upb = wts[eb][1]
                    uv = v4(upb.t)
                    tt("dve", GA, GA.t[:], pAcc, pAcc.t[:], ag, ag.t[:], ALU.mult)
                    half = (n % 2) * 512
                    for q in range(4):
                        tr(pTb, pTb.t[:, half + q * 128:half + (q + 1) * 128], GA, GA.t[:, q * 128:(q + 1) * 128], ident_b)
                    cp("act", gat, gat.t[:], pTb, pTb.t[:, half:half + 512].rearrange("p (a b) -> p a b", a=4))
                    for q in range(4):
                        for nn in range(2):
                            mm(pY[i][nn], pY[i][nn].t[:], gat, gat.t[:, q, :], upb, uv[:, q, nn * 512:(nn + 1) * 512],
                               start=(eb == 0 and q == 0), stop=(eb == 31 and q == 3))

                NU = 64
                stA(0)
                stB(0)
                for n in range(1, NU):
                    stA(n)
                    stB(n)
                    stC(n - 1)
                stC(NU - 1)
                for i in range(2):
                    for n in range(2):
                        tt("dve", hf, hf.t[:, n * 512:(n + 1) * 512], pY[i][n], pY[i][n].t[:],
                           xh[i], xh[i].t[:, n * 512:(n + 1) * 512], ALU.add)
                    act(sq, sq.t[:], hf, hf.t[:], AF.Square, wr=[ss], accum_out=ss.t[:])
                    rsqrt_col(rstd, ss, 1.0 / 1024, EPS)
                    yo = yo_p[i]
                    stt("dve", yo, yo.t[:], hf, hf.t[:], rstd.t[:, 0:1], fnl, fnl.t[:], ALU.mult, ALU.mult, reads=[rstd])
                    S.dma("pool", y_d[t0 + i * 128:t0 + (i + 1) * 128, :], yo.t[:], reads=[yo], writes=[Tn(None)])

        for st in range(NST):
            if stage >= 1:
                mixer(st)
                S.barrier()
            if stage >= 2:
                peer(st)
                S.barrier(switch=((st + 1) % sw_every == 0 and st + 1 < NST))
        S.finish()
        print("ninst", S.ninst, {k: v["count"] for k, v in S.eng.items()})
    return nc


def host_inputs(inp):
    f = lambda a: np.ascontiguousarray(np.asarray(a, dtype=np.float32))
    w_in = np.asarray(inp["w_in"])[0]
    cols = []
    for h in range(8):
        for base in (512, 1536, 2560, 3584):
            cols.append(np.arange(base + h * 128, base + (h + 1) * 128))
    cols.append(np.arange(0, 512))
    cols.append(np.arange(4624, 6672))
    cols.append(np.arange(4608, 4624))
    cols = np.concatenate(cols)
    assert cols.shape[0] == IN_COLS
    r = np.arange(128)
    d = {}
    d["w_in_p"] = f(w_in[:, cols])
    d["mnw"] = f(np.asarray(inp["mix_norm_w"])[0].reshape(8, 128).T)
    d["fnw"] = f(np.asarray(inp["ffn_norm_w"])[0].reshape(8, 128).T)
    d["pool_w_p"] = f(np.asarray(inp["pool_w"])[0].transpose(1, 0, 2))
    d["pool_scale_p"] = f(np.asarray(inp["pool_scale"])[0].reshape(4, 128).T)
    d["conv_w_p"] = f(np.asarray(inp["conv_w"])[0].T.reshape(24, 128, 4).transpose(1, 0, 2))
    d["a_log_p"] = f(np.broadcast_to(np.asarray(inp["a_log"])[0][None, :], (128, 8)))
    d["dt_bias_p"] = f(np.broadcast_to(np.asarray(inp["dt_bias"])[0][None, :], (128, 8)))
    d["dn_norm_p"] = f(np.asarray(inp["dn_norm_w"])[0].reshape(128, 1))
    d["w_pool_up"] = f(np.asarray(inp["w_pool_up"])[0])
    d["w_dn_up"] = f(np.asarray(inp["w_dn_up"])[0])
    d["w_mix_out"] = f(np.asarray(inp["w_mix_out"])[0])
    d["peer_w_query"] = f(np.asarray(inp["peer_w_query"])[0])
    k1 = np.asarray(inp["peer_keys_1"])[0]
    k2 = np.asarray(inp["peer_keys_2"])[0]
    kt = np.stack([k1, k2], axis=1).reshape(16, 128, 128)
    d["keys_t"] = f(kt.transpose(2, 0, 1))
    d["peer_down_t"] = f(np.asarray(inp["peer_down"])[0].T)
    d["peer_up"] = f(np.asarray(inp["peer_up"])[0])
    d["final_w_p"] = f(np.broadcast_to(np.asarray(inp["final_norm_w"])[None, :], (128, 1024)))
    d["c_ident"] = f(np.eye(128))
    d["c_tri"] = f(r[:, None] <= r[None, :])
    d["c_negu"] = f(np.where(r[None, :] >= r[:, None], 0.0, NEG))
    d["c_negls"] = f(np.where(r[:, None] > r[None, :], 0.0, NEG))
    d["c_ones"] = f(np.ones((128, 128)))
    pf = np.ones((4, 16), np.float32)
    for g in range(4):
        w = 2 << g
        for t in range(16):
            pf[g, t] = w / min(t + 1, w)
    d["c_poolfix"] = f(np.broadcast_to(pf[None], (128, 4, 16)))
    return d


_NC_CACHE = {}


def kernel(**inputs):
    x = np.asarray(inputs["x"], dtype=np.float32)
    B, S_TOK, _ = x.shape
    if S_TOK not in _NC_CACHE:
        _NC_CACHE[S_TOK] = build(S_TOK)
    nc = _NC_CACHE[S_TOK]
    shared = host_inputs(inputs)
    in_maps = []
    for b in range(B):
        m = dict(shared)
        m["x"] = np.ascontiguousarray(x[b])
        in_maps.append(m)
    res = run_bass_kernel_spmd(nc, in_maps, core_ids=list(range(B)))
    return np.stack([np.asarray(r["y"]) for r in res.results], axis=0).astype(np.float32)
```

```python
from contextlib import ExitStack
import numpy as np
import concourse.bass as bass
import concourse.mybir as mybir
from concourse.bass_utils import run_bass_kernel_spmd

F32 = mybir.dt.float32
BF16 = mybir.dt.bfloat16
AF = mybir.ActivationFunctionType
ALU = mybir.AluOpType

NEG = -30000.0
EPS = 1e-6
IN_COLS = 6672
NE = 16384


class Buf:
    def __init__(self):
        self.last_w = None
        self.readers = []


class Tn:
    def __init__(self, t, b=None, psum=False):
        self.t = t
        self.b = b if b is not None else Buf()
        self.psum = psum


class Sched:
    CE = ("pe", "act", "dve", "pool")

    def __init__(self, nc, ctx, ndma=8, nsets=2):
        self.nc = nc
        self.eng = {}
        self.sets = [{} for _ in range(nsets)]
        for nm, obj in (("pe", nc.tensor), ("act", nc.scalar), ("dve", nc.vector),
                        ("pool", nc.gpsimd), ("sp", nc.sync)):
            for k in range(nsets):
                if nm != "sp":
                    self.sets[k][nm] = ctx.enter_context(nc.semaphore(f"s{k}_" + nm))
            self.eng[nm] = dict(name=nm, obj=obj, sem=self.sets[0].get(nm), count=0, waited={})
        self.epoch = 0
        self.dq = {}
        for q in ("sp", "pool"):
            sems = [ctx.enter_context(nc.semaphore(f"d_{q}{i}")) for i in range(ndma)]
            self.dq[q] = dict(sems=sems, n=0)
        self.ndma = ndma
        self.ninst = 0

    def _wait(self, e, tok):
        sem, val, ep = tok
        if ep is not None and ep < self.epoch:
            return
        if e["name"] == "pe" and sem is e["sem"]:
            return
        key = id(sem)
        w = e["waited"]
        if w.get(key, 0) >= val:
            return
        e["obj"].wait_ge(sem, val)
        w[key] = val
        self.ninst += 1

    def _deps(self, e, reads, writes):
        for b in reads:
            if b.last_w is not None:
                self._wait(e, b.last_w)
        for b in writes:
            if b.last_w is not None:
                self._wait(e, b.last_w)
            for r in b.readers:
                self._wait(e, r)

    def _commit(self, tok, reads, writes):
        for b in reads:
            b.readers = [r for r in b.readers if r[0] is not tok[0]] + [tok]
        for b in writes:
            b.last_w = tok
            b.readers = []

    def op(self, en, fn, reads=(), writes=()):
        e = self.eng[en]
        rb = [x.b for x in reads]
        wb = [x.b for x in writes] + [x.b for x in reads if x.psum]
        self._deps(e, rb, wb)
        inst = fn(e["obj"])
        e["count"] += 1
        inst.then_inc(e["sem"], 1)
        tok = (e["sem"], e["count"], self.epoch)
        self._commit(tok, rb, wb)
        self.ninst += 1
        return tok

    def dma(self, q, out, in_, reads=(), writes=()):
        e = self.eng[q]
        d = self.dq[q]
        n = d["n"]
        sem = d["sems"][n % self.ndma]
        if n >= self.ndma:
            self._wait(e, (sem, 16 * (n // self.ndma), None))
        rb = [x.b for x in reads]
        wb = [x.b for x in writes]
        self._deps(e, rb, wb)
        e["obj"].dma_start(out=out, in_=in_).then_inc(sem, 16)
        d["n"] = n + 1
        tok = (sem, 16 * (n // self.ndma + 1), None)
        self._commit(tok, rb, wb)
        self.ninst += 1
        return tok

    def _sync_all(self):
        for a in self.CE + ("sp",):
            for b in self.CE:
                if a != b and self.eng[b]["count"] > 0:
                    self._wait(self.eng[a], (self.eng[b]["sem"], self.eng[b]["count"], self.epoch))

    def barrier(self, switch=False):
        self._sync_all()
        if not switch:
            return
        self.epoch += 1
        new = self.sets[self.epoch]
        for nm, e in self.eng.items():
            e["sem"] = new.get(nm)
            e["count"] = 0

    def finish(self):
        for q, d in self.dq.items():
            e = self.eng[q]
            for i, sem in enumerate(d["sems"]):
                cnt = (d["n"] - i + self.ndma - 1) // self.ndma
                if cnt > 0:
                    e["obj"].wait_ge(sem, 16 * cnt)


def build(S_TOK, dbg=False, stage=99, sw_every=2):
    NST = S_TOK // 256
    nc = bass.Bass("TRN2", target_bir_lowering=False)

    def din(name, shape):
        return nc.dram_tensor(name, list(shape), F32, kind="ExternalInput").ap()

    x_d = din("x", [S_TOK, 1024])
    win_d = din("w_in_p", [1024, IN_COLS])
    mnw_d = din("mnw", [128, 8])
    fnw_d = din("fnw", [128, 8])
    pw_d = din("pool_w_p", [128, 4, 128])
    psc_d = din("pool_scale_p", [128, 4])
    cw_d = din("conv_w_p", [128, 24, 4])
    alog_d = din("a_log_p", [128, 8])
    dtb_d = din("dt_bias_p", [128, 8])
    dnw_d = din("dn_norm_p", [128, 1])
    wpu_d = din("w_pool_up", [512, 1024])
    wdu_d = din("w_dn_up", [1024, 1024])
    wo_d = din("w_mix_out", [1024, 1024])
    wq_d = din("peer_w_query", [1024, 2048])
    kt_d = din("keys_t", [128, 16, 128])
    dnT_d = din("peer_down_t", [1024, NE])
    up_d = din("peer_up", [NE, 1024])
    fnl_d = din("final_w_p", [128, 1024])
    ident_d = din("c_ident", [128, 128])
    tri_d = din("c_tri", [128, 128])
    negu_d = din("c_negu", [128, 128])
    negls_d = din("c_negls", [128, 128])
    ones_d = din("c_ones", [128, 128])
    pfix_d = din("c_poolfix", [128, 4, 16])
    y_d = nc.dram_tensor("y", [S_TOK, 1024], F32, kind="ExternalOutput").ap()
    if dbg:
        h1_d = nc.dram_tensor("h1dbg", [S_TOK, 1024], F32, kind="ExternalOutput").ap()

    win_s = Tn(nc.dram_tensor("win_s", [128, 8, IN_COLS], BF16).ap())
    wq_s = Tn(nc.dram_tensor("wq_s", [128, 8, 2048], BF16).ap())
    dn_s = Tn(nc.dram_tensor("dn_s", [128, 8, NE], BF16).ap())
    up_s = Tn(nc.dram_tensor("up_s", [128, 128, 1024], BF16).ap())

    with ExitStack() as ctx:
        NEP = (NST + sw_every - 1) // sw_every
        S = Sched(nc, ctx, nsets=NEP + 1)

        uid = [0]

        def un_(name):
            uid[0] += 1
            return f"t{uid[0]}_{name}"

        def sb(cx, name, shape, dt=F32):
            return Tn(cx.enter_context(nc.sbuf_tensor(un_(name), list(shape), dt)))

        def ps(cx, name, shape, dt=F32):
            return Tn(cx.enter_context(nc.psum_tensor(un_(name), list(shape), dt)), psum=True)

        def mm(o, o_ap, l, l_ap, r, r_ap, start=True, stop=True):
            S.op("pe", lambda e: e.matmul(o_ap, lhsT=l_ap, rhs=r_ap, start=start, stop=stop),
                 reads=[l, r], writes=[o])

        def tr(o, o_ap, i, i_ap, idn):
            S.op("pe", lambda e: e.transpose(out=o_ap, in_=i_ap, identity=idn.t[:]),
                 reads=[i, idn], writes=[o])

        def act(o, o_ap, i, i_ap, func, reads=(), wr=(), **kw):
            S.op("act", lambda e: e.activation(out=o_ap, in_=i_ap, func=func, **kw),
                 reads=[i] + list(reads), writes=[o] + list(wr))

        def ts(en, o, o_ap, i, i_ap, s1, s2, op0, op1=None, reads=()):
            if op1 is None:
                S.op(en, lambda e: e.tensor_scalar(out=o_ap, in0=i_ap, scalar1=s1, scalar2=None, op0=op0),
                     reads=[i] + list(reads), writes=[o])
            else:
                S.op(en, lambda e: e.tensor_scalar(out=o_ap, in0=i_ap, scalar1=s1, scalar2=s2, op0=op0, op1=op1),
                     reads=[i] + list(reads), writes=[o])

        def tt(en, o, o_ap, a, a_ap, b, b_ap, op):
            S.op(en, lambda e: e.tensor_tensor(out=o_ap, in0=a_ap, in1=b_ap, op=op),
                 reads=[a, b], writes=[o])

        def stt(en, o, o_ap, a, a_ap, sc, b, b_ap, op0, op1, reads=()):
            S.op(en, lambda e: e.scalar_tensor_tensor(out=o_ap, in0=a_ap, scalar=sc, in1=b_ap, op0=op0, op1=op1),
                 reads=[a, b] + list(reads), writes=[o])

        def cp(en, o, o_ap, i, i_ap):
            if en == "act":
                S.op("act", lambda e: e.copy(out=o_ap, in_=i_ap), reads=[i], writes=[o])
            else:
                S.op(en, lambda e: e.tensor_copy(out=o_ap, in_=i_ap), reads=[i], writes=[o])

        def rsqrt_col(o, i, scale, eps, reads=()):
            ts("dve", o, o.t[:], i, i.t[:], scale, eps, ALU.mult, ALU.add)
            act(o, o.t[:], o, o.t[:], AF.Sqrt)
            S.op("dve", lambda e: e.reciprocal(out=o.t[:], in_=o.t[:]), reads=[o], writes=[o])

        ident_f = sb(ctx, "ident_f", [128, 128])
        ident_b = sb(ctx, "ident_b", [128, 128], BF16)
        tri = sb(ctx, "tri", [128, 128])
        negu = sb(ctx, "negu", [128, 128])
        negls = sb(ctx, "negls", [128, 128])
        ones = sb(ctx, "ones", [128, 128])
        pfix = sb(ctx, "pfix", [128, 4, 16])
        mnw = sb(ctx, "mnw", [128, 8])
        fnw = sb(ctx, "fnw", [128, 8])
        psc = sb(ctx, "psc", [128, 4])
        cw = sb(ctx, "cw", [128, 24, 4])
        nA = sb(ctx, "nA", [128, 8])
        dtb = sb(ctx, "dtb", [128, 8])
        dnw = sb(ctx, "dnw", [128, 1])
        fnl = sb(ctx, "fnl", [128, 1024])
        wpu = sb(ctx, "wpu", [128, 4, 1024], BF16)
        wdu = sb(ctx, "wdu", [128, 8, 1024], BF16)
        wo = sb(ctx, "wo", [128, 8, 1024], BF16)
        pw = sb(ctx, "pw", [128, 4, 128], BF16)
        wsm = sb(ctx, "wsm", [128, 8, 16], BF16)
        KT = sb(ctx, "KT", [128, 16, 128], BF16)
        wpool = [sb(ctx, f"wpool{i}", [128, 4096], BF16) for i in range(5)]
        wp_n = [0]
        Sst = [sb(ctx, f"Sst{h}", [128, 128]) for h in range(8)]
        ccar = [sb(ctx, f"ccar{b}", [128, 3]) for b in range(24)]
        xab = [sb(ctx, f"xab{g}", [128, 272]) for g in range(4)]
        xh = [sb(ctx, f"xh{i}", [128, 1024]) for i in range(2)]
        yo_p = [sb(ctx, f"yo{i}", [128, 1024]) for i in range(2)]

        def wload(src, src_ap, view):
            t = wpool[wp_n[0] % len(wpool)]
            wp_n[0] += 1
            S.dma("sp", view(t.t), src_ap, reads=[src], writes=[t])
            return t

        v8 = lambda t: t[:].rearrange("p (c n) -> p c n", c=8)
        v4 = lambda t: t[:].rearrange("p (c n) -> p c n", c=4)

        dummy = Tn(None)
        for (t, d) in ((ident_f, ident_d), (tri, tri_d), (negu, negu_d), (negls, negls_d), (ones, ones_d),
                       (pfix, pfix_d), (mnw, mnw_d), (fnw, fnw_d), (psc, psc_d), (cw, cw_d), (nA, alog_d),
                       (dtb, dtb_d), (dnw, dnw_d), (fnl, fnl_d)):
            S.dma("sp", t.t[:], d, writes=[t])
        cp("dve", ident_b, ident_b.t[:], ident_f, ident_f.t[:])
        act(nA, nA.t[:], nA, nA.t[:], AF.Exp)
        ts("dve", nA, nA.t[:], nA, nA.t[:], -1.0, None, ALU.mult)
        for h in range(8):
            S.op("dve", lambda e: e.memset(Sst[h].t[:], 0.0), writes=[Sst[h]])
        for b in range(24):
            S.op("pool", lambda e: e.memset(ccar[b].t[:], 0.0), writes=[ccar[b]])
        for g in range(4):
            S.op("pool", lambda e: e.memset(xab[g].t[:], 0.0), writes=[xab[g]])

        with ExitStack() as pc:
            stg = [sb(pc, f"stg{i}", [128, 4096]) for i in range(2)]
            sn = [0]

            def prep(src_ap, shape3, dst, dst_ap, scale_t=None, scale_ap=None):
                a, b = shape3
                st_ = stg[sn[0] % 2]
                sn[0] += 1
                sv = st_.t[:, 0:a * b].rearrange("p (a b) -> p a b", a=a)
                S.dma("sp", sv, src_ap, writes=[st_])
                ob = wpool[wp_n[0] % len(wpool)]
                wp_n[0] += 1
                ov = ob.t[:, 0:a * b].rearrange("p (a b) -> p a b", a=a)
                en = "dve" if sn[0] % 2 == 0 else "pool"
                if scale_t is None:
                    cp("act" if sn[0] % 2 == 0 else "dve", ob, ov, st_, sv)
                else:
                    tt(en, ob, ov, st_, sv, scale_t, scale_ap, ALU.mult)
                if dst is None:
                    return ob, ov
                S.dma("pool", dst_ap, ov, reads=[ob], writes=[dst])
                return ob, ov

            for (wt, d, a, b, rs) in ((wpu, wpu_d, 4, 1024, "(g p) n -> p g n"),
                                      (wo, wo_d, 8, 1024, "(c p) n -> p c n")):
                for half in range(a // 4):
                    ob, ov = prep(d.rearrange(rs, p=128)[:, half * 4:(half + 1) * 4, :], (4, 1024), None, None)
                    cp("dve", wt, wt.t[:, half * 4:(half + 1) * 4, :], ob, ov)
            for half in range(2):
                ob, ov = prep(wdu_d.rearrange("(h p) n -> p h n", p=128)[:, half * 4:(half + 1) * 4, :], (4, 1024),
                              None, None, dnw, dnw.t[:, 0:1].unsqueeze(2).to_broadcast([128, 4, 1024]))
                cp("dve", wdu, wdu.t[:, half * 4:(half + 1) * 4, :], ob, ov)
            ob, ov = prep(pw_d, (4, 128), None, None)
            cp("dve", pw, pw.t[:], ob, ov)
            ob, ov = prep(kt_d, (16, 128), None, None)
            cp("dve", KT, KT.t[:], ob, ov)
            winv = win_d.rearrange("(c p) n -> p c n", p=128)
            for blk in range(14):
                c0 = blk * 512
                n = min(512, IN_COLS - c0)
                ob, ov = prep(winv[:, :, c0:c0 + n], (8, n), win_s, win_s.t[:, :, c0:c0 + n],
                              mnw, mnw.t[:].unsqueeze(2).to_broadcast([128, 8, n]))
                if blk == 13:
                    cp("dve", wsm, wsm.t[:], ob, ov)
            wqv = wq_d.rearrange("(c p) n -> p c n", p=128)
            for blk in range(4):
                prep(wqv[:, :, blk * 512:(blk + 1) * 512], (8, 512), wq_s, wq_s.t[:, :, blk * 512:(blk + 1) * 512],
                     fnw, fnw.t[:].unsqueeze(2).to_broadcast([128, 8, 512]))
            dnv = dnT_d.rearrange("(c p) e -> p c e", p=128)
            upv = up_d.rearrange("(ch p) d -> p ch d", p=128)
            for blk in range(32 if stage >= 2 else 0):
                prep(dnv[:, :, blk * 512:(blk + 1) * 512], (8, 512), dn_s, dn_s.t[:, :, blk * 512:(blk + 1) * 512],
                     fnw, fnw.t[:].unsqueeze(2).to_broadcast([128, 8, 512]))
                prep(upv[:, blk * 4:(blk + 1) * 4, :], (4, 1024), up_s, up_s.t[:, blk * 4:(blk + 1) * 4, :])
        S.barrier(switch=True)

        def mixer(st):
            t0 = st * 256
            with ExitStack() as mx:
                pTb = ps(mx, "pTb", [128, 1024], BF16)
                big = [ps(mx, f"big{k}", [128, 512]) for k in range(4)]
                bn = [0]
                qbank = [mx.enter_context(nc.psum_tensor(un_(f"qb{k}"), [128, 512], F32)) for k in range(3)]
                qsl = [Tn(qbank[k], psum=True) for k in range(3)]
                qn = [0]

                def nbig():
                    b = big[bn[0] % 4]
                    bn[0] += 1
                    return b

                def nq():
                    k = qn[0] % 12
                    qn[0] += 1
                    s = qsl[k % 3]
                    qq = k // 3
                    return s, s.t[:, qq * 128:(qq + 1) * 128]

                sq = sb(mx, "sq", [128, 1024])
                xsb = sb(mx, "xsb", [128, 1024], BF16)
                xT = sb(mx, "xT", [128, 8, 256], BF16)
                ss = sb(mx, "ss", [128, 1])
                rstd = sb(mx, "rstd", [128, 1])
                raw = [sb(mx, f"raw{k}", [128, 259]) for k in range(3)]
                cacc = sb(mx, "cacc", [128, 256])
                qkvT = [sb(mx, f"qkvT{k}", [128, 256]) for k in range(3)]
                szT = sb(mx, "szT", [128, 256])
                sqn = sb(mx, "sqn", [128, 256])
                rn = sb(mx, "rn", [128, 256])
                lg = [sb(mx, f"lg{i}", [128, 16]) for i in range(2)]
                beta = [sb(mx, f"beta{i}", [128, 8]) for i in range(2)]
                nbeta = [sb(mx, f"nbeta{i}", [128, 8]) for i in range(2)]
                gg = [sb(mx, f"gg{i}", [128, 8]) for i in range(2)]
                gc = [sb(mx, f"gc{i}", [128, 8]) for i in range(2)]
                egl = [sb(mx, f"egl{i}", [128, 8]) for i in range(2)]
                kds = [sb(mx, f"kds{i}", [128, 8]) for i in range(2)]
                bgs = [sb(mx, f"bgs{i}", [128, 8]) for i in range(2)]
                kbg = sb(mx, "kbg", [128, 128])
                kdec = sb(mx, "kdec", [128, 128])
                vb = sb(mx, "vb", [128, 128])
                trig = sb(mx, "trig", [128, 128])
                Am = sb(mx, "Am", [128, 128])
                EU = sb(mx, "EU", [128, 128])
                DLs = sb(mx, "DLs", [128, 128])
                egrow = sb(mx, "egrow", [128, 128])
                Mb = [sb(mx, f"Mb{k}", [128, 128]) for k in range(2)]
                MTb = [sb(mx, f"MTb{k}", [128, 128]) for k in range(2)]
                PTb = [sb(mx, f"PTb{k}", [128, 128]) for k in range(2)]
                attnT = sb(mx, "attnT", [128, 128])
                qgT = sb(mx, "qgT", [128, 128])
                nwT = sb(mx, "nwT", [128, 128])
                vn = sb(mx, "vn", [128, 128])
                oss = sb(mx, "oss", [128, 1])
                orst = sb(mx, "orst", [128, 1])
                osq = sb(mx, "osq", [128, 128])
                onb = sb(mx, "onb", [128, 128], BF16)
                onT = sb(mx, "onT", [128, 8, 256], BF16)
                pa = [sb(mx, f"pa{k}", [128, 272]) for k in range(2)]
                pooledb = sb(mx, "pooledb", [128, 256], BF16)
                paT = sb(mx, "paT", [128, 4, 256], BF16)
                sga = sb(mx, "sga", [128, 256])
                m1 = sb(mx, "m1", [128, 256])
                sgb = sb(mx, "sgb", [128, 256])
                m2 = sb(mx, "m2", [128, 256])
                mT = sb(mx, "mT", [128, 8, 256], BF16)

                for i in range(2):
                    S.dma("sp", xh[i].t[:], x_d[t0 + i * 128:t0 + (i + 1) * 128, :], writes=[xh[i]])
                    act(sq, sq.t[:], xh[i], xh[i].t[:], AF.Square, wr=[ss], accum_out=ss.t[:])
                    rsqrt_col(rstd, ss, 1.0 / 1024, EPS)
                    ts("dve", xsb, xsb.t[:], xh[i], xh[i].t[:], rstd.t[:, 0:1], None, ALU.mult, reads=[rstd])
                    for c in range(8):
                        tr(pTb, pTb.t[:, c * 128:(c + 1) * 128], xsb, xsb.t[:, c * 128:(c + 1) * 128], ident_b)
                    cp("act", xT, xT.t[:, :, i * 128:(i + 1) * 128], pTb,
                       pTb.t[:].rearrange("p (c n) -> p c n", c=8))

                for i in range(2):
                    s_, s_ap = nq()
                    for c in range(8):
                        mm(s_, s_ap[:, 0:16], xT, xT.t[:, c, i * 128:(i + 1) * 128], wsm, wsm.t[:, c, :],
                           start=(c == 0), stop=(c == 7))
                    cp("dve", lg[i], lg[i].t[:], s_, s_ap[:, 0:16])
                    act(beta[i], beta[i].t[:], lg[i], lg[i].t[:, 0:8], AF.Sigmoid)
                    ts("dve", nbeta[i], nbeta[i].t[:], beta[i], beta[i].t[:], -1.0, None, ALU.mult)
                    tt("dve", gg[i], gg[i].t[:], lg[i], lg[i].t[:, 8:16], dtb, dtb.t[:], ALU.add)
                    act(gg[i], gg[i].t[:], gg[i], gg[i].t[:], AF.Exp)
                    act(gg[i], gg[i].t[:], gg[i], gg[i].t[:], AF.Ln, bias=1.0)
                    tt("dve", gg[i], gg[i].t[:], gg[i], gg[i].t[:], nA, nA.t[:], ALU.mult)
                    s_, s_ap = nq()
                    mm(s_, s_ap[:, 0:8], tri, tri.t[:], gg[i], gg[i].t[:])
                    cp("dve", gc[i], gc[i].t[:], s_, s_ap[:, 0:8])
                    s_, s_ap = nq()
                    mm(s_, s_ap[:, 0:8], ones, ones.t[:], gg[i], gg[i].t[:])
                    tt("dve", kds[i], kds[i].t[:], s_, s_ap[:, 0:8], gc[i], gc[i].t[:], ALU.subtract)
                    act(kds[i], kds[i].t[:], kds[i], kds[i].t[:], AF.Exp)
                    act(egl[i], egl[i].t[:], s_, s_ap[:, 0:8], AF.Exp)
                    act(bgs[i], bgs[i].t[:], gc[i], gc[i].t[:], AF.Exp)
                    tt("dve", bgs[i], bgs[i].t[:], bgs[i], bgs[i].t[:], beta[i], beta[i].t[:], ALU.mult)

                for h in range(8):
                    wb = wload(win_s, win_s.t[:, :, h * 512:(h + 1) * 512], v8)
                    wv = v8(wb.t)
                    for k in range(4):
                        pb = nbig()
                        for c in range(8):
                            mm(pb, pb.t[:, 0:256], wb, wv[:, c, k * 128:(k + 1) * 128], xT, xT.t[:, c, :],
                               start=(c == 0), stop=(c == 7))
                        if k == 3:
                            act(szT, szT.t[:], pb, pb.t[:, 0:256], AF.Silu)
                            continue
                        blk = k * 8 + h
                        r = raw[k]
                        cp("pool", r, r.t[:, 0:3], ccar[blk], ccar[blk].t[:])
                        cp("act", r, r.t[:, 3:259], pb, pb.t[:, 0:256])
                        cp("pool", ccar[blk], ccar[blk].t[:], r, r.t[:, 256:259])
                        ts("dve", cacc, cacc.t[:], r, r.t[:, 0:256], cw.t[:, blk, 0:1], None, ALU.mult, reads=[cw])
                        for j in range(1, 4):
                            stt("dve", cacc, cacc.t[:], r, r.t[:, j:j + 256], cw.t[:, blk, j:j + 1], cacc, cacc.t[:],
                                ALU.mult, ALU.add, reads=[cw])
                        act(qkvT[k], qkvT[k].t[:], cacc, cacc.t[:], AF.Silu)
                        if k < 2:
                            tt("dve", sqn, sqn.t[:], qkvT[k], qkvT[k].t[:], qkvT[k], qkvT[k].t[:], ALU.mult)
                            pn = nbig()
                            mm(pn, pn.t[:, 0:256], ones, ones.t[:], sqn, sqn.t[:])
                            ts("dve", rn, rn.t[:], pn, pn.t[:, 0:256], EPS, None, ALU.add)
                            act(rn, rn.t[:], rn, rn.t[:], AF.Sqrt)
                            S.op("dve", lambda e: e.reciprocal(out=rn.t[:], in_=rn.t[:]), reads=[rn], writes=[rn])
                            sc = (128.0 ** -0.5) if k == 0 else 1.0
                            stt("dve", qkvT[k], qkvT[k].t[:], qkvT[k], qkvT[k].t[:], sc, rn, rn.t[:],
                                ALU.mult, ALU.mult)
                    qT_, kT_, vT_ = qkvT
                    for i in range(2):
                        sl = slice(i * 128, (i + 1) * 128)
                        hh = slice(h, h + 1)
                        k_s, k_ap = nq()
                        tr(k_s, k_ap, kT_, kT_.t[:, sl], ident_f)
                        v_s, v_ap = nq()
                        tr(v_s, v_ap, vT_, vT_.t[:, sl], ident_f)
                        g_s, g_ap = nq()
                        mm(g_s, g_ap, kT_, kT_.t[:, sl], kT_, kT_.t[:, sl])
                        a_s, a_ap = nq()
                        mm(a_s, a_ap, kT_, kT_.t[:, sl], qT_, qT_.t[:, sl])
                        ts("dve", kbg, kbg.t[:], k_s, k_ap, bgs[i].t[:, hh], None, ALU.mult, reads=[bgs[i]])
                        act(kdec, kdec.t[:], k_s, k_ap, AF.Copy, reads=[kds[i]], scale=kds[i].t[:, hh])
                        act(vb, vb.t[:], v_s, v_ap, AF.Copy, reads=[beta[i]], scale=beta[i].t[:, hh])
                        ts("pool", trig, trig.t[:], tri, tri.t[:], gg[i].t[:, hh], None, ALU.mult, reads=[gg[i]])
                        r_s, r_ap = nq()
                        mm(r_s, r_ap, ones, ones.t[:], trig, trig.t[:])
                        ts("dve", Am, Am.t[:], r_s, r_ap, gc[i].t[:, hh], None, ALU.subtract, reads=[gc[i]])
                        act(egrow, egrow.t[:], r_s, r_ap, AF.Exp)
                        tt("pool", EU, EU.t[:], Am, Am.t[:], negu, negu.t[:], ALU.add)
                        act(EU, EU.t[:], EU, EU.t[:], AF.Exp)
                        tt("pool", DLs, DLs.t[:], negls, negls.t[:], Am, Am.t[:], ALU.subtract)
                        act(DLs, DLs.t[:], DLs, DLs.t[:], AF.Exp)
                        M, MT, PT = Mb[0], MTb[0], PTb[0]
                        stt("dve", M, M.t[:], g_s, g_ap, nbeta[i].t[:, hh], DLs, DLs.t[:], ALU.mult, ALU.mult,
                            reads=[nbeta[i]])
                        tt("dve", attnT, attnT.t[:], a_s, a_ap, EU, EU.t[:], ALU.mult)
                        tt("pool", qgT, qgT.t[:], qT_, qT_.t[:, sl], egrow, egrow.t[:], ALU.mult)
                        t_s, t_ap = nq()
                        tr(t_s, t_ap, M, M.t[:], ident_f)
                        cp("act", MT, MT.t[:], t_s, t_ap)
                        tt("dve", PT, PT.t[:], t_s, t_ap, ident_f, ident_f.t[:], ALU.add)
                        for kk in range(6):
                            Mn, MTn, PTn = Mb[(kk + 1) % 2], MTb[(kk + 1) % 2], PTb[(kk + 1) % 2]
                            s1, s1ap = nq()
                            mm(s1, s1ap, MT, MT.t[:], M, M.t[:])
                            cp("act", Mn, Mn.t[:], s1, s1ap)
                            if kk < 5:
                                s2, s2ap = nq()
                                mm(s2, s2ap, M, M.t[:], MT, MT.t[:])
                                cp("dve", MTn, MTn.t[:], s2, s2ap)
                            s3, s3ap = nq()
                            mm(s3, s3ap, Mn, Mn.t[:], PT, PT.t[:])
                            tt("dve", PTn, PTn.t[:], s3, s3ap, PT, PT.t[:], ALU.add)
                            M, MT, PT = Mn, MTn, PTn
                        w_s, w_ap = nq()
                        mm(w_s, w_ap, kbg, kbg.t[:], PT, PT.t[:])
                        S.op("act", lambda e: e.mul(out=nwT.t[:], in_=w_ap, mul=-1.0), reads=[w_s], writes=[nwT])
                        n_s, n_ap = nq()
                        mm(n_s, n_ap, PT, PT.t[:], vb, vb.t[:], start=True, stop=False)
                        mm(n_s, n_ap, nwT, nwT.t[:], Sst[h], Sst[h].t[:], start=False, stop=True)
                        cp("dve", vn, vn.t[:], n_s, n_ap)
                        o_s, o_ap = nq()
                        mm(o_s, o_ap, qgT, qgT.t[:], Sst[h], Sst[h].t[:], start=True, stop=False)
                        mm(o_s, o_ap, attnT, attnT.t[:], vn, vn.t[:], start=False, stop=True)
                        u_s, u_ap = nq()
                        mm(u_s, u_ap, kdec, kdec.t[:], vn, vn.t[:])
                        stt("dve", Sst[h], Sst[h].t[:], Sst[h], Sst[h].t[:], egl[i].t[:, hh], u_s, u_ap,
                            ALU.mult, ALU.add, reads=[egl[i]])
                        act(osq, osq.t[:], o_s, o_ap, AF.Square, wr=[oss], accum_out=oss.t[:])
                        rsqrt_col(orst, oss, 1.0 / 128, EPS)
                        act(onb, onb.t[:], o_s, o_ap, AF.Copy, reads=[orst], scale=orst.t[:, 0:1])
                        pslot = pTb.t[:, (h % 8) * 128:(h % 8 + 1) * 128]
                        tr(pTb, pslot, onb, onb.t[:], ident_b)
                        tt("dve", onT, onT.t[:, h, sl], pTb, pslot, szT, szT.t[:, sl], ALU.mult)

                wb = wload(win_s, win_s.t[:, :, 4096:4608], v8)
                wv = v8(wb.t)
                for g in range(4):
                    win_ = 2 << g
                    pb = nbig()
                    for c in range(8):
                        mm(pb, pb.t[:, 0:256], wb, wv[:, c, g * 128:(g + 1) * 128], xT, xT.t[:, c, :],
                           start=(c == 0), stop=(c == 7))
                    xg = xab[g]
                    cp("pool", xg, xg.t[:, 0:16], xg, xg.t[:, 256:272])
                    cp("act", xg, xg.t[:, 16:272], pb, pb.t[:, 0:256])
                    src = xg
                    sh = 1
                    for stp in range(g + 1):
                        dst = pa[stp % 2]
                        lo = 2 * sh - 1
                        tt("dve", dst, dst.t[:, lo:272], src, src.t[:, lo:272], src, src.t[:, lo - sh:272 - sh], ALU.add)
                        src = dst
                        sh *= 2
                    if st == 0:
                        tt("dve", src, src.t[:, 16:32], src, src.t[:, 16:32], pfix, pfix.t[:, g, :], ALU.mult)
                    stt("dve", pooledb, pooledb.t[:], src, src.t[:, 16:272], 1.0 / win_, xg, xg.t[:, 16:272],
                        ALU.mult, ALU.subtract)
                    pb2 = nbig()
                    mm(pb2, pb2.t[:, 0:256], pw, pw.t[:, g, :], pooledb, pooledb.t[:])
                    act(paT, paT.t[:, g, :], pb2, pb2.t[:, 0:256], AF.Copy, reads=[psc], scale=psc.t[:, g:g + 1])

                wga = [wload(win_s, win_s.t[:, :, 4608 + k * 512:4608 + (k + 1) * 512], v8) for k in range(2)]
                wgb = [wload(win_s, win_s.t[:, :, 5632 + k * 512:5632 + (k + 1) * 512], v8) for k in range(2)]
                for j in range(8):
                    js = slice(j * 128, (j + 1) * 128)
                    jw = slice((j % 4) * 128, (j % 4 + 1) * 128)
                    pA, pB, pC, pD = big[0], big[1], big[2], big[3]
                    for g in range(4):
                        mm(pA, pA.t[:, 0:256], wpu, wpu.t[:, g, js], paT, paT.t[:, g, :], start=(g == 0), stop=(g == 3))
                    wa = wga[j // 4]
                    for c in range(8):
                        mm(pB, pB.t[:, 0:256], wa, v8(wa.t)[:, c, jw], xT, xT.t[:, c, :], start=(c == 0), stop=(c == 7))
                    for hd in range(8):
                        mm(pC, pC.t[:, 0:256], wdu, wdu.t[:, hd, js], onT, onT.t[:, hd, :], start=(hd == 0), stop=(hd == 7))
                    wg = wgb[j // 4]
                    for c in range(8):
                        mm(pD, pD.t[:, 0:256], wg, v8(wg.t)[:, c, jw], xT, xT.t[:, c, :], start=(c == 0), stop=(c == 7))
                    act(sga, sga.t[:], pB, pB.t[:, 0:256], AF.Sigmoid)
                    tt("dve", m1, m1.t[:], pA, pA.t[:, 0:256], sga, sga.t[:], ALU.mult)
                    act(sgb, sgb.t[:], pD, pD.t[:, 0:256], AF.Sigmoid)
                    tt("dve", m2, m2.t[:], pC, pC.t[:, 0:256], sgb, sgb.t[:], ALU.mult)
                    tt("pool", mT, mT.t[:, j, :], m1, m1.t[:], m2, m2.t[:], ALU.add)

                for i in range(2):
                    for n in range(2):
                        pb = nbig()
                        for k in range(8):
                            mm(pb, pb.t[:], mT, mT.t[:, k, i * 128:(i + 1) * 128], wo, wo.t[:, k, n * 512:(n + 1) * 512],
                               start=(k == 0), stop=(k == 7))
                        tt("dve", xh[i], xh[i].t[:, n * 512:(n + 1) * 512], pb, pb.t[:],
                           xh[i], xh[i].t[:, n * 512:(n + 1) * 512], ALU.add)
                    if dbg:
                        S.dma("pool", h1_d[t0 + i * 128:t0 + (i + 1) * 128, :], xh[i].t[:], reads=[xh[i]], writes=[Tn(None)])

        def peer(st):
            t0 = st * 256
            with ExitStack() as px:
                pY = [[ps(px, f"pY{i}{n}", [128, 512]) for n in range(2)] for i in range(2)]
                pU = [ps(px, f"pU{k}", [128, 512]) for k in range(2)]
                pTb = ps(px, "pTbp", [128, 1024], BF16)
                pM = ps(px, "pM", [128, 512])
                hsb = sb(px, "hsb", [128, 1024], BF16)
                xn2T = sb(px, "xn2T", [128, 8, 256], BF16)
                ss = sb(px, "pss", [128, 1])
                rstd = sb(px, "prstd", [128, 1])
                qT = sb(px, "pqT", [128, 16, 256], BF16)
                sc = [sb(px, f"sc{i}", [128, 2048]) for i in range(2)]
                v16 = [sb(px, f"v16{k}", [128, 16]) for k in range(2)]
                tmp128 = sb(px, "tmp128", [128, 128])
                cand = sb(px, "cand", [128, 256])
                cand2 = sb(px, "cand2", [128, 256])
                c16 = sb(px, "c16", [128, 16])
                ex16 = sb(px, "ex16", [128, 16])
                negm = sb(px, "negm", [128, 1])
                zz = sb(px, "zz", [128, 1])
                taum = [sb(px, f"taum{i}", [128, 8]) for i in range(2)]
                ttau = [sb(px, f"ttau{i}", [128, 8]) for i in range(2)]
                cand3 = cand
                c24 = sb(px, "c24", [128, 8])
                tsum = sb(px, "tsum", [128, 1])
                E1n = [sb(px, f"E1n{i}", [128, 8, 128]) for i in range(2)]
                E2 = [sb(px, f"E2{i}", [128, 8, 128]) for i in range(2)]
                gms = [[sb(px, f"gm{u}_{h}", [128, 512], BF16) for h in range(8)] for u in range(2)]
                ncc = [sb(px, f"ncc{i}", [128, 8]) for i in range(2)]
                Ag = [sb(px, f"Ag{k}", [128, 512]) for k in range(2)]
                Tt = [sb(px, f"Tt{k}", [128, 512]) for k in range(4)]
                GA = sb(px, "GA", [128, 512], BF16)
                GAT = [sb(px, f"GAT{k}", [128, 4, 128], BF16) for k in range(2)]
                hf = sb(px, "hf", [128, 1024])

                for i in range(2):
                    act(hf, hf.t[:], xh[i], xh[i].t[:], AF.Square, wr=[ss], accum_out=ss.t[:])
                    rsqrt_col(rstd, ss, 1.0 / 1024, EPS)
                    ts("dve", hsb, hsb.t[:], xh[i], xh[i].t[:], rstd.t[:, 0:1], None, ALU.mult, reads=[rstd])
                    for c in range(8):
                        tr(pTb, pTb.t[:, c * 128:(c + 1) * 128], hsb, hsb.t[:, c * 128:(c + 1) * 128], ident_b)
                    cp("act", xn2T, xn2T.t[:, :, i * 128:(i + 1) * 128], pTb,
                       pTb.t[:].rearrange("p (c n) -> p c n", c=8))
                for blk in range(4):
                    wb = wload(wq_s, wq_s.t[:, :, blk * 512:(blk + 1) * 512], v8)
                    wv = v8(wb.t)
                    for jj in range(4):
                        jb = blk * 4 + jj
                        for c in range(8):
                            mm(pM, pM.t[:, 0:256], wb, wv[:, c, jj * 128:(jj + 1) * 128], xn2T, xn2T.t[:, c, :],
                               start=(c == 0), stop=(c == 7))
                        cp("act" if jb % 2 == 0 else "dve", qT, qT.t[:, jb, :], pM, pM.t[:, 0:256])
                for i in range(2):
                    for b4 in range(4):
                        for jj in range(4):
                            jb = b4 * 4 + jj
                            mm(pM, pM.t[:, jj * 128:(jj + 1) * 128], qT, qT.t[:, jb, i * 128:(i + 1) * 128],
                               KT, KT.t[:, jb, :])
                        cp("act", sc[i], sc[i].t[:, b4 * 512:(b4 + 1) * 512], pM, pM.t[:])
                for i in range(2):
                    for h in range(8):
                        for half in range(2):
                            sv = sc[i].t[:, (h * 2 + half) * 128:(h * 2 + half + 1) * 128]
                            vv = v16[half]
                            S.op("dve", lambda e: e.max(out=vv.t[:, 0:8], in_=sv), reads=[sc[i]], writes=[vv])
                            S.op("dve", lambda e: e.match_replace(out=tmp128.t[:], in_to_replace=vv.t[:, 0:8],
                                                                  in_values=sv, imm_value=-1e30),
                                 reads=[sc[i], vv], writes=[tmp128])
                            S.op("dve", lambda e: e.max(out=vv.t[:, 8:16], in_=tmp128.t[:]), reads=[tmp128], writes=[vv])
                        tt("dve", cand, cand.t[:].rearrange("p (a b) -> p a b", a=16),
                           v16[0], v16[0].t[:].unsqueeze(2).to_broadcast([128, 16, 16]),
                           v16[1], v16[1].t[:].unsqueeze(1).to_broadcast([128, 16, 16]), ALU.add)
                        S.op("dve", lambda e: e.max(out=c16.t[:, 0:8], in_=cand.t[:]), reads=[cand], writes=[c16])
                        S.op("dve", lambda e: e.match_replace(out=cand2.t[:], in_to_replace=c16.t[:, 0:8],
                                                              in_values=cand.t[:], imm_value=-1e30),
                             reads=[cand, c16], writes=[cand2])
                        S.op("dve", lambda e: e.max(out=c16.t[:, 8:16], in_=cand2.t[:]), reads=[cand2], writes=[c16])
                        ts("dve", negm, negm.t[:], c16, c16.t[:, 0:1], -1.0, None, ALU.mult)
                        act(ex16, ex16.t[:], c16, c16.t[:], AF.Exp, reads=[negm], wr=[zz], bias=negm.t[:, 0:1],
                            accum_out=zz.t[:])
                        act(zz, zz.t[:], zz, zz.t[:], AF.Ln)
                        tt("dve", ncc[i], ncc[i].t[:, h:h + 1], negm, negm.t[:], zz, zz.t[:], ALU.subtract)
                        S.op("dve", lambda e: e.match_replace(out=cand3.t[:], in_to_replace=c16.t[:, 8:16],
                                                              in_values=cand2.t[:], imm_value=-1e30),
                             reads=[cand2, c16], writes=[cand3])
                        S.op("dve", lambda e: e.max(out=c24.t[:], in_=cand3.t[:]), reads=[cand3], writes=[c24])
                        tt("dve", tsum, tsum.t[:], c16, c16.t[:, 15:16], c24, c24.t[:, 0:1], ALU.add)
                        ts("dve", taum[i], taum[i].t[:, h:h + 1], tsum, tsum.t[:], 0.5, None, ALU.mult)
                    tt("dve", ttau[i], ttau[i].t[:], taum[i], taum[i].t[:], ncc[i], ncc[i].t[:], ALU.add)
                    act(ttau[i], ttau[i].t[:], ttau[i], ttau[i].t[:], AF.Exp)
                    for h in range(8):
                        act(E1n[i], E1n[i].t[:, h, :], sc[i], sc[i].t[:, (h * 2) * 128:(h * 2 + 1) * 128], AF.Exp,
                            reads=[ncc[i]], bias=ncc[i].t[:, h:h + 1])
                    act(E2[i], E2[i].t[:], sc[i],
                        sc[i].t[:].rearrange("p (h t k) -> p h t k", h=8, t=2)[:, :, 1, :], AF.Exp)
                pu = pU[0]
                pAccs = [pM, pU[1]]
                wts = {}
                cnt = dict(tn=0)

                def stA(n):
                    eb, i = divmod(n, 2)
                    if i == 0:
                        wts[eb] = (wload(dn_s, dn_s.t[:, :, eb * 512:(eb + 1) * 512], v8),
                                   wload(up_s, up_s.t[:, eb * 4:(eb + 1) * 4, :], v4))
                    dnb = wts[eb][0]
                    dv = v8(dnb.t)
                    ag = Ag[n % 2]
                    for c in range(8):
                        mm(pu, pu.t[:], xn2T, xn2T.t[:, c, i * 128:(i + 1) * 128], dnb, dv[:, c, :],
                           start=(c == 0), stop=(c == 7))
                    act(ag, ag.t[:], pu, pu.t[:], AF.Gelu)

                def stT(n):
                    eb, i = divmod(n, 2)
                    for h in range(8):
                        Tb = Tt[cnt["tn"] % 4]
                        cnt["tn"] += 1
                        if h < 2:
                            for q in range(4):
                                act(Tb, Tb.t[:, q * 128:(q + 1) * 128], E2[i], E2[i].t[:, h, :], AF.Copy,
                                    reads=[E1n[i]], scale=E1n[i].t[:, h, eb * 4 + q:eb * 4 + q + 1])
                        else:
                            tt("dve" if h == 7 else "pool", Tb, Tb.t[:].rearrange("p (a b) -> p a b", a=4),
                               E1n[i], E1n[i].t[:, h, eb * 4:eb * 4 + 4].unsqueeze(2).to_broadcast([128, 4, 128]),
                               E2[i], E2[i].t[:, h, :].unsqueeze(1).to_broadcast([128, 4, 128]), ALU.mult)
                        gm = gms[n % 2][h]
                        stt("dve", gm, gm.t[:], Tb, Tb.t[:], ttau[i].t[:, h:h + 1], Tb, Tb.t[:],
                            ALU.is_ge, ALU.mult, reads=[ttau[i]])

                def stAcc(n):
                    pAcc = pAccs[n % 2]
                    for h in range(8):
                        gm = gms[n % 2][h]
                        mm(pAcc, pAcc.t[:], ident_b, ident_b.t[:], gm, gm.t[:], start=(h == 0), stop=(h == 7))

                def stG(n):
                    pAcc = pAccs[n % 2]
                    ag = Ag[n % 2]
                    gat = GAT[n % 2]
                    tt("dve", GA, GA.t[:], pAcc, pAcc.t[:], ag, ag.t[:], ALU.mult)
                    half = (n % 2) * 512
                    for q in range(4):
                        tr(pTb, pTb.t[:, half + q * 128:half + (q + 1) * 128], GA, GA.t[:, q * 128:(q + 1) * 128], ident_b)
                    cp("act", gat, gat.t[:], pTb, pTb.t[:, half:half + 512].rearrange("p (a b) -> p a b", a=4))

                def stY(n):
                    eb, i = divmod(n, 2)
                    gat = GAT[n % 2]
                    upb = wts[eb][1]
                    uv = v4(upb.t)
                    for q in range(4):
                        for nn in range(2):
                            mm(pY[i][nn], pY[i][nn].t[:], gat, gat.t[:, q, :], upb, uv[:, q, nn * 512:(nn + 1) * 512],
                               start=(eb == 0 and q == 0), stop=(eb == 31 and q == 3))

                NU = 64
                for k in range(NU + 2):
                    if k < NU:
                        stA(k)
                        stT(k)
                    if 0 <= k - 1 < NU:
                        stAcc(k - 1)
                    if 0 <= k - 2 < NU:
                        stY(k - 2)
                    if 0 <= k - 1 < NU:
                        stG(k - 1)
                for i in range(2):
                    for n in range(2):
                        tt("dve", hf, hf.t[:, n * 512:(n + 1) * 512], pY[i][n], pY[i][n].t[:],
                           xh[i], xh[i].t[:, n * 512:(n + 1) * 512], ALU.add)
                    act(yo_p[i], yo_p[i].t[:], hf, hf.t[:], AF.Square, wr=[ss], accum_out=ss.t[:])
                    rsqrt_col(rstd, ss, 1.0 / 1024, EPS)
                    yo = yo_p[i]
                    stt("dve", yo, yo.t[:], hf, hf.t[:], rstd.t[:, 0:1], fnl, fnl.t[:], ALU.mult, ALU.mult, reads=[rstd])
                    S.dma("pool", y_d[t0 + i * 128:t0 + (i + 1) * 128, :], yo.t[:], reads=[yo], writes=[Tn(None)])

        for st in range(NST):
            if stage >= 1:
                mixer(st)
                S.barrier()
            if stage >= 2:
                peer(st)
                S.barrier(switch=((st + 1) % sw_every == 0 and st + 1 < NST))
        S.finish()
        print("ninst", S.ninst, {k: v["count"] for k, v in S.eng.items()})
    return nc


def host_inputs(inp):
    f = lambda a: np.ascontiguousarray(np.asarray(a, dtype=np.float32))
    w_in = np.asarray(inp["w_in"])[0]
    cols = []
    for h in range(8):
        for base in (512, 1536, 2560, 3584):
            cols.append(np.arange(base + h * 128, base + (h + 1) * 128))
    cols.append(np.arange(0, 512))
    cols.append(np.arange(4624, 6672))
    cols.append(np.arange(4608, 4624))
    cols = np.concatenate(cols)
    assert cols.shape[0] == IN_COLS
    r = np.arange(128)
    d = {}
    d["w_in_p"] = f(w_in[:, cols])
    d["mnw"] = f(np.asarray(inp["mix_norm_w"])[0].reshape(8, 128).T)
    d["fnw"] = f(np.asarray(inp["ffn_norm_w"])[0].reshape(8, 128).T)
    d["pool_w_p"] = f(np.asarray(inp["pool_w"])[0].transpose(1, 0, 2))
    d["pool_scale_p"] = f(np.asarray(inp["pool_scale"])[0].reshape(4, 128).T)
    d["conv_w_p"] = f(np.asarray(inp["conv_w"])[0].T.reshape(24, 128, 4).transpose(1, 0, 2))
    d["a_log_p"] = f(np.broadcast_to(np.asarray(inp["a_log"])[0][None, :], (128, 8)))
    d["dt_bias_p"] = f(np.broadcast_to(np.asarray(inp["dt_bias"])[0][None, :], (128, 8)))
    d["dn_norm_p"] = f(np.asarray(inp["dn_norm_w"])[0].reshape(128, 1))
    d["w_pool_up"] = f(np.asarray(inp["w_pool_up"])[0])
    d["w_dn_up"] = f(np.asarray(inp["w_dn_up"])[0])
    d["w_mix_out"] = f(np.asarray(inp["w_mix_out"])[0])
    d["peer_w_query"] = f(np.asarray(inp["peer_w_query"])[0])
    k1 = np.asarray(inp["peer_keys_1"])[0]
    k2 = np.asarray(inp["peer_keys_2"])[0]
    kt = np.stack([k1, k2], axis=1).reshape(16, 128, 128)
    d["keys_t"] = f(kt.transpose(2, 0, 1))
    d["peer_down_t"] = f(np.asarray(inp["peer_down"])[0].T)
    d["peer_up"] = f(np.asarray(inp["peer_up"])[0])
    d["final_w_p"] = f(np.broadcast_to(np.asarray(inp["final_norm_w"])[None, :], (128, 1024)))
    d["c_ident"] = f(np.eye(128))
    d["c_tri"] = f(r[:, None] <= r[None, :])
    d["c_negu"] = f(np.where(r[None, :] >= r[:, None], 0.0, NEG))
    d["c_negls"] = f(np.where(r[:, None] > r[None, :], 0.0, NEG))
    d["c_ones"] = f(np.ones((128, 128)))
    pf = np.ones((4, 16), np.float32)
    for g in range(4):
        w = 2 << g
        for t in range(16):
            pf[g, t] = w / min(t + 1, w)
    d["c_poolfix"] = f(np.broadcast_to(pf[None], (128, 4, 16)))
    return d


_NC_CACHE = {}


def kernel(**inputs):
    x = np.asarray(inputs["x"], dtype=np.float32)
    B, S_TOK, _ = x.shape
    if S_TOK not in _NC_CACHE:
        _NC_CACHE[S_TOK] = build(S_TOK)
    nc = _NC_CACHE[S_TOK]
    shared = host_inputs(inputs)
    in_maps = []
    for b in range(B):
        m = dict(shared)
        m["x"] = np.ascontiguousarray(x[b])
        in_maps.append(m)
    res = run_bass_kernel_spmd(nc, in_maps, core_ids=list(range(B)))
    return np.stack([np.asarray(r["y"]) for r in res.results], axis=0).astype(np.float32)
```

```python
from contextlib import ExitStack
import numpy as np
import concourse.bass as bass
import concourse.mybir as mybir
from concourse.bass_utils import run_bass_kernel_spmd

F32 = mybir.dt.float32
BF16 = mybir.dt.bfloat16
AF = mybir.ActivationFunctionType
ALU = mybir.AluOpType

NEG = -30000.0
EPS = 1e-6
IN_COLS = 6672
NE = 16384


class Buf:
    def __init__(self):
        self.last_w = None
        self.readers = []


class Tn:
    def __init__(self, t, b=None, psum=False):
        self.t = t
        self.b = b if b is not None else Buf()
        self.psum = psum


class Sched:
    CE = ("pe", "act", "dve", "pool")

    def __init__(self, nc, ctx, ndma=8, nsets=2):
        self.nc = nc
        self.eng = {}
        self.sets = [{} for _ in range(nsets)]
        for nm, obj in (("pe", nc.tensor), ("act", nc.scalar), ("dve", nc.vector),
                        ("pool", nc.gpsimd), ("sp", nc.sync)):
            for k in range(nsets):
                if nm != "sp":
                    self.sets[k][nm] = ctx.enter_context(nc.semaphore(f"s{k}_" + nm))
            self.eng[nm] = dict(name=nm, obj=obj, sem=self.sets[0].get(nm), count=0, waited={})
        self.epoch = 0
        self.dq = {}
        for q in ("sp", "pool"):
            sems = [ctx.enter_context(nc.semaphore(f"d_{q}{i}")) for i in range(ndma)]
            self.dq[q] = dict(sems=sems, n=0)
        self.ndma = ndma
        self.ninst = 0
        self.hook = None
        self.nosw = 0

    def _wait(self, e, tok):
        sem, val, ep = tok
        if ep is not None and ep < self.epoch:
            return
        if e["name"] == "pe" and sem is e["sem"]:
            return
        key = id(sem)
        w = e["waited"]
        if w.get(key, 0) >= val:
            return
        e["obj"].wait_ge(sem, val)
        w[key] = val
        self.ninst += 1

    def _deps(self, e, reads, writes):
        for b in reads:
            if b.last_w is not None:
                self._wait(e, b.last_w)
        for b in writes:
            if b.last_w is not None:
                self._wait(e, b.last_w)
            for r in b.readers:
                self._wait(e, r)

    def _commit(self, tok, reads, writes):
        for b in reads:
            b.readers = [r for r in b.readers if r[0] is not tok[0]] + [tok]
        for b in writes:
            b.last_w = tok
            b.readers = []

    def op(self, en, fn, reads=(), writes=()):
        e = self.eng[en]
        rb = [x.b for x in reads]
        wb = [x.b for x in writes] + [x.b for x in reads if x.psum]
        self._deps(e, rb, wb)
        inst = fn(e["obj"])
        e["count"] += 1
        inst.then_inc(e["sem"], 1)
        tok = (e["sem"], e["count"], self.epoch)
        self._commit(tok, rb, wb)
        self.ninst += 1
        if self.hook is not None:
            self.hook()
        return tok

    def dma(self, q, out, in_, reads=(), writes=()):
        e = self.eng[q]
        d = self.dq[q]
        n = d["n"]
        sem = d["sems"][n % self.ndma]
        if n >= self.ndma:
            self._wait(e, (sem, 16 * (n // self.ndma), None))
        rb = [x.b for x in reads]
        wb = [x.b for x in writes]
        self._deps(e, rb, wb)
        e["obj"].dma_start(out=out, in_=in_).then_inc(sem, 16)
        d["n"] = n + 1
        tok = (sem, 16 * (n // self.ndma + 1), None)
        self._commit(tok, rb, wb)
        self.ninst += 1
        return tok

    def run_interleaved(self, fns):
        import threading
        n = len(fns)
        go = [threading.Semaphore(0) for _ in range(n)]
        main = threading.Semaphore(0)
        done = [False] * n
        errs = []
        cur = [0]

        def nxt(i):
            for j in range(i + 1, i + 1 + n):
                if not done[j % n]:
                    return j % n
            return None

        def hook():
            if self.nosw:
                return
            i = cur[0]
            j = nxt(i)
            if j is None or j == i:
                return
            cur[0] = j
            go[j].release()
            go[i].acquire()

        def worker(i):
            go[i].acquire()
            try:
                fns[i]()
            except BaseException as ex:
                errs.append(ex)
            done[i] = True
            j = nxt(i)
            if j is None:
                main.release()
            else:
                cur[0] = j
                go[j].release()

        ths = [threading.Thread(target=worker, args=(i,)) for i in range(n)]
        for t in ths:
            t.start()
        old = self.hook
        self.hook = hook
        cur[0] = 0
        go[0].release()
        main.acquire()
        for t in ths:
            t.join()
        self.hook = old
        if errs:
            raise errs[0]

    def _sync_all(self):
        for a in self.CE + ("sp",):
            for b in self.CE:
                if a != b and self.eng[b]["count"] > 0:
                    self._wait(self.eng[a], (self.eng[b]["sem"], self.eng[b]["count"], self.epoch))

    def barrier(self, switch=False):
        self._sync_all()
        if not switch:
            return
        self.epoch += 1
        new = self.sets[self.epoch]
        for nm, e in self.eng.items():
            e["sem"] = new.get(nm)
            e["count"] = 0

    def finish(self):
        for q, d in self.dq.items():
            e = self.eng[q]
            for i, sem in enumerate(d["sems"]):
                cnt = (d["n"] - i + self.ndma - 1) // self.ndma
                if cnt > 0:
                    e["obj"].wait_ge(sem, 16 * cnt)


def build(S_TOK, dbg=False, stage=99, sw_every=2):
    NST = S_TOK // 256
    nc = bass.Bass("TRN2", target_bir_lowering=False)

    def din(name, shape):
        return nc.dram_tensor(name, list(shape), F32, kind="ExternalInput").ap()

    x_d = din("x", [S_TOK, 1024])
    win_d = din("w_in_p", [1024, IN_COLS])
    mnw_d = din("mnw", [128, 8])
    fnw_d = din("fnw", [128, 8])
    pw_d = din("pool_w_p", [128, 4, 128])
    psc_d = din("pool_scale_p", [128, 4])
    cw_d = din("conv_w_p", [128, 24, 4])
    alog_d = din("a_log_p", [128, 8])
    dtb_d = din("dt_bias_p", [128, 8])
    dnw_d = din("dn_norm_p", [128, 1])
    wpu_d = din("w_pool_up", [512, 1024])
    wdu_d = din("w_dn_up", [1024, 1024])
    wo_d = din("w_mix_out", [1024, 1024])
    wq_d = din("peer_w_query", [1024, 2048])
    kt_d = din("keys_t", [128, 16, 128])
    dnT_d = din("peer_down_t", [1024, NE])
    up_d = din("peer_up", [NE, 1024])
    fnl_d = din("final_w_p", [128, 1024])
    ident_d = din("c_ident", [128, 128])
    tri_d = din("c_tri", [128, 128])
    negu_d = din("c_negu", [128, 128])
    negls_d = din("c_negls", [128, 128])
    ones_d = din("c_ones", [128, 128])
    pfix_d = din("c_poolfix", [128, 4, 16])
    y_d = nc.dram_tensor("y", [S_TOK, 1024], F32, kind="ExternalOutput").ap()
    if dbg:
        h1_d = nc.dram_tensor("h1dbg", [S_TOK, 1024], F32, kind="ExternalOutput").ap()

    win_s = Tn(nc.dram_tensor("win_s", [128, 8, IN_COLS], BF16).ap())
    wq_s = Tn(nc.dram_tensor("wq_s", [128, 8, 2048], BF16).ap())
    dn_s = Tn(nc.dram_tensor("dn_s", [128, 8, NE], BF16).ap())
    up_s = Tn(nc.dram_tensor("up_s", [128, 128, 1024], BF16).ap())

    with ExitStack() as ctx:
        NEP = (NST + sw_every - 1) // sw_every
        S = Sched(nc, ctx, nsets=NEP + 1)

        uid = [0]

        def un_(name):
            uid[0] += 1
            return f"t{uid[0]}_{name}"

        def sb(cx, name, shape, dt=F32):
            return Tn(cx.enter_context(nc.sbuf_tensor(un_(name), list(shape), dt)))

        def ps(cx, name, shape, dt=F32):
            return Tn(cx.enter_context(nc.psum_tensor(un_(name), list(shape), dt)), psum=True)

        def mm(o, o_ap, l, l_ap, r, r_ap, start=True, stop=True):
            S.op("pe", lambda e: e.matmul(o_ap, lhsT=l_ap, rhs=r_ap, start=start, stop=stop),
                 reads=[l, r], writes=[o])

        def tr(o, o_ap, i, i_ap, idn):
            S.op("pe", lambda e: e.transpose(out=o_ap, in_=i_ap, identity=idn.t[:]),
                 reads=[i, idn], writes=[o])

        def act(o, o_ap, i, i_ap, func, reads=(), wr=(), **kw):
            S.op("act", lambda e: e.activation(out=o_ap, in_=i_ap, func=func, **kw),
                 reads=[i] + list(reads), writes=[o] + list(wr))

        def ts(en, o, o_ap, i, i_ap, s1, s2, op0, op1=None, reads=()):
            if op1 is None:
                S.op(en, lambda e: e.tensor_scalar(out=o_ap, in0=i_ap, scalar1=s1, scalar2=None, op0=op0),
                     reads=[i] + list(reads), writes=[o])
            else:
                S.op(en, lambda e: e.tensor_scalar(out=o_ap, in0=i_ap, scalar1=s1, scalar2=s2, op0=op0, op1=op1),
                     reads=[i] + list(reads), writes=[o])

        def tt(en, o, o_ap, a, a_ap, b, b_ap, op):
            S.op(en, lambda e: e.tensor_tensor(out=o_ap, in0=a_ap, in1=b_ap, op=op),
                 reads=[a, b], writes=[o])

        def stt(en, o, o_ap, a, a_ap, sc, b, b_ap, op0, op1, reads=()):
            S.op(en, lambda e: e.scalar_tensor_tensor(out=o_ap, in0=a_ap, scalar=sc, in1=b_ap, op0=op0, op1=op1),
                 reads=[a, b] + list(reads), writes=[o])

        def cp(en, o, o_ap, i, i_ap):
            if en == "act":
                S.op("act", lambda e: e.copy(out=o_ap, in_=i_ap), reads=[i], writes=[o])
            else:
                S.op(en, lambda e: e.tensor_copy(out=o_ap, in_=i_ap), reads=[i], writes=[o])

        def rsqrt_col(o, i, scale, eps, reads=()):
            ts("dve", o, o.t[:], i, i.t[:], scale, eps, ALU.mult, ALU.add)
            act(o, o.t[:], o, o.t[:], AF.Sqrt)
            S.op("dve", lambda e: e.reciprocal(out=o.t[:], in_=o.t[:]), reads=[o], writes=[o])

        ident_f = sb(ctx, "ident_f", [128, 128])
        ident_b = sb(ctx, "ident_b", [128, 128], BF16)
        tri = sb(ctx, "tri", [128, 128])
        negu = sb(ctx, "negu", [128, 128])
        negls = sb(ctx, "negls", [128, 128])
        ones = sb(ctx, "ones", [128, 128])
        pfix = sb(ctx, "pfix", [128, 4, 16])
        mnw = sb(ctx, "mnw", [128, 8])
        fnw = sb(ctx, "fnw", [128, 8])
        psc = sb(ctx, "psc", [128, 4])
        cw = sb(ctx, "cw", [128, 24, 4])
        nA = sb(ctx, "nA", [128, 8])
        dtb = sb(ctx, "dtb", [128, 8])
        dnw = sb(ctx, "dnw", [128, 1])
        fnl = sb(ctx, "fnl", [128, 1024])
        wpu = sb(ctx, "wpu", [128, 4, 1024], BF16)
        wdu = sb(ctx, "wdu", [128, 8, 1024], BF16)
        wo = sb(ctx, "wo", [128, 8, 1024], BF16)
        pw = sb(ctx, "pw", [128, 4, 128], BF16)
        wsm = sb(ctx, "wsm", [128, 8, 16], BF16)
        KT = sb(ctx, "KT", [128, 16, 128], BF16)
        wpool = [sb(ctx, f"wpool{i}", [128, 4096], BF16) for i in range(5)]
        wp_n = [0]
        Sst = [sb(ctx, f"Sst{h}", [128, 128]) for h in range(8)]
        ccar = [sb(ctx, f"ccar{b}", [128, 3]) for b in range(24)]
        xab = [sb(ctx, f"xab{g}", [128, 272]) for g in range(4)]
        xh = [sb(ctx, f"xh{i}", [128, 1024]) for i in range(2)]
        yo_p = [sb(ctx, f"yo{i}", [128, 1024]) for i in range(2)]

        def wload(src, src_ap, view):
            t = wpool[wp_n[0] % len(wpool)]
            wp_n[0] += 1
            S.dma("sp", view(t.t), src_ap, reads=[src], writes=[t])
            return t

        v8 = lambda t: t[:].rearrange("p (c n) -> p c n", c=8)
        v4 = lambda t: t[:].rearrange("p (c n) -> p c n", c=4)

        dummy = Tn(None)
        for (t, d) in ((ident_f, ident_d), (tri, tri_d), (negu, negu_d), (negls, negls_d), (ones, ones_d),
                       (pfix, pfix_d), (mnw, mnw_d), (fnw, fnw_d), (psc, psc_d), (cw, cw_d), (nA, alog_d),
                       (dtb, dtb_d), (dnw, dnw_d), (fnl, fnl_d)):
            S.dma("sp", t.t[:], d, writes=[t])
        cp("dve", ident_b, ident_b.t[:], ident_f, ident_f.t[:])
        act(nA, nA.t[:], nA, nA.t[:], AF.Exp)
        ts("dve", nA, nA.t[:], nA, nA.t[:], -1.0, None, ALU.mult)
        for h in range(8):
            S.op("dve", lambda e: e.memset(Sst[h].t[:], 0.0), writes=[Sst[h]])
        for b in range(24):
            S.op("pool", lambda e: e.memset(ccar[b].t[:], 0.0), writes=[ccar[b]])
        for g in range(4):
            S.op("pool", lambda e: e.memset(xab[g].t[:], 0.0), writes=[xab[g]])

        with ExitStack() as pc:
            stg = [sb(pc, f"stg{i}", [128, 4096]) for i in range(2)]
            sn = [0]

            def prep(src_ap, shape3, dst, dst_ap, scale_t=None, scale_ap=None):
                a, b = shape3
                st_ = stg[sn[0] % 2]
                sn[0] += 1
                sv = st_.t[:, 0:a * b].rearrange("p (a b) -> p a b", a=a)
                S.dma("sp", sv, src_ap, writes=[st_])
                ob = wpool[wp_n[0] % len(wpool)]
                wp_n[0] += 1
                ov = ob.t[:, 0:a * b].rearrange("p (a b) -> p a b", a=a)
                en = "dve" if sn[0] % 2 == 0 else "pool"
                if scale_t is None:
                    cp("act" if sn[0] % 2 == 0 else "dve", ob, ov, st_, sv)
                else:
                    tt(en, ob, ov, st_, sv, scale_t, scale_ap, ALU.mult)
                if dst is None:
                    return ob, ov
                S.dma("pool", dst_ap, ov, reads=[ob], writes=[dst])
                return ob, ov

            for (wt, d, a, b, rs) in ((wpu, wpu_d, 4, 1024, "(g p) n -> p g n"),
                                      (wo, wo_d, 8, 1024, "(c p) n -> p c n")):
                for half in range(a // 4):
                    ob, ov = prep(d.rearrange(rs, p=128)[:, half * 4:(half + 1) * 4, :], (4, 1024), None, None)
                    cp("dve", wt, wt.t[:, half * 4:(half + 1) * 4, :], ob, ov)
            for half in range(2):
                ob, ov = prep(wdu_d.rearrange("(h p) n -> p h n", p=128)[:, half * 4:(half + 1) * 4, :], (4, 1024),
                              None, None, dnw, dnw.t[:, 0:1].unsqueeze(2).to_broadcast([128, 4, 1024]))
                cp("dve", wdu, wdu.t[:, half * 4:(half + 1) * 4, :], ob, ov)
            ob, ov = prep(pw_d, (4, 128), None, None)
            cp("dve", pw, pw.t[:], ob, ov)
            ob, ov = prep(kt_d, (16, 128), None, None)
            cp("dve", KT, KT.t[:], ob, ov)
            winv = win_d.rearrange("(c p) n -> p c n", p=128)
            for blk in range(14):
                c0 = blk * 512
                n = min(512, IN_COLS - c0)
                ob, ov = prep(winv[:, :, c0:c0 + n], (8, n), win_s, win_s.t[:, :, c0:c0 + n],
                              mnw, mnw.t[:].unsqueeze(2).to_broadcast([128, 8, n]))
                if blk == 13:
                    cp("dve", wsm, wsm.t[:], ob, ov)
            wqv = wq_d.rearrange("(c p) n -> p c n", p=128)
            for blk in range(4):
                prep(wqv[:, :, blk * 512:(blk + 1) * 512], (8, 512), wq_s, wq_s.t[:, :, blk * 512:(blk + 1) * 512],
                     fnw, fnw.t[:].unsqueeze(2).to_broadcast([128, 8, 512]))
            dnv = dnT_d.rearrange("(c p) e -> p c e", p=128)
            upv = up_d.rearrange("(ch p) d -> p ch d", p=128)
            for blk in range(32 if stage >= 2 else 0):
                prep(dnv[:, :, blk * 512:(blk + 1) * 512], (8, 512), dn_s, dn_s.t[:, :, blk * 512:(blk + 1) * 512],
                     fnw, fnw.t[:].unsqueeze(2).to_broadcast([128, 8, 512]))
                prep(upv[:, blk * 4:(blk + 1) * 4, :], (4, 1024), up_s, up_s.t[:, blk * 4:(blk + 1) * 4, :])
        S.barrier(switch=True)

        def mixer(st):
            t0 = st * 256
            with ExitStack() as mx:
                pTb = ps(mx, "pTb", [128, 1024], BF16)
                big = [ps(mx, f"big{k}", [128, 512]) for k in range(4)]
                bn = [0]
                qbank = [mx.enter_context(nc.psum_tensor(un_(f"qb{k}"), [128, 512], F32)) for k in range(3)]
                qsl = [Tn(qbank[k], psum=True) for k in range(3)]
                qn = [0]

                def nbig():
                    b = big[bn[0] % 4]
                    bn[0] += 1
                    return b

                def nq():
                    k = qn[0] % 12
                    qn[0] += 1
                    s = qsl[k % 3]
                    qq = k // 3
                    return s, s.t[:, qq * 128:(qq + 1) * 128]

                sq = sb(mx, "sq", [128, 1024])
                xsb = sb(mx, "xsb", [128, 1024], BF16)
                xT = sb(mx, "xT", [128, 8, 256], BF16)
                ss = sb(mx, "ss", [128, 1])
                rstd = sb(mx, "rstd", [128, 1])
                def mk_slot():
                    d_ = {}
                    d_["raw"] = [sb(mx, f"raw{k}", [128, 259]) for k in range(3)]
                    d_["cacc"] = sb(mx, "cacc", [128, 256])
                    d_["qkvT"] = [sb(mx, f"qkvT{k}", [128, 256]) for k in range(3)]
                    d_["szT"] = sb(mx, "szT", [128, 256])
                    d_["sqn"] = sb(mx, "sqn", [128, 256])
                    d_["rn"] = sb(mx, "rn", [128, 256])
                    d_["kbg"] = sb(mx, "kbg", [128, 128])
                    d_["kdec"] = sb(mx, "kdec", [128, 128])
                    d_["vb"] = sb(mx, "vb", [128, 128])
                    d_["trig"] = sb(mx, "trig", [128, 128])
                    d_["Am"] = sb(mx, "Am", [128, 128])
                    d_["EU"] = sb(mx, "EU", [128, 128])
                    d_["DLs"] = sb(mx, "DLs", [128, 128])
                    d_["egrow"] = sb(mx, "egrow", [128, 128])
                    d_["Mb"] = [sb(mx, f"Mb{k}", [128, 128]) for k in range(2)]
                    d_["MTb"] = [sb(mx, f"MTb{k}", [128, 128]) for k in range(2)]
                    d_["PTb"] = [sb(mx, f"PTb{k}", [128, 128]) for k in range(2)]
                    d_["attnT"] = sb(mx, "attnT", [128, 128])
                    d_["qgT"] = sb(mx, "qgT", [128, 128])
                    d_["nwT"] = sb(mx, "nwT", [128, 128])
                    d_["vn"] = sb(mx, "vn", [128, 128])
                    d_["oss"] = sb(mx, "oss", [128, 1])
                    d_["orst"] = sb(mx, "orst", [128, 1])
                    d_["osq"] = sb(mx, "osq", [128, 128])
                    d_["onb"] = sb(mx, "onb", [128, 128], BF16)
                    return d_
                SL = [mk_slot(), mk_slot()]
                lg = [sb(mx, f"lg{i}", [128, 16]) for i in range(2)]
                beta = [sb(mx, f"beta{i}", [128, 8]) for i in range(2)]
                nbeta = [sb(mx, f"nbeta{i}", [128, 8]) for i in range(2)]
                gg = [sb(mx, f"gg{i}", [128, 8]) for i in range(2)]
                gc = [sb(mx, f"gc{i}", [128, 8]) for i in range(2)]
                egl = [sb(mx, f"egl{i}", [128, 8]) for i in range(2)]
                kds = [sb(mx, f"kds{i}", [128, 8]) for i in range(2)]
                bgs = [sb(mx, f"bgs{i}", [128, 8]) for i in range(2)]
                onT = sb(mx, "onT", [128, 8, 256], BF16)
                pa = [sb(mx, f"pa{k}", [128, 272]) for k in range(2)]
                pooledb = sb(mx, "pooledb", [128, 256], BF16)
                paT = sb(mx, "paT", [128, 4, 256], BF16)
                sga = sb(mx, "sga", [128, 256])
                m1 = sb(mx, "m1", [128, 256])
                sgb = sb(mx, "sgb", [128, 256])
                m2 = sb(mx, "m2", [128, 256])
                mT = sb(mx, "mT", [128, 8, 256], BF16)

                for i in range(2):
                    S.dma("sp", xh[i].t[:], x_d[t0 + i * 128:t0 + (i + 1) * 128, :], writes=[xh[i]])
                    act(sq, sq.t[:], xh[i], xh[i].t[:], AF.Square, wr=[ss], accum_out=ss.t[:])
                    rsqrt_col(rstd, ss, 1.0 / 1024, EPS)
                    ts("dve", xsb, xsb.t[:], xh[i], xh[i].t[:], rstd.t[:, 0:1], None, ALU.mult, reads=[rstd])
                    for c in range(8):
                        tr(pTb, pTb.t[:, c * 128:(c + 1) * 128], xsb, xsb.t[:, c * 128:(c + 1) * 128], ident_b)
                    cp("act", xT, xT.t[:, :, i * 128:(i + 1) * 128], pTb,
                       pTb.t[:].rearrange("p (c n) -> p c n", c=8))

                for i in range(2):
                    s_, s_ap = nq()
                    for c in range(8):
                        mm(s_, s_ap[:, 0:16], xT, xT.t[:, c, i * 128:(i + 1) * 128], wsm, wsm.t[:, c, :],
                           start=(c == 0), stop=(c == 7))
                    cp("dve", lg[i], lg[i].t[:], s_, s_ap[:, 0:16])
                    act(beta[i], beta[i].t[:], lg[i], lg[i].t[:, 0:8], AF.Sigmoid)
                    ts("dve", nbeta[i], nbeta[i].t[:], beta[i], beta[i].t[:], -1.0, None, ALU.mult)
                    tt("dve", gg[i], gg[i].t[:], lg[i], lg[i].t[:, 8:16], dtb, dtb.t[:], ALU.add)
                    act(gg[i], gg[i].t[:], gg[i], gg[i].t[:], AF.Exp)
                    act(gg[i], gg[i].t[:], gg[i], gg[i].t[:], AF.Ln, bias=1.0)
                    tt("dve", gg[i], gg[i].t[:], gg[i], gg[i].t[:], nA, nA.t[:], ALU.mult)
                    s_, s_ap = nq()
                    mm(s_, s_ap[:, 0:8], tri, tri.t[:], gg[i], gg[i].t[:])
                    cp("dve", gc[i], gc[i].t[:], s_, s_ap[:, 0:8])
                    s_, s_ap = nq()
                    mm(s_, s_ap[:, 0:8], ones, ones.t[:], gg[i], gg[i].t[:])
                    tt("dve", kds[i], kds[i].t[:], s_, s_ap[:, 0:8], gc[i], gc[i].t[:], ALU.subtract)
                    act(kds[i], kds[i].t[:], kds[i], kds[i].t[:], AF.Exp)
                    act(egl[i], egl[i].t[:], s_, s_ap[:, 0:8], AF.Exp)
                    act(bgs[i], bgs[i].t[:], gc[i], gc[i].t[:], AF.Exp)
                    tt("dve", bgs[i], bgs[i].t[:], bgs[i], bgs[i].t[:], beta[i], beta[i].t[:], ALU.mult)

                def head_body(h, slot):
                    d_ = SL[slot]
                    raw = d_["raw"]
                    cacc = d_["cacc"]
                    qkvT = d_["qkvT"]
                    szT = d_["szT"]
                    sqn = d_["sqn"]
                    rn = d_["rn"]
                    kbg = d_["kbg"]
                    kdec = d_["kdec"]
                    vb = d_["vb"]
                    trig = d_["trig"]
                    Am = d_["Am"]
                    EU = d_["EU"]
                    DLs = d_["DLs"]
                    egrow = d_["egrow"]
                    Mb = d_["Mb"]
                    MTb = d_["MTb"]
                    PTb = d_["PTb"]
                    attnT = d_["attnT"]
                    qgT = d_["qgT"]
                    nwT = d_["nwT"]
                    vn = d_["vn"]
                    oss = d_["oss"]
                    orst = d_["orst"]
                    osq = d_["osq"]
                    onb = d_["onb"]
                    wb = wload(win_s, win_s.t[:, :, h * 512:(h + 1) * 512], v8)
                    wv = v8(wb.t)
                    for k in range(4):
                        pb = nbig()
                        for c in range(8):
                            mm(pb, pb.t[:, 0:256], wb, wv[:, c, k * 128:(k + 1) * 128], xT, xT.t[:, c, :],
                               start=(c == 0), stop=(c == 7))
                        if k == 3:
                            act(szT, szT.t[:], pb, pb.t[:, 0:256], AF.Silu)
                            continue
                        blk = k * 8 + h
                        r = raw[k]
                        cp("pool", r, r.t[:, 0:3], ccar[blk], ccar[blk].t[:])
                        cp("act", r, r.t[:, 3:259], pb, pb.t[:, 0:256])
                        cp("pool", ccar[blk], ccar[blk].t[:], r, r.t[:, 256:259])
                        ts("dve", cacc, cacc.t[:], r, r.t[:, 0:256], cw.t[:, blk, 0:1], None, ALU.mult, reads=[cw])
                        for j in range(1, 4):
                            stt("dve", cacc, cacc.t[:], r, r.t[:, j:j + 256], cw.t[:, blk, j:j + 1], cacc, cacc.t[:],
                                ALU.mult, ALU.add, reads=[cw])
                        act(qkvT[k], qkvT[k].t[:], cacc, cacc.t[:], AF.Silu)
                        if k < 2:
                            tt("dve", sqn, sqn.t[:], qkvT[k], qkvT[k].t[:], qkvT[k], qkvT[k].t[:], ALU.mult)
                            pn = nbig()
                            mm(pn, pn.t[:, 0:256], ones, ones.t[:], sqn, sqn.t[:])
                            ts("dve", rn, rn.t[:], pn, pn.t[:, 0:256], EPS, None, ALU.add)
                            act(rn, rn.t[:], rn, rn.t[:], AF.Sqrt)
                            S.op("dve", lambda e: e.reciprocal(out=rn.t[:], in_=rn.t[:]), reads=[rn], writes=[rn])
                            sc = (128.0 ** -0.5) if k == 0 else 1.0
                            stt("dve", qkvT[k], qkvT[k].t[:], qkvT[k], qkvT[k].t[:], sc, rn, rn.t[:],
                                ALU.mult, ALU.mult)
                    qT_, kT_, vT_ = qkvT
                    for i in range(2):
                        sl = slice(i * 128, (i + 1) * 128)
                        hh = slice(h, h + 1)
                        k_s, k_ap = nq()
                        tr(k_s, k_ap, kT_, kT_.t[:, sl], ident_f)
                        v_s, v_ap = nq()
                        tr(v_s, v_ap, vT_, vT_.t[:, sl], ident_f)
                        g_s, g_ap = nq()
                        mm(g_s, g_ap, kT_, kT_.t[:, sl], kT_, kT_.t[:, sl])
                        a_s, a_ap = nq()
                        mm(a_s, a_ap, kT_, kT_.t[:, sl], qT_, qT_.t[:, sl])
                        ts("dve", kbg, kbg.t[:], k_s, k_ap, bgs[i].t[:, hh], None, ALU.mult, reads=[bgs[i]])
                        act(kdec, kdec.t[:], k_s, k_ap, AF.Copy, reads=[kds[i]], scale=kds[i].t[:, hh])
                        act(vb, vb.t[:], v_s, v_ap, AF.Copy, reads=[beta[i]], scale=beta[i].t[:, hh])
                        ts("pool", trig, trig.t[:], tri, tri.t[:], gg[i].t[:, hh], None, ALU.mult, reads=[gg[i]])
                        r_s, r_ap = nq()
                        mm(r_s, r_ap, ones, ones.t[:], trig, trig.t[:])
                        ts("dve", Am, Am.t[:], r_s, r_ap, gc[i].t[:, hh], None, ALU.subtract, reads=[gc[i]])
                        act(egrow, egrow.t[:], r_s, r_ap, AF.Exp)
                        tt("pool", EU, EU.t[:], Am, Am.t[:], negu, negu.t[:], ALU.add)
                        act(EU, EU.t[:], EU, EU.t[:], AF.Exp)
                        tt("pool", DLs, DLs.t[:], negls, negls.t[:], Am, Am.t[:], ALU.subtract)
                        act(DLs, DLs.t[:], DLs, DLs.t[:], AF.Exp)
                        M, MT, PT = Mb[0], MTb[0], PTb[0]
                        stt("dve", M, M.t[:], g_s, g_ap, nbeta[i].t[:, hh], DLs, DLs.t[:], ALU.mult, ALU.mult,
                            reads=[nbeta[i]])
                        tt("dve", attnT, attnT.t[:], a_s, a_ap, EU, EU.t[:], ALU.mult)
                        tt("pool", qgT, qgT.t[:], qT_, qT_.t[:, sl], egrow, egrow.t[:], ALU.mult)
                        t_s, t_ap = nq()
                        tr(t_s, t_ap, M, M.t[:], ident_f)
                        cp("act", MT, MT.t[:], t_s, t_ap)
                        tt("dve", PT, PT.t[:], t_s, t_ap, ident_f, ident_f.t[:], ALU.add)
                        for kk in range(6):
                            Mn, MTn, PTn = Mb[(kk + 1) % 2], MTb[(kk + 1) % 2], PTb[(kk + 1) % 2]
                            s1, s1ap = nq()
                            mm(s1, s1ap, MT, MT.t[:], M, M.t[:])
                            cp("act", Mn, Mn.t[:], s1, s1ap)
                            if kk < 5:
                                s2, s2ap = nq()
                                mm(s2, s2ap, M, M.t[:], MT, MT.t[:])
                                cp("dve", MTn, MTn.t[:], s2, s2ap)
                            s3, s3ap = nq()
                            mm(s3, s3ap, Mn, Mn.t[:], PT, PT.t[:])
                            tt("dve", PTn, PTn.t[:], s3, s3ap, PT, PT.t[:], ALU.add)
                            M, MT, PT = Mn, MTn, PTn
                        w_s, w_ap = nq()
                        mm(w_s, w_ap, kbg, kbg.t[:], PT, PT.t[:])
                        S.op("act", lambda e: e.mul(out=nwT.t[:], in_=w_ap, mul=-1.0), reads=[w_s], writes=[nwT])
                        n_s, n_ap = nq()
                        S.nosw += 1
                        mm(n_s, n_ap, PT, PT.t[:], vb, vb.t[:], start=True, stop=False)
                        S.nosw -= 1
                        mm(n_s, n_ap, nwT, nwT.t[:], Sst[h], Sst[h].t[:], start=False, stop=True)
                        cp("dve", vn, vn.t[:], n_s, n_ap)
                        o_s, o_ap = nq()
                        S.nosw += 1
                        mm(o_s, o_ap, qgT, qgT.t[:], Sst[h], Sst[h].t[:], start=True, stop=False)
                        S.nosw -= 1
                        mm(o_s, o_ap, attnT, attnT.t[:], vn, vn.t[:], start=False, stop=True)
                        u_s, u_ap = nq()
                        mm(u_s, u_ap, kdec, kdec.t[:], vn, vn.t[:])
                        stt("dve", Sst[h], Sst[h].t[:], Sst[h], Sst[h].t[:], egl[i].t[:, hh], u_s, u_ap,
                            ALU.mult, ALU.add, reads=[egl[i]])
                        act(osq, osq.t[:], o_s, o_ap, AF.Square, wr=[oss], accum_out=oss.t[:])
                        rsqrt_col(orst, oss, 1.0 / 128, EPS)
                        act(onb, onb.t[:], o_s, o_ap, AF.Copy, reads=[orst], scale=orst.t[:, 0:1])
                        pslot = pTb.t[:, (h % 8) * 128:(h % 8 + 1) * 128]
                        tr(pTb, pslot, onb, onb.t[:], ident_b)
                        tt("dve", onT, onT.t[:, h, sl], pTb, pslot, szT, szT.t[:, sl], ALU.mult)

                for hp in range(0, 8, 2):
                    S.run_interleaved([lambda hp=hp: head_body(hp, 0), lambda hp=hp: head_body(hp + 1, 1)])

                wb = wload(win_s, win_s.t[:, :, 4096:4608], v8)
                wv = v8(wb.t)
                for g in range(4):
                    win_ = 2 << g
                    pb = nbig()
                    for c in range(8):
                        mm(pb, pb.t[:, 0:256], wb, wv[:, c, g * 128:(g + 1) * 128], xT, xT.t[:, c, :],
                           start=(c == 0), stop=(c == 7))
                    xg = xab[g]
                    cp("pool", xg, xg.t[:, 0:16], xg, xg.t[:, 256:272])
                    cp("act", xg, xg.t[:, 16:272], pb, pb.t[:, 0:256])
                    src = xg
                    sh = 1
                    for stp in range(g + 1):
                        dst = pa[stp % 2]
                        lo = 2 * sh - 1
                        tt("dve", dst, dst.t[:, lo:272], src, src.t[:, lo:272], src, src.t[:, lo - sh:272 - sh], ALU.add)
                        src = dst
                        sh *= 2
                    if st == 0:
                        tt("dve", src, src.t[:, 16:32], src, src.t[:, 16:32], pfix, pfix.t[:, g, :], ALU.mult)
                    stt("dve", pooledb, pooledb.t[:], src, src.t[:, 16:272], 1.0 / win_, xg, xg.t[:, 16:272],
                        ALU.mult, ALU.subtract)
                    pb2 = nbig()
                    mm(pb2, pb2.t[:, 0:256], pw, pw.t[:, g, :], pooledb, pooledb.t[:])
                    act(paT, paT.t[:, g, :], pb2, pb2.t[:, 0:256], AF.Copy, reads=[psc], scale=psc.t[:, g:g + 1])

                wga = [wload(win_s, win_s.t[:, :, 4608 + k * 512:4608 + (k + 1) * 512], v8) for k in range(2)]
                wgb = [wload(win_s, win_s.t[:, :, 5632 + k * 512:5632 + (k + 1) * 512], v8) for k in range(2)]
                for j in range(8):
                    js = slice(j * 128, (j + 1) * 128)
                    jw = slice((j % 4) * 128, (j % 4 + 1) * 128)
                    pA, pB, pC, pD = big[0], big[1], big[2], big[3]
                    for g in range(4):
                        mm(pA, pA.t[:, 0:256], wpu, wpu.t[:, g, js], paT, paT.t[:, g, :], start=(g == 0), stop=(g == 3))
                    wa = wga[j // 4]
                    for c in range(8):
                        mm(pB, pB.t[:, 0:256], wa, v8(wa.t)[:, c, jw], xT, xT.t[:, c, :], start=(c == 0), stop=(c == 7))
                    for hd in range(8):
                        mm(pC, pC.t[:, 0:256], wdu, wdu.t[:, hd, js], onT, onT.t[:, hd, :], start=(hd == 0), stop=(hd == 7))
                    wg = wgb[j // 4]
                    for c in range(8):
                        mm(pD, pD.t[:, 0:256], wg, v8(wg.t)[:, c, jw], xT, xT.t[:, c, :], start=(c == 0), stop=(c == 7))
                    act(sga, sga.t[:], pB, pB.t[:, 0:256], AF.Sigmoid)
                    tt("dve", m1, m1.t[:], pA, pA.t[:, 0:256], sga, sga.t[:], ALU.mult)
                    act(sgb, sgb.t[:], pD, pD.t[:, 0:256], AF.Sigmoid)
                    tt("dve", m2, m2.t[:], pC, pC.t[:, 0:256], sgb, sgb.t[:], ALU.mult)
                    tt("pool", mT, mT.t[:, j, :], m1, m1.t[:], m2, m2.t[:], ALU.add)

                for i in range(2):
                    for n in range(2):
                        pb = nbig()
                        for k in range(8):
                            mm(pb, pb.t[:], mT, mT.t[:, k, i * 128:(i + 1) * 128], wo, wo.t[:, k, n * 512:(n + 1) * 512],
                               start=(k == 0), stop=(k == 7))
                        tt("dve", xh[i], xh[i].t[:, n * 512:(n + 1) * 512], pb, pb.t[:],
                           xh[i], xh[i].t[:, n * 512:(n + 1) * 512], ALU.add)
                    if dbg:
                        S.dma("pool", h1_d[t0 + i * 128:t0 + (i + 1) * 128, :], xh[i].t[:], reads=[xh[i]], writes=[Tn(None)])

        def peer(st):
            t0 = st * 256
            with ExitStack() as px:
                pY = [[ps(px, f"pY{i}{n}", [128, 512]) for n in range(2)] for i in range(2)]
                pU = [ps(px, f"pU{k}", [128, 512]) for k in range(2)]
                pTb = ps(px, "pTbp", [128, 1024], BF16)
                pM = ps(px, "pM", [128, 512])
                hsb = sb(px, "hsb", [128, 1024], BF16)
                xn2T = sb(px, "xn2T", [128, 8, 256], BF16)
                ss = sb(px, "pss", [128, 1])
                rstd = sb(px, "prstd", [128, 1])
                qT = sb(px, "pqT", [128, 16, 256], BF16)
                sc = [sb(px, f"sc{i}", [128, 2048]) for i in range(2)]
                v16 = [sb(px, f"v16{k}", [128, 16]) for k in range(2)]
                tmp128 = sb(px, "tmp128", [128, 128])
                cand = sb(px, "cand", [128, 256])
                cand2 = sb(px, "cand2", [128, 256])
                c16 = sb(px, "c16", [128, 16])
                ex16 = sb(px, "ex16", [128, 16])
                negm = sb(px, "negm", [128, 1])
                zz = sb(px, "zz", [128, 1])
                taum = [sb(px, f"taum{i}", [128, 8]) for i in range(2)]
                ttau = [sb(px, f"ttau{i}", [128, 8]) for i in range(2)]
                cand3 = cand
                c24 = sb(px, "c24", [128, 8])
                tsum = sb(px, "tsum", [128, 1])
                E1n = [sb(px, f"E1n{i}", [128, 8, 128]) for i in range(2)]
                E2 = [sb(px, f"E2{i}", [128, 8, 128]) for i in range(2)]
                gms = [[sb(px, f"gm{u}_{h}", [128, 512], BF16) for h in range(8)] for u in range(2)]
                ncc = [sb(px, f"ncc{i}", [128, 8]) for i in range(2)]
                Ag = [sb(px, f"Ag{k}", [128, 512]) for k in range(2)]
                Tt = [sb(px, f"Tt{k}", [128, 512]) for k in range(4)]
                GA = sb(px, "GA", [128, 512], BF16)
                GAT = [sb(px, f"GAT{k}", [128, 4, 128], BF16) for k in range(2)]
                hf = sb(px, "hf", [128, 1024])

                for i in range(2):
                    act(hf, hf.t[:], xh[i], xh[i].t[:], AF.Square, wr=[ss], accum_out=ss.t[:])
                    rsqrt_col(rstd, ss, 1.0 / 1024, EPS)
                    ts("dve", hsb, hsb.t[:], xh[i], xh[i].t[:], rstd.t[:, 0:1], None, ALU.mult, reads=[rstd])
                    for c in range(8):
                        tr(pTb, pTb.t[:, c * 128:(c + 1) * 128], hsb, hsb.t[:, c * 128:(c + 1) * 128], ident_b)
                    cp("act", xn2T, xn2T.t[:, :, i * 128:(i + 1) * 128], pTb,
                       pTb.t[:].rearrange("p (c n) -> p c n", c=8))
                for blk in range(4):
                    wb = wload(wq_s, wq_s.t[:, :, blk * 512:(blk + 1) * 512], v8)
                    wv = v8(wb.t)
                    for jj in range(4):
                        jb = blk * 4 + jj
                        for c in range(8):
                            mm(pM, pM.t[:, 0:256], wb, wv[:, c, jj * 128:(jj + 1) * 128], xn2T, xn2T.t[:, c, :],
                               start=(c == 0), stop=(c == 7))
                        cp("act" if jb % 2 == 0 else "dve", qT, qT.t[:, jb, :], pM, pM.t[:, 0:256])
                for i in range(2):
                    for b4 in range(4):
                        for jj in range(4):
                            jb = b4 * 4 + jj
                            mm(pM, pM.t[:, jj * 128:(jj + 1) * 128], qT, qT.t[:, jb, i * 128:(i + 1) * 128],
                               KT, KT.t[:, jb, :])
                        cp("act", sc[i], sc[i].t[:, b4 * 512:(b4 + 1) * 512], pM, pM.t[:])
                for i in range(2):
                    for h in range(8):
                        for half in range(2):
                            sv = sc[i].t[:, (h * 2 + half) * 128:(h * 2 + half + 1) * 128]
                            vv = v16[half]
                            S.op("dve", lambda e: e.max(out=vv.t[:, 0:8], in_=sv), reads=[sc[i]], writes=[vv])
                            S.op("dve", lambda e: e.match_replace(out=tmp128.t[:], in_to_replace=vv.t[:, 0:8],
                                                                  in_values=sv, imm_value=-1e30),
                                 reads=[sc[i], vv], writes=[tmp128])
                            S.op("dve", lambda e: e.max(out=vv.t[:, 8:16], in_=tmp128.t[:]), reads=[tmp128], writes=[vv])
                        tt("dve", cand, cand.t[:].rearrange("p (a b) -> p a b", a=16),
                           v16[0], v16[0].t[:].unsqueeze(2).to_broadcast([128, 16, 16]),
                           v16[1], v16[1].t[:].unsqueeze(1).to_broadcast([128, 16, 16]), ALU.add)
                        S.op("dve", lambda e: e.max(out=c16.t[:, 0:8], in_=cand.t[:]), reads=[cand], writes=[c16])
                        S.op("dve", lambda e: e.match_replace(out=cand2.t[:], in_to_replace=c16.t[:, 0:8],
                                                              in_values=cand.t[:], imm_value=-1e30),
                             reads=[cand, c16], writes=[cand2])
                        S.op("dve", lambda e: e.max(out=c16.t[:, 8:16], in_=cand2.t[:]), reads=[cand2], writes=[c16])
                        ts("dve", negm, negm.t[:], c16, c16.t[:, 0:1], -1.0, None, ALU.mult)
                        act(ex16, ex16.t[:], c16, c16.t[:], AF.Exp, reads=[negm], wr=[zz], bias=negm.t[:, 0:1],
                            accum_out=zz.t[:])
                        act(zz, zz.t[:], zz, zz.t[:], AF.Ln)
                        tt("dve", ncc[i], ncc[i].t[:, h:h + 1], negm, negm.t[:], zz, zz.t[:], ALU.subtract)
                        S.op("dve", lambda e: e.match_replace(out=cand3.t[:], in_to_replace=c16.t[:, 8:16],
                                                              in_values=cand2.t[:], imm_value=-1e30),
                             reads=[cand2, c16], writes=[cand3])
                        S.op("dve", lambda e: e.max(out=c24.t[:], in_=cand3.t[:]), reads=[cand3], writes=[c24])
                        tt("dve", tsum, tsum.t[:], c16, c16.t[:, 15:16], c24, c24.t[:, 0:1], ALU.add)
                        ts("dve", taum[i], taum[i].t[:, h:h + 1], tsum, tsum.t[:], 0.5, None, ALU.mult)
                    tt("dve", ttau[i], ttau[i].t[:], taum[i], taum[i].t[:], ncc[i], ncc[i].t[:], ALU.add)
                    act(ttau[i], ttau[i].t[:], ttau[i], ttau[i].t[:], AF.Exp)
                    for h in range(8):
                        act(E1n[i], E1n[i].t[:, h, :], sc[i], sc[i].t[:, (h * 2) * 128:(h * 2 + 1) * 128], AF.Exp,
                            reads=[ncc[i]], bias=ncc[i].t[:, h:h + 1])
                    act(E2[i], E2[i].t[:], sc[i],
                        sc[i].t[:].rearrange("p (h t k) -> p h t k", h=8, t=2)[:, :, 1, :], AF.Exp)
                pu = pU[0]
                pAccs = [pM, pU[1]]
                wts = {}
                cnt = dict(tn=0)

                def stA(n):
                    eb, i = divmod(n, 2)
                    if i == 0:
                        wts[eb] = (wload(dn_s, dn_s.t[:, :, eb * 512:(eb + 1) * 512], v8),
                                   wload(up_s, up_s.t[:, eb * 4:(eb + 1) * 4, :], v4))
                    dnb = wts[eb][0]
                    dv = v8(dnb.t)
                    ag = Ag[n % 2]
                    for c in range(8):
                        mm(pu, pu.t[:], xn2T, xn2T.t[:, c, i * 128:(i + 1) * 128], dnb, dv[:, c, :],
                           start=(c == 0), stop=(c == 7))
                    act(ag, ag.t[:], pu, pu.t[:], AF.Gelu)

                def stT(n, mid=None):
                    eb, i = divmod(n, 2)
                    for h in range(8):
                        if h == 4 and mid is not None:
                            mid()
                        Tb = Tt[cnt["tn"] % 4]
                        cnt["tn"] += 1
                        if h < 2 or h == 7:
                            for q in range(4):
                                act(Tb, Tb.t[:, q * 128:(q + 1) * 128], E2[i], E2[i].t[:, h, :], AF.Copy,
                                    reads=[E1n[i]], scale=E1n[i].t[:, h, eb * 4 + q:eb * 4 + q + 1])
                        else:
                            tt("pool", Tb, Tb.t[:].rearrange("p (a b) -> p a b", a=4),
                               E1n[i], E1n[i].t[:, h, eb * 4:eb * 4 + 4].unsqueeze(2).to_broadcast([128, 4, 128]),
                               E2[i], E2[i].t[:, h, :].unsqueeze(1).to_broadcast([128, 4, 128]), ALU.mult)
                        gm = gms[n % 2][h]
                        stt("dve", gm, gm.t[:], Tb, Tb.t[:], ttau[i].t[:, h:h + 1], Tb, Tb.t[:],
                            ALU.is_ge, ALU.mult, reads=[ttau[i]])

                def stAcc(n):
                    pAcc = pAccs[n % 2]
                    for h in range(8):
                        gm = gms[n % 2][h]
                        mm(pAcc, pAcc.t[:], ident_b, ident_b.t[:], gm, gm.t[:], start=(h == 0), stop=(h == 7))

                def stG(n):
                    pAcc = pAccs[n % 2]
                    ag = Ag[n % 2]
                    gat = GAT[n % 2]
                    tt("dve", GA, GA.t[:], pAcc, pAcc.t[:], ag, ag.t[:], ALU.mult)
                    half = (n % 2) * 512
                    for q in range(4):
                        tr(pTb, pTb.t[:, half + q * 128:half + (q + 1) * 128], GA, GA.t[:, q * 128:(q + 1) * 128], ident_b)
                    cp("act", gat, gat.t[:], pTb, pTb.t[:, half:half + 512].rearrange("p (a b) -> p a b", a=4))

                def stY(n):
                    eb, i = divmod(n, 2)
                    gat = GAT[n % 2]
                    upb = wts[eb][1]
                    uv = v4(upb.t)
                    for q in range(4):
                        for nn in range(2):
                            mm(pY[i][nn], pY[i][nn].t[:], gat, gat.t[:, q, :], upb, uv[:, q, nn * 512:(nn + 1) * 512],
                               start=(eb == 0 and q == 0), stop=(eb == 31 and q == 3))

                NU = 64
                for k in range(NU + 2):
                    if k < NU:
                        stA(k)
                    if 0 <= k - 1 < NU:
                        stAcc(k - 1)
                    if 0 <= k - 2 < NU:
                        stY(k - 2)
                    gfn = (lambda kk=k: stG(kk - 1)) if 0 <= k - 1 < NU else None
                    if k < NU:
                        stT(k, mid=gfn)
                    elif gfn is not None:
                        gfn()
                for i in range(2):
                    for n in range(2):
                        tt("dve", hf, hf.t[:, n * 512:(n + 1) * 512], pY[i][n], pY[i][n].t[:],
                           xh[i], xh[i].t[:, n * 512:(n + 1) * 512], ALU.add)
                    act(yo_p[i], yo_p[i].t[:], hf, hf.t[:], AF.Square, wr=[ss], accum_out=ss.t[:])
                    rsqrt_col(rstd, ss, 1.0 / 1024, EPS)
                    yo = yo_p[i]
                    stt("dve", yo, yo.t[:], hf, hf.t[:], rstd.t[:, 0:1], fnl, fnl.t[:], ALU.mult, ALU.mult, reads=[rstd])
                    S.dma("pool", y_d[t0 + i * 128:t0 + (i + 1) * 128, :], yo.t[:], reads=[yo], writes=[Tn(None)])

        for st in range(NST):
            if stage >= 1:
                mixer(st)
                S.barrier()
            if stage >= 2:
                peer(st)
                S.barrier(switch=((st + 1) % sw_every == 0 and st + 1 < NST))
        S.finish()
        print("ninst", S.ninst, {k: v["count"] for k, v in S.eng.items()})
    return nc


def host_inputs(inp):
    f = lambda a: np.ascontiguousarray(np.asarray(a, dtype=np.float32))
    w_in = np.asarray(inp["w_in"])[0]
    cols = []
    for h in range(8):
        for base in (512, 1536, 2560, 3584):
            cols.append(np.arange(base + h * 128, base + (h + 1) * 128))
    cols.append(np.arange(0, 512))
    cols.append(np.arange(4624, 6672))
    cols.append(np.arange(4608, 4624))
    cols = np.concatenate(cols)
    assert cols.shape[0] == IN_COLS
    r = np.arange(128)
    d = {}
    d["w_in_p"] = f(w_in[:, cols])
    d["mnw"] = f(np.asarray(inp["mix_norm_w"])[0].reshape(8, 128).T)
    d["fnw"] = f(np.asarray(inp["ffn_norm_w"])[0].reshape(8, 128).T)
    d["pool_w_p"] = f(np.asarray(inp["pool_w"])[0].transpose(1, 0, 2))
    d["pool_scale_p"] = f(np.asarray(inp["pool_scale"])[0].reshape(4, 128).T)
    d["conv_w_p"] = f(np.asarray(inp["conv_w"])[0].T.reshape(24, 128, 4).transpose(1, 0, 2))
    d["a_log_p"] = f(np.broadcast_to(np.asarray(inp["a_log"])[0][None, :], (128, 8)))
    d["dt_bias_p"] = f(np.broadcast_to(np.asarray(inp["dt_bias"])[0][None, :], (128, 8)))
    d["dn_norm_p"] = f(np.asarray(inp["dn_norm_w"])[0].reshape(128, 1))
    d["w_pool_up"] = f(np.asarray(inp["w_pool_up"])[0])
    d["w_dn_up"] = f(np.asarray(inp["w_dn_up"])[0])
    d["w_mix_out"] = f(np.asarray(inp["w_mix_out"])[0])
    d["peer_w_query"] = f(np.asarray(inp["peer_w_query"])[0])
    k1 = np.asarray(inp["peer_keys_1"])[0]
    k2 = np.asarray(inp["peer_keys_2"])[0]
    kt = np.stack([k1, k2], axis=1).reshape(16, 128, 128)
    d["keys_t"] = f(kt.transpose(2, 0, 1))
    d["peer_down_t"] = f(np.asarray(inp["peer_down"])[0].T)
    d["peer_up"] = f(np.asarray(inp["peer_up"])[0])
    d["final_w_p"] = f(np.broadcast_to(np.asarray(inp["final_norm_w"])[None, :], (128, 1024)))
    d["c_ident"] = f(np.eye(128))
    d["c_tri"] = f(r[:, None] <= r[None, :])
    d["c_negu"] = f(np.where(r[None, :] >= r[:, None], 0.0, NEG))
    d["c_negls"] = f(np.where(r[:, None] > r[None, :], 0.0, NEG))
    d["c_ones"] = f(np.ones((128, 128)))
    pf = np.ones((4, 16), np.float32)
    for g in range(4):
        w = 2 << g
        for t in range(16):
            pf[g, t] = w / min(t + 1, w)
    d["c_poolfix"] = f(np.broadcast_to(pf[None], (128, 4, 16)))
    return d


_NC_CACHE = {}


def kernel(**inputs):
    x = np.asarray(inputs["x"], dtype=np.float32)
    B, S_TOK, _ = x.shape
    if S_TOK not in _NC_CACHE:
        _NC_CACHE[S_TOK] = build(S_TOK)
    nc = _NC_CACHE[S_TOK]
    shared = host_inputs(inputs)
    in_maps = []
    for b in range(B):
        m = dict(shared)
        m["x"] = np.ascontiguousarray(x[b])
        in_maps.append(m)
    res = run_bass_kernel_spmd(nc, in_maps, core_ids=list(range(B)))
    return np.stack([np.asarray(r["y"]) for r in res.results], axis=0).astype(np.float32)
```

```python
from contextlib import ExitStack
import numpy as np
import concourse.bass as bass
import concourse.mybir as mybir
from concourse.bass_utils import run_bass_kernel_spmd

F32 = mybir.dt.float32
BF16 = mybir.dt.bfloat16
AF = mybir.ActivationFunctionType
ALU = mybir.AluOpType

NEG = -30000.0
EPS = 1e-6
IN_COLS = 6672
NE = 16384


class Buf:
    def __init__(self):
        self.last_w = None
        self.readers = []


class Tn:
    def __init__(self, t, b=None, psum=False):
        self.t = t
        self.b = b if b is not None else Buf()
        self.psum = psum


class Sched:
    CE = ("pe", "act", "dve", "pool")

    def __init__(self, nc, ctx, ndma=8, nsets=2):
        self.nc = nc
        self.eng = {}
        self.sets = [{} for _ in range(nsets)]
        for nm, obj in (("pe", nc.tensor), ("act", nc.scalar), ("dve", nc.vector),
                        ("pool", nc.gpsimd), ("sp", nc.sync)):
            for k in range(nsets):
                if nm != "sp":
                    self.sets[k][nm] = ctx.enter_context(nc.semaphore(f"s{k}_" + nm))
            self.eng[nm] = dict(name=nm, obj=obj, sem=self.sets[0].get(nm), count=0, waited={})
        self.epoch = 0
        self.dq = {}
        for q in ("sp", "pool"):
            sems = [ctx.enter_context(nc.semaphore(f"d_{q}{i}")) for i in range(ndma)]
            self.dq[q] = dict(sems=sems, n=0)
        self.ndma = ndma
        self.ninst = 0
        self.hook = None
        self.nosw = 0

    def _wait(self, e, tok):
        sem, val, ep = tok
        if ep is not None and ep < self.epoch:
            return
        if e["name"] == "pe" and sem is e["sem"]:
            return
        key = id(sem)
        w = e["waited"]
        if w.get(key, 0) >= val:
            return
        e["obj"].wait_ge(sem, val)
        w[key] = val
        self.ninst += 1

    def _deps(self, e, reads, writes):
        for b in reads:
            if b.last_w is not None:
                self._wait(e, b.last_w)
        for b in writes:
            if b.last_w is not None:
                self._wait(e, b.last_w)
            for r in b.readers:
                self._wait(e, r)

    def _commit(self, tok, reads, writes):
        for b in reads:
            b.readers = [r for r in b.readers if r[0] is not tok[0]] + [tok]
        for b in writes:
            b.last_w = tok
            b.readers = []

    def op(self, en, fn, reads=(), writes=()):
        e = self.eng[en]
        rb = [x.b for x in reads]
        wb = [x.b for x in writes] + [x.b for x in reads if x.psum]
        self._deps(e, rb, wb)
        inst = fn(e["obj"])
        e["count"] += 1
        inst.then_inc(e["sem"], 1)
        tok = (e["sem"], e["count"], self.epoch)
        self._commit(tok, rb, wb)
        self.ninst += 1
        if self.hook is not None:
            self.hook()
        return tok

    def dma(self, q, out, in_, reads=(), writes=()):
        e = self.eng[q]
        d = self.dq[q]
        n = d["n"]
        sem = d["sems"][n % self.ndma]
        if n >= self.ndma:
            self._wait(e, (sem, 16 * (n // self.ndma), None))
        rb = [x.b for x in reads]
        wb = [x.b for x in writes]
        self._deps(e, rb, wb)
        e["obj"].dma_start(out=out, in_=in_).then_inc(sem, 16)
        d["n"] = n + 1
        tok = (sem, 16 * (n // self.ndma + 1), None)
        self._commit(tok, rb, wb)
        self.ninst += 1
        return tok

    def run_interleaved(self, fns):
        import threading
        n = len(fns)
        go = [threading.Semaphore(0) for _ in range(n)]
        main = threading.Semaphore(0)
        done = [False] * n
        errs = []
        cur = [0]

        def nxt(i):
            for j in range(i + 1, i + 1 + n):
                if not done[j % n]:
                    return j % n
            return None

        def hook():
            if self.nosw:
                return
            i = cur[0]
            j = nxt(i)
            if j is None or j == i:
                return
            cur[0] = j
            go[j].release()
            go[i].acquire()

        def worker(i):
            go[i].acquire()
            try:
                fns[i]()
            except BaseException as ex:
                errs.append(ex)
            done[i] = True
            j = nxt(i)
            if j is None:
                main.release()
            else:
                cur[0] = j
                go[j].release()

        ths = [threading.Thread(target=worker, args=(i,)) for i in range(n)]
        for t in ths:
            t.start()
        old = self.hook
        self.hook = hook
        cur[0] = 0
        go[0].release()
        main.acquire()
        for t in ths:
            t.join()
        self.hook = old
        if errs:
            raise errs[0]

    def _sync_all(self):
        for a in self.CE + ("sp",):
            for b in self.CE:
                if a != b and self.eng[b]["count"] > 0:
                    self._wait(self.eng[a], (self.eng[b]["sem"], self.eng[b]["count"], self.epoch))

    def barrier(self, switch=False):
        self._sync_all()
        if not switch:
            return
        self.epoch += 1
        new = self.sets[self.epoch]
        for nm, e in self.eng.items():
            e["sem"] = new.get(nm)
            e["count"] = 0

    def finish(self):
        for q, d in self.dq.items():
            e = self.eng[q]
            for i, sem in enumerate(d["sems"]):
                cnt = (d["n"] - i + self.ndma - 1) // self.ndma
                if cnt > 0:
                    e["obj"].wait_ge(sem, 16 * cnt)


def build(S_TOK, dbg=False, stage=99, sw_every=2):
    NST = S_TOK // 256
    nc = bass.Bass("TRN2", target_bir_lowering=False)

    def din(name, shape):
        return nc.dram_tensor(name, list(shape), F32, kind="ExternalInput").ap()

    x_d = din("x", [S_TOK, 1024])
    win_d = din("w_in_p", [1024, IN_COLS])
    mnw_d = din("mnw", [128, 8])
    fnw_d = din("fnw", [128, 8])
    pw_d = din("pool_w_p", [128, 4, 128])
    psc_d = din("pool_scale_p", [128, 4])
    cw_d = din("conv_w_p", [128, 24, 4])
    alog_d = din("a_log_p", [128, 8])
    dtb_d = din("dt_bias_p", [128, 8])
    dnw_d = din("dn_norm_p", [128, 1])
    wpu_d = din("w_pool_up", [512, 1024])
    wdu_d = din("w_dn_up", [1024, 1024])
    wo_d = din("w_mix_out", [1024, 1024])
    wq_d = din("peer_w_query", [1024, 2048])
    kt_d = din("keys_t", [128, 16, 128])
    dnT_d = din("peer_down_t", [1024, NE])
    up_d = din("peer_up", [NE, 1024])
    fnl_d = din("final_w_p", [128, 1024])
    ident_d = din("c_ident", [128, 128])
    tri_d = din("c_tri", [128, 128])
    negu_d = din("c_negu", [128, 128])
    negls_d = din("c_negls", [128, 128])
    ones_d = din("c_ones", [128, 128])
    pfix_d = din("c_poolfix", [128, 4, 16])
    y_d = nc.dram_tensor("y", [S_TOK, 1024], F32, kind="ExternalOutput").ap()
    if dbg:
        h1_d = nc.dram_tensor("h1dbg", [S_TOK, 1024], F32, kind="ExternalOutput").ap()

    win_s = Tn(nc.dram_tensor("win_s", [128, 8, IN_COLS], BF16).ap())
    wq_s = Tn(nc.dram_tensor("wq_s", [128, 8, 2048], BF16).ap())
    dn_s = Tn(nc.dram_tensor("dn_s", [128, 8, NE], BF16).ap())
    up_s = Tn(nc.dram_tensor("up_s", [128, 128, 1024], BF16).ap())

    with ExitStack() as ctx:
        NEP = (NST + sw_every - 1) // sw_every
        S = Sched(nc, ctx, nsets=NEP + 1)

        uid = [0]

        def un_(name):
            uid[0] += 1
            return f"t{uid[0]}_{name}"

        def sb(cx, name, shape, dt=F32):
            return Tn(cx.enter_context(nc.sbuf_tensor(un_(name), list(shape), dt)))

        def ps(cx, name, shape, dt=F32):
            return Tn(cx.enter_context(nc.psum_tensor(un_(name), list(shape), dt)), psum=True)

        def mm(o, o_ap, l, l_ap, r, r_ap, start=True, stop=True):
            S.op("pe", lambda e: e.matmul(o_ap, lhsT=l_ap, rhs=r_ap, start=start, stop=stop),
                 reads=[l, r], writes=[o])

        def tr(o, o_ap, i, i_ap, idn):
            S.op("pe", lambda e: e.transpose(out=o_ap, in_=i_ap, identity=idn.t[:]),
                 reads=[i, idn], writes=[o])

        def act(o, o_ap, i, i_ap, func, reads=(), wr=(), **kw):
            S.op("act", lambda e: e.activation(out=o_ap, in_=i_ap, func=func, **kw),
                 reads=[i] + list(reads), writes=[o] + list(wr))

        def ts(en, o, o_ap, i, i_ap, s1, s2, op0, op1=None, reads=()):
            if op1 is None:
                S.op(en, lambda e: e.tensor_scalar(out=o_ap, in0=i_ap, scalar1=s1, scalar2=None, op0=op0),
                     reads=[i] + list(reads), writes=[o])
            else:
                S.op(en, lambda e: e.tensor_scalar(out=o_ap, in0=i_ap, scalar1=s1, scalar2=s2, op0=op0, op1=op1),
                     reads=[i] + list(reads), writes=[o])

        def tt(en, o, o_ap, a, a_ap, b, b_ap, op):
            S.op(en, lambda e: e.tensor_tensor(out=o_ap, in0=a_ap, in1=b_ap, op=op),
                 reads=[a, b], writes=[o])

        def stt(en, o, o_ap, a, a_ap, sc, b, b_ap, op0, op1, reads=()):
            S.op(en, lambda e: e.scalar_tensor_tensor(out=o_ap, in0=a_ap, scalar=sc, in1=b_ap, op0=op0, op1=op1),
                 reads=[a, b] + list(reads), writes=[o])

        def cp(en, o, o_ap, i, i_ap):
            if en == "act":
                S.op("act", lambda e: e.copy(out=o_ap, in_=i_ap), reads=[i], writes=[o])
            else:
                S.op(en, lambda e: e.tensor_copy(out=o_ap, in_=i_ap), reads=[i], writes=[o])

        def rsqrt_col(o, i, scale, eps, reads=()):
            ts("dve", o, o.t[:], i, i.t[:], scale, eps, ALU.mult, ALU.add)
            act(o, o.t[:], o, o.t[:], AF.Sqrt)
            S.op("dve", lambda e: e.reciprocal(out=o.t[:], in_=o.t[:]), reads=[o], writes=[o])

        ident_f = sb(ctx, "ident_f", [128, 128])
        ident_b = sb(ctx, "ident_b", [128, 128], BF16)
        tri = sb(ctx, "tri", [128, 128])
        negu = sb(ctx, "negu", [128, 128])
        negls = sb(ctx, "negls", [128, 128])
        ones = sb(ctx, "ones", [128, 128])
        pfix = sb(ctx, "pfix", [128, 4, 16])
        mnw = sb(ctx, "mnw", [128, 8])
        fnw = sb(ctx, "fnw", [128, 8])
        psc = sb(ctx, "psc", [128, 4])
        cw = sb(ctx, "cw", [128, 24, 4])
        nA = sb(ctx, "nA", [128, 8])
        dtb = sb(ctx, "dtb", [128, 8])
        dnw = sb(ctx, "dnw", [128, 1])
        fnl = sb(ctx, "fnl", [128, 1024])
        wpu = sb(ctx, "wpu", [128, 4, 1024], BF16)
        wdu = sb(ctx, "wdu", [128, 8, 1024], BF16)
        wo = sb(ctx, "wo", [128, 8, 1024], BF16)
        pw = sb(ctx, "pw", [128, 4, 128], BF16)
        wsm = sb(ctx, "wsm", [128, 8, 16], BF16)
        KT = sb(ctx, "KT", [128, 16, 128], BF16)
        wpool = [sb(ctx, f"wpool{i}", [128, 4096], BF16) for i in range(5)]
        wp_n = [0]
        Sst = [sb(ctx, f"Sst{h}", [128, 128]) for h in range(8)]
        ccar = [sb(ctx, f"ccar{b}", [128, 3]) for b in range(24)]
        xab = [sb(ctx, f"xab{g}", [128, 272]) for g in range(4)]
        xh = [sb(ctx, f"xh{i}", [128, 1024]) for i in range(2)]
        yo_p = [sb(ctx, f"yo{i}", [128, 1024]) for i in range(2)]

        def wload(src, src_ap, view):
            t = wpool[wp_n[0] % len(wpool)]
            wp_n[0] += 1
            S.dma("sp", view(t.t), src_ap, reads=[src], writes=[t])
            return t

        v8 = lambda t: t[:].rearrange("p (c n) -> p c n", c=8)
        v4 = lambda t: t[:].rearrange("p (c n) -> p c n", c=4)

        dummy = Tn(None)
        for (t, d) in ((ident_f, ident_d), (tri, tri_d), (negu, negu_d), (negls, negls_d), (ones, ones_d),
                       (pfix, pfix_d), (mnw, mnw_d), (fnw, fnw_d), (psc, psc_d), (cw, cw_d), (nA, alog_d),
                       (dtb, dtb_d), (dnw, dnw_d), (fnl, fnl_d)):
            S.dma("sp", t.t[:], d, writes=[t])
        cp("dve", ident_b, ident_b.t[:], ident_f, ident_f.t[:])
        act(nA, nA.t[:], nA, nA.t[:], AF.Exp)
        ts("dve", nA, nA.t[:], nA, nA.t[:], -1.0, None, ALU.mult)
        for h in range(8):
            S.op("dve", lambda e: e.memset(Sst[h].t[:], 0.0), writes=[Sst[h]])
        for b in range(24):
            S.op("pool", lambda e: e.memset(ccar[b].t[:], 0.0), writes=[ccar[b]])
        for g in range(4):
            S.op("pool", lambda e: e.memset(xab[g].t[:], 0.0), writes=[xab[g]])

        with ExitStack() as pc:
            stg = [sb(pc, f"stg{i}", [128, 4096]) for i in range(2)]
            sn = [0]

            def prep(src_ap, shape3, dst, dst_ap, scale_t=None, scale_ap=None):
                a, b = shape3
                st_ = stg[sn[0] % 2]
                sn[0] += 1
                sv = st_.t[:, 0:a * b].rearrange("p (a b) -> p a b", a=a)
                S.dma("sp", sv, src_ap, writes=[st_])
                ob = wpool[wp_n[0] % len(wpool)]
                wp_n[0] += 1
                ov = ob.t[:, 0:a * b].rearrange("p (a b) -> p a b", a=a)
                en = "dve" if sn[0] % 2 == 0 else "pool"
                if scale_t is None:
                    cp("act" if sn[0] % 2 == 0 else "dve", ob, ov, st_, sv)
                else:
                    tt(en, ob, ov, st_, sv, scale_t, scale_ap, ALU.mult)
                if dst is None:
                    return ob, ov
                S.dma("pool", dst_ap, ov, reads=[ob], writes=[dst])
                return ob, ov

            for (wt, d, a, b, rs) in ((wpu, wpu_d, 4, 1024, "(g p) n -> p g n"),
                                      (wo, wo_d, 8, 1024, "(c p) n -> p c n")):
                for half in range(a // 4):
                    ob, ov = prep(d.rearrange(rs, p=128)[:, half * 4:(half + 1) * 4, :], (4, 1024), None, None)
                    cp("dve", wt, wt.t[:, half * 4:(half + 1) * 4, :], ob, ov)
            for half in range(2):
                ob, ov = prep(wdu_d.rearrange("(h p) n -> p h n", p=128)[:, half * 4:(half + 1) * 4, :], (4, 1024),
                              None, None, dnw, dnw.t[:, 0:1].unsqueeze(2).to_broadcast([128, 4, 1024]))
                cp("dve", wdu, wdu.t[:, half * 4:(half + 1) * 4, :], ob, ov)
            ob, ov = prep(pw_d, (4, 128), None, None)
            cp("dve", pw, pw.t[:], ob, ov)
            ob, ov = prep(kt_d, (16, 128), None, None)
            cp("dve", KT, KT.t[:], ob, ov)
            winv = win_d.rearrange("(c p) n -> p c n", p=128)
            for blk in range(14):
                c0 = blk * 512
                n = min(512, IN_COLS - c0)
                ob, ov = prep(winv[:, :, c0:c0 + n], (8, n), win_s, win_s.t[:, :, c0:c0 + n],
                              mnw, mnw.t[:].unsqueeze(2).to_broadcast([128, 8, n]))
                if blk == 13:
                    cp("dve", wsm, wsm.t[:], ob, ov)
            wqv = wq_d.rearrange("(c p) n -> p c n", p=128)
            for blk in range(4):
                prep(wqv[:, :, blk * 512:(blk + 1) * 512], (8, 512), wq_s, wq_s.t[:, :, blk * 512:(blk + 1) * 512],
                     fnw, fnw.t[:].unsqueeze(2).to_broadcast([128, 8, 512]))
            dnv = dnT_d.rearrange("(c p) e -> p c e", p=128)
            upv = up_d.rearrange("(ch p) d -> p ch d", p=128)
            for blk in range(32 if stage >= 2 else 0):
                prep(dnv[:, :, blk * 512:(blk + 1) * 512], (8, 512), dn_s, dn_s.t[:, :, blk * 512:(blk + 1) * 512],
                     fnw, fnw.t[:].unsqueeze(2).to_broadcast([128, 8, 512]))
                prep(upv[:, blk * 4:(blk + 1) * 4, :], (4, 1024), up_s, up_s.t[:, blk * 4:(blk + 1) * 4, :])
        S.barrier(switch=True)

        def mixer(st):
            t0 = st * 256
            with ExitStack() as mx:
                pTb = ps(mx, "pTb", [128, 1024], BF16)
                big = [ps(mx, f"big{k}", [128, 512]) for k in range(4)]
                bn = [0]
                qbank = [mx.enter_context(nc.psum_tensor(un_(f"qb{k}"), [128, 512], F32)) for k in range(3)]
                qsl = [Tn(qbank[k], psum=True) for k in range(3)]
                qn = [0]

                def nbig():
                    b = big[bn[0] % 4]
                    bn[0] += 1
                    return b

                def nq():
                    k = qn[0] % 12
                    qn[0] += 1
                    s = qsl[k % 3]
                    qq = k // 3
                    return s, s.t[:, qq * 128:(qq + 1) * 128]

                sq = sb(mx, "sq", [128, 1024])
                xsb = sb(mx, "xsb", [128, 1024], BF16)
                xT = sb(mx, "xT", [128, 8, 256], BF16)
                ss = sb(mx, "ss", [128, 1])
                rstd = sb(mx, "rstd", [128, 1])
                def mk_slot():
                    d_ = {}
                    d_["raw"] = [sb(mx, f"raw{k}", [128, 259]) for k in range(3)]
                    d_["cacc"] = sb(mx, "cacc", [128, 256])
                    d_["qkvT"] = [sb(mx, f"qkvT{k}", [128, 256]) for k in range(3)]
                    d_["szT"] = sb(mx, "szT", [128, 256])
                    d_["sqn"] = sb(mx, "sqn", [128, 256])
                    d_["rn"] = sb(mx, "rn", [128, 256])
                    d_["kbg"] = sb(mx, "kbg", [128, 128])
                    d_["kdec"] = sb(mx, "kdec", [128, 128])
                    d_["vb"] = sb(mx, "vb", [128, 128])
                    d_["trig"] = sb(mx, "trig", [128, 128])
                    d_["Am"] = sb(mx, "Am", [128, 128])
                    d_["EU"] = sb(mx, "EU", [128, 128])
                    d_["DLs"] = sb(mx, "DLs", [128, 128])
                    d_["egrow"] = sb(mx, "egrow", [128, 128])
                    d_["Mb"] = [sb(mx, f"Mb{k}", [128, 128]) for k in range(2)]
                    d_["MTb"] = [sb(mx, f"MTb{k}", [128, 128]) for k in range(2)]
                    d_["PTb"] = [sb(mx, f"PTb{k}", [128, 128]) for k in range(2)]
                    d_["attnT"] = sb(mx, "attnT", [128, 128])
                    d_["qgT"] = sb(mx, "qgT", [128, 128])
                    d_["nwT"] = sb(mx, "nwT", [128, 128])
                    d_["vn"] = sb(mx, "vn", [128, 128])
                    d_["oss"] = sb(mx, "oss", [128, 1])
                    d_["orst"] = sb(mx, "orst", [128, 1])
                    d_["osq"] = sb(mx, "osq", [128, 128])
                    d_["onb"] = sb(mx, "onb", [128, 128], BF16)
                    return d_
                SL = [mk_slot(), mk_slot()]
                lg = [sb(mx, f"lg{i}", [128, 16]) for i in range(2)]
                beta = [sb(mx, f"beta{i}", [128, 8]) for i in range(2)]
                nbeta = [sb(mx, f"nbeta{i}", [128, 8]) for i in range(2)]
                gg = [sb(mx, f"gg{i}", [128, 8]) for i in range(2)]
                gc = [sb(mx, f"gc{i}", [128, 8]) for i in range(2)]
                egl = [sb(mx, f"egl{i}", [128, 8]) for i in range(2)]
                kds = [sb(mx, f"kds{i}", [128, 8]) for i in range(2)]
                bgs = [sb(mx, f"bgs{i}", [128, 8]) for i in range(2)]
                onT = sb(mx, "onT", [128, 8, 256], BF16)
                pa = [sb(mx, f"pa{k}", [128, 272]) for k in range(2)]
                pooledb = sb(mx, "pooledb", [128, 256], BF16)
                paT = sb(mx, "paT", [128, 4, 256], BF16)
                sga = sb(mx, "sga", [128, 256])
                m1 = sb(mx, "m1", [128, 256])
                sgb = sb(mx, "sgb", [128, 256])
                m2 = sb(mx, "m2", [128, 256])
                mT = sb(mx, "mT", [128, 8, 256], BF16)

                for i in range(2):
                    S.dma("sp", xh[i].t[:], x_d[t0 + i * 128:t0 + (i + 1) * 128, :], writes=[xh[i]])
                    act(sq, sq.t[:], xh[i], xh[i].t[:], AF.Square, wr=[ss], accum_out=ss.t[:])
                    rsqrt_col(rstd, ss, 1.0 / 1024, EPS)
                    ts("dve", xsb, xsb.t[:], xh[i], xh[i].t[:], rstd.t[:, 0:1], None, ALU.mult, reads=[rstd])
                    for c in range(8):
                        tr(pTb, pTb.t[:, c * 128:(c + 1) * 128], xsb, xsb.t[:, c * 128:(c + 1) * 128], ident_b)
                    cp("act", xT, xT.t[:, :, i * 128:(i + 1) * 128], pTb,
                       pTb.t[:].rearrange("p (c n) -> p c n", c=8))

                for i in range(2):
                    s_, s_ap = nq()
                    for c in range(8):
                        mm(s_, s_ap[:, 0:16], xT, xT.t[:, c, i * 128:(i + 1) * 128], wsm, wsm.t[:, c, :],
                           start=(c == 0), stop=(c == 7))
                    cp("dve", lg[i], lg[i].t[:], s_, s_ap[:, 0:16])
                    act(beta[i], beta[i].t[:], lg[i], lg[i].t[:, 0:8], AF.Sigmoid)
                    ts("dve", nbeta[i], nbeta[i].t[:], beta[i], beta[i].t[:], -1.0, None, ALU.mult)
                    tt("dve", gg[i], gg[i].t[:], lg[i], lg[i].t[:, 8:16], dtb, dtb.t[:], ALU.add)
                    act(gg[i], gg[i].t[:], gg[i], gg[i].t[:], AF.Exp)
                    act(gg[i], gg[i].t[:], gg[i], gg[i].t[:], AF.Ln, bias=1.0)
                    tt("dve", gg[i], gg[i].t[:], gg[i], gg[i].t[:], nA, nA.t[:], ALU.mult)
                    s_, s_ap = nq()
                    mm(s_, s_ap[:, 0:8], tri, tri.t[:], gg[i], gg[i].t[:])
                    cp("dve", gc[i], gc[i].t[:], s_, s_ap[:, 0:8])
                    s_, s_ap = nq()
                    mm(s_, s_ap[:, 0:8], ones, ones.t[:], gg[i], gg[i].t[:])
                    tt("dve", kds[i], kds[i].t[:], s_, s_ap[:, 0:8], gc[i], gc[i].t[:], ALU.subtract)
                    act(kds[i], kds[i].t[:], kds[i], kds[i].t[:], AF.Exp)
                    act(egl[i], egl[i].t[:], s_, s_ap[:, 0:8], AF.Exp)
                    act(bgs[i], bgs[i].t[:], gc[i], gc[i].t[:], AF.Exp)
                    tt("dve", bgs[i], bgs[i].t[:], bgs[i], bgs[i].t[:], beta[i], beta[i].t[:], ALU.mult)

                def head_body(h, slot):
                    d_ = SL[slot]
                    raw = d_["raw"]
                    cacc = d_["cacc"]
                    qkvT = d_["qkvT"]
                    szT = d_["szT"]
                    sqn = d_["sqn"]
                    rn = d_["rn"]
                    kbg = d_["kbg"]
                    kdec = d_["kdec"]
                    vb = d_["vb"]
                    trig = d_["trig"]
                    Am = d_["Am"]
                    EU = d_["EU"]
                    DLs = d_["DLs"]
                    egrow = d_["egrow"]
                    Mb = d_["Mb"]
                    MTb = d_["MTb"]
                    PTb = d_["PTb"]
                    attnT = d_["attnT"]
                    qgT = d_["qgT"]
                    nwT = d_["nwT"]
                    vn = d_["vn"]
                    oss = d_["oss"]
                    orst = d_["orst"]
                    osq = d_["osq"]
                    onb = d_["onb"]
                    wb = wload(win_s, win_s.t[:, :, h * 512:(h + 1) * 512], v8)
                    wv = v8(wb.t)
                    for k in range(4):
                        pb = nbig()
                        for c in range(8):
                            mm(pb, pb.t[:, 0:256], wb, wv[:, c, k * 128:(k + 1) * 128], xT, xT.t[:, c, :],
                               start=(c == 0), stop=(c == 7))
                        if k == 3:
                            act(szT, szT.t[:], pb, pb.t[:, 0:256], AF.Silu)
                            continue
                        blk = k * 8 + h
                        r = raw[k]
                        cp("pool", r, r.t[:, 0:3], ccar[blk], ccar[blk].t[:])
                        cp("act", r, r.t[:, 3:259], pb, pb.t[:, 0:256])
                        cp("pool", ccar[blk], ccar[blk].t[:], r, r.t[:, 256:259])
                        ts("dve", cacc, cacc.t[:], r, r.t[:, 0:256], cw.t[:, blk, 0:1], None, ALU.mult, reads=[cw])
                        for j in range(1, 4):
                            stt("dve", cacc, cacc.t[:], r, r.t[:, j:j + 256], cw.t[:, blk, j:j + 1], cacc, cacc.t[:],
                                ALU.mult, ALU.add, reads=[cw])
                        act(qkvT[k], qkvT[k].t[:], cacc, cacc.t[:], AF.Silu)
                        if k < 2:
                            tt("dve", sqn, sqn.t[:], qkvT[k], qkvT[k].t[:], qkvT[k], qkvT[k].t[:], ALU.mult)
                            pn = nbig()
                            mm(pn, pn.t[:, 0:256], ones, ones.t[:], sqn, sqn.t[:])
                            ts("dve", rn, rn.t[:], pn, pn.t[:, 0:256], EPS, None, ALU.add)
                            act(rn, rn.t[:], rn, rn.t[:], AF.Sqrt)
                            S.op("dve", lambda e: e.reciprocal(out=rn.t[:], in_=rn.t[:]), reads=[rn], writes=[rn])
                            sc = (128.0 ** -0.5) if k == 0 else 1.0
                            stt("dve", qkvT[k], qkvT[k].t[:], qkvT[k], qkvT[k].t[:], sc, rn, rn.t[:],
                                ALU.mult, ALU.mult)
                    qT_, kT_, vT_ = qkvT
                    for i in range(2):
                        sl = slice(i * 128, (i + 1) * 128)
                        hh = slice(h, h + 1)
                        k_s, k_ap = nq()
                        tr(k_s, k_ap, kT_, kT_.t[:, sl], ident_f)
                        v_s, v_ap = nq()
                        tr(v_s, v_ap, vT_, vT_.t[:, sl], ident_f)
                        g_s, g_ap = nq()
                        mm(g_s, g_ap, kT_, kT_.t[:, sl], kT_, kT_.t[:, sl])
                        a_s, a_ap = nq()
                        mm(a_s, a_ap, kT_, kT_.t[:, sl], qT_, qT_.t[:, sl])
                        ts("dve", kbg, kbg.t[:], k_s, k_ap, bgs[i].t[:, hh], None, ALU.mult, reads=[bgs[i]])
                        act(kdec, kdec.t[:], k_s, k_ap, AF.Copy, reads=[kds[i]], scale=kds[i].t[:, hh])
                        act(vb, vb.t[:], v_s, v_ap, AF.Copy, reads=[beta[i]], scale=beta[i].t[:, hh])
                        ts("pool", trig, trig.t[:], tri, tri.t[:], gg[i].t[:, hh], None, ALU.mult, reads=[gg[i]])
                        r_s, r_ap = nq()
                        mm(r_s, r_ap, ones, ones.t[:], trig, trig.t[:])
                        ts("dve", Am, Am.t[:], r_s, r_ap, gc[i].t[:, hh], None, ALU.subtract, reads=[gc[i]])
                        act(egrow, egrow.t[:], r_s, r_ap, AF.Exp)
                        tt("pool", EU, EU.t[:], Am, Am.t[:], negu, negu.t[:], ALU.add)
                        act(EU, EU.t[:], EU, EU.t[:], AF.Exp)
                        tt("pool", DLs, DLs.t[:], negls, negls.t[:], Am, Am.t[:], ALU.subtract)
                        act(DLs, DLs.t[:], DLs, DLs.t[:], AF.Exp)
                        M, MT, PT = Mb[0], MTb[0], PTb[0]
                        stt("dve", M, M.t[:], g_s, g_ap, nbeta[i].t[:, hh], DLs, DLs.t[:], ALU.mult, ALU.mult,
                            reads=[nbeta[i]])
                        tt("dve", attnT, attnT.t[:], a_s, a_ap, EU, EU.t[:], ALU.mult)
                        tt("pool", qgT, qgT.t[:], qT_, qT_.t[:, sl], egrow, egrow.t[:], ALU.mult)
                        t_s, t_ap = nq()
                        tr(t_s, t_ap, M, M.t[:], ident_f)
                        cp("act", MT, MT.t[:], t_s, t_ap)
                        tt("dve", PT, PT.t[:], t_s, t_ap, ident_f, ident_f.t[:], ALU.add)
                        for kk in range(6):
                            Mn, MTn, PTn = Mb[(kk + 1) % 2], MTb[(kk + 1) % 2], PTb[(kk + 1) % 2]
                            s1, s1ap = nq()
                            mm(s1, s1ap, MT, MT.t[:], M, M.t[:])
                            cp("act", Mn, Mn.t[:], s1, s1ap)
                            if kk < 5:
                                s2, s2ap = nq()
                                mm(s2, s2ap, M, M.t[:], MT, MT.t[:])
                                cp("dve", MTn, MTn.t[:], s2, s2ap)
                            s3, s3ap = nq()
                            mm(s3, s3ap, Mn, Mn.t[:], PT, PT.t[:])
                            tt("dve", PTn, PTn.t[:], s3, s3ap, PT, PT.t[:], ALU.add)
                            M, MT, PT = Mn, MTn, PTn
                        w_s, w_ap = nq()
                        mm(w_s, w_ap, kbg, kbg.t[:], PT, PT.t[:])
                        S.op("act", lambda e: e.mul(out=nwT.t[:], in_=w_ap, mul=-1.0), reads=[w_s], writes=[nwT])
                        n_s, n_ap = nq()
                        S.nosw += 1
                        mm(n_s, n_ap, PT, PT.t[:], vb, vb.t[:], start=True, stop=False)
                        S.nosw -= 1
                        mm(n_s, n_ap, nwT, nwT.t[:], Sst[h], Sst[h].t[:], start=False, stop=True)
                        cp("dve", vn, vn.t[:], n_s, n_ap)
                        o_s, o_ap = nq()
                        S.nosw += 1
                        mm(o_s, o_ap, qgT, qgT.t[:], Sst[h], Sst[h].t[:], start=True, stop=False)
                        S.nosw -= 1
                        mm(o_s, o_ap, attnT, attnT.t[:], vn, vn.t[:], start=False, stop=True)
                        u_s, u_ap = nq()
                        mm(u_s, u_ap, kdec, kdec.t[:], vn, vn.t[:])
                        stt("dve", Sst[h], Sst[h].t[:], Sst[h], Sst[h].t[:], egl[i].t[:, hh], u_s, u_ap,
                            ALU.mult, ALU.add, reads=[egl[i]])
                        act(osq, osq.t[:], o_s, o_ap, AF.Square, wr=[oss], accum_out=oss.t[:])
                        rsqrt_col(orst, oss, 1.0 / 128, EPS)
                        act(onb, onb.t[:], o_s, o_ap, AF.Copy, reads=[orst], scale=orst.t[:, 0:1])
                        pslot = pTb.t[:, (h % 8) * 128:(h % 8 + 1) * 128]
                        tr(pTb, pslot, onb, onb.t[:], ident_b)
                        tt("dve", onT, onT.t[:, h, sl], pTb, pslot, szT, szT.t[:, sl], ALU.mult)

                for hp in range(0, 8, 2):
                    S.run_interleaved([lambda hp=hp: head_body(hp, 0), lambda hp=hp: head_body(hp + 1, 1)])

                wb = wload(win_s, win_s.t[:, :, 4096:4608], v8)
                wv = v8(wb.t)
                for g in range(4):
                    win_ = 2 << g
                    pb = nbig()
                    for c in range(8):
                        mm(pb, pb.t[:, 0:256], wb, wv[:, c, g * 128:(g + 1) * 128], xT, xT.t[:, c, :],
                           start=(c == 0), stop=(c == 7))
                    xg = xab[g]
                    cp("pool", xg, xg.t[:, 0:16], xg, xg.t[:, 256:272])
                    cp("act", xg, xg.t[:, 16:272], pb, pb.t[:, 0:256])
                    src = xg
                    sh = 1
                    for stp in range(g + 1):
                        dst = pa[stp % 2]
                        lo = 2 * sh - 1
                        tt("dve", dst, dst.t[:, lo:272], src, src.t[:, lo:272], src, src.t[:, lo - sh:272 - sh], ALU.add)
                        src = dst
                        sh *= 2
                    if st == 0:
                        tt("dve", src, src.t[:, 16:32], src, src.t[:, 16:32], pfix, pfix.t[:, g, :], ALU.mult)
                    stt("dve", pooledb, pooledb.t[:], src, src.t[:, 16:272], 1.0 / win_, xg, xg.t[:, 16:272],
                        ALU.mult, ALU.subtract)
                    pb2 = nbig()
                    mm(pb2, pb2.t[:, 0:256], pw, pw.t[:, g, :], pooledb, pooledb.t[:])
                    act(paT, paT.t[:, g, :], pb2, pb2.t[:, 0:256], AF.Copy, reads=[psc], scale=psc.t[:, g:g + 1])

                wga = [wload(win_s, win_s.t[:, :, 4608 + k * 512:4608 + (k + 1) * 512], v8) for k in range(2)]
                wgb = [wload(win_s, win_s.t[:, :, 5632 + k * 512:5632 + (k + 1) * 512], v8) for k in range(2)]
                for j in range(8):
                    js = slice(j * 128, (j + 1) * 128)
                    jw = slice((j % 4) * 128, (j % 4 + 1) * 128)
                    pA, pB, pC, pD = big[0], big[1], big[2], big[3]
                    for g in range(4):
                        mm(pA, pA.t[:, 0:256], wpu, wpu.t[:, g, js], paT, paT.t[:, g, :], start=(g == 0), stop=(g == 3))
                    wa = wga[j // 4]
                    for c in range(8):
                        mm(pB, pB.t[:, 0:256], wa, v8(wa.t)[:, c, jw], xT, xT.t[:, c, :], start=(c == 0), stop=(c == 7))
                    for hd in range(8):
                        mm(pC, pC.t[:, 0:256], wdu, wdu.t[:, hd, js], onT, onT.t[:, hd, :], start=(hd == 0), stop=(hd == 7))
                    wg = wgb[j // 4]
                    for c in range(8):
                        mm(pD, pD.t[:, 0:256], wg, v8(wg.t)[:, c, jw], xT, xT.t[:, c, :], start=(c == 0), stop=(c == 7))
                    act(sga, sga.t[:], pB, pB.t[:, 0:256], AF.Sigmoid)
                    tt("dve", m1, m1.t[:], pA, pA.t[:, 0:256], sga, sga.t[:], ALU.mult)
                    act(sgb, sgb.t[:], pD, pD.t[:, 0:256], AF.Sigmoid)
                    tt("dve", m2, m2.t[:], pC, pC.t[:, 0:256], sgb, sgb.t[:], ALU.mult)
                    tt("pool", mT, mT.t[:, j, :], m1, m1.t[:], m2, m2.t[:], ALU.add)

                for i in range(2):
                    for n in range(2):
                        pb = nbig()
                        for k in range(8):
                            mm(pb, pb.t[:], mT, mT.t[:, k, i * 128:(i + 1) * 128], wo, wo.t[:, k, n * 512:(n + 1) * 512],
                               start=(k == 0), stop=(k == 7))
                        tt("dve", xh[i], xh[i].t[:, n * 512:(n + 1) * 512], pb, pb.t[:],
                           xh[i], xh[i].t[:, n * 512:(n + 1) * 512], ALU.add)
                    if dbg:
                        S.dma("pool", h1_d[t0 + i * 128:t0 + (i + 1) * 128, :], xh[i].t[:], reads=[xh[i]], writes=[Tn(None)])

        def peer(st):
            t0 = st * 256
            with ExitStack() as px:
                pY = [[ps(px, f"pY{i}{n}", [128, 512]) for n in range(2)] for i in range(2)]
                pU = [ps(px, f"pU{k}", [128, 512]) for k in range(2)]
                pTb = ps(px, "pTbp", [128, 1024], BF16)
                pM = ps(px, "pM", [128, 512])
                hsb = sb(px, "hsb", [128, 1024], BF16)
                xn2T = sb(px, "xn2T", [128, 8, 256], BF16)
                ss = sb(px, "pss", [128, 1])
                rstd = sb(px, "prstd", [128, 1])
                qT = sb(px, "pqT", [128, 16, 256], BF16)
                sc = [sb(px, f"sc{i}", [128, 2048]) for i in range(2)]
                def mk_tk():
                    d_ = {}
                    d_["v16"] = [sb(px, f"v16{k}", [128, 16]) for k in range(2)]
                    d_["tmp128"] = sb(px, "tmp128", [128, 128])
                    d_["cand"] = sb(px, "cand", [128, 256])
                    d_["cand2"] = sb(px, "cand2", [128, 256])
                    d_["c16"] = sb(px, "c16", [128, 16])
                    d_["ex16"] = sb(px, "ex16", [128, 16])
                    d_["negm"] = sb(px, "negm", [128, 1])
                    d_["zz"] = sb(px, "zz", [128, 1])
                    d_["c24"] = sb(px, "c24", [128, 8])
                    d_["tsum"] = sb(px, "tsum", [128, 1])
                    d_["cand3"] = d_["cand"]
                    return d_
                TK = [mk_tk(), mk_tk()]
                taum = [sb(px, f"taum{i}", [128, 8]) for i in range(2)]
                ttau = [sb(px, f"ttau{i}", [128, 8]) for i in range(2)]
                E1n = [sb(px, f"E1n{i}", [128, 8, 128]) for i in range(2)]
                E2 = [sb(px, f"E2{i}", [128, 8, 128]) for i in range(2)]
                gms = [[sb(px, f"gm{u}_{h}", [128, 512], BF16) for h in range(8)] for u in range(2)]
                ncc = [sb(px, f"ncc{i}", [128, 8]) for i in range(2)]
                Ag = [sb(px, f"Ag{k}", [128, 512]) for k in range(2)]
                Tt = [sb(px, f"Tt{k}", [128, 512]) for k in range(4)]
                GA = sb(px, "GA", [128, 512], BF16)
                GAT = [sb(px, f"GAT{k}", [128, 4, 128], BF16) for k in range(2)]
                hf = sb(px, "hf", [128, 1024])

                for i in range(2):
                    act(hf, hf.t[:], xh[i], xh[i].t[:], AF.Square, wr=[ss], accum_out=ss.t[:])
                    rsqrt_col(rstd, ss, 1.0 / 1024, EPS)
                    ts("dve", hsb, hsb.t[:], xh[i], xh[i].t[:], rstd.t[:, 0:1], None, ALU.mult, reads=[rstd])
                    for c in range(8):
                        tr(pTb, pTb.t[:, c * 128:(c + 1) * 128], hsb, hsb.t[:, c * 128:(c + 1) * 128], ident_b)
                    cp("act", xn2T, xn2T.t[:, :, i * 128:(i + 1) * 128], pTb,
                       pTb.t[:].rearrange("p (c n) -> p c n", c=8))
                for blk in range(4):
                    wb = wload(wq_s, wq_s.t[:, :, blk * 512:(blk + 1) * 512], v8)
                    wv = v8(wb.t)
                    for jj in range(4):
                        jb = blk * 4 + jj
                        for c in range(8):
                            mm(pM, pM.t[:, 0:256], wb, wv[:, c, jj * 128:(jj + 1) * 128], xn2T, xn2T.t[:, c, :],
                               start=(c == 0), stop=(c == 7))
                        cp("act" if jb % 2 == 0 else "dve", qT, qT.t[:, jb, :], pM, pM.t[:, 0:256])
                for i in range(2):
                    for b4 in range(4):
                        for jj in range(4):
                            jb = b4 * 4 + jj
                            mm(pM, pM.t[:, jj * 128:(jj + 1) * 128], qT, qT.t[:, jb, i * 128:(i + 1) * 128],
                               KT, KT.t[:, jb, :])
                        cp("act", sc[i], sc[i].t[:, b4 * 512:(b4 + 1) * 512], pM, pM.t[:])
                def topk_body(i, slot):
                    d_ = TK[slot]
                    v16 = d_["v16"]
                    tmp128 = d_["tmp128"]
                    cand = d_["cand"]
                    cand2 = d_["cand2"]
                    c16 = d_["c16"]
                    ex16 = d_["ex16"]
                    negm = d_["negm"]
                    zz = d_["zz"]
                    c24 = d_["c24"]
                    tsum = d_["tsum"]
                    cand3 = d_["cand3"]
                    for h in range(8):
                        for half in range(2):
                            sv = sc[i].t[:, (h * 2 + half) * 128:(h * 2 + half + 1) * 128]
                            vv = v16[half]
                            S.op("dve", lambda e: e.max(out=vv.t[:, 0:8], in_=sv), reads=[sc[i]], writes=[vv])
                            S.op("dve", lambda e: e.match_replace(out=tmp128.t[:], in_to_replace=vv.t[:, 0:8],
                                                                  in_values=sv, imm_value=-1e30),
                                 reads=[sc[i], vv], writes=[tmp128])
                            S.op("dve", lambda e: e.max(out=vv.t[:, 8:16], in_=tmp128.t[:]), reads=[tmp128], writes=[vv])
                        tt("dve", cand, cand.t[:].rearrange("p (a b) -> p a b", a=16),
                           v16[0], v16[0].t[:].unsqueeze(2).to_broadcast([128, 16, 16]),
                           v16[1], v16[1].t[:].unsqueeze(1).to_broadcast([128, 16, 16]), ALU.add)
                        S.op("dve", lambda e: e.max(out=c16.t[:, 0:8], in_=cand.t[:]), reads=[cand], writes=[c16])
                        S.op("dve", lambda e: e.match_replace(out=cand2.t[:], in_to_replace=c16.t[:, 0:8],
                                                              in_values=cand.t[:], imm_value=-1e30),
                             reads=[cand, c16], writes=[cand2])
                        S.op("dve", lambda e: e.max(out=c16.t[:, 8:16], in_=cand2.t[:]), reads=[cand2], writes=[c16])
                        ts("dve", negm, negm.t[:], c16, c16.t[:, 0:1], -1.0, None, ALU.mult)
                        act(ex16, ex16.t[:], c16, c16.t[:], AF.Exp, reads=[negm], wr=[zz], bias=negm.t[:, 0:1],
                            accum_out=zz.t[:])
                        act(zz, zz.t[:], zz, zz.t[:], AF.Ln)
                        tt("dve", ncc[i], ncc[i].t[:, h:h + 1], negm, negm.t[:], zz, zz.t[:], ALU.subtract)
                        S.op("dve", lambda e: e.match_replace(out=cand3.t[:], in_to_replace=c16.t[:, 8:16],
                                                              in_values=cand2.t[:], imm_value=-1e30),
                             reads=[cand2, c16], writes=[cand3])
                        S.op("dve", lambda e: e.max(out=c24.t[:], in_=cand3.t[:]), reads=[cand3], writes=[c24])
                        tt("dve", tsum, tsum.t[:], c16, c16.t[:, 15:16], c24, c24.t[:, 0:1], ALU.add)
                        ts("dve", taum[i], taum[i].t[:, h:h + 1], tsum, tsum.t[:], 0.5, None, ALU.mult)
                S.run_interleaved([lambda: topk_body(0, 0), lambda: topk_body(1, 1)])
                for i in range(2):
                    tt("dve", ttau[i], ttau[i].t[:], taum[i], taum[i].t[:], ncc[i], ncc[i].t[:], ALU.add)
                    act(ttau[i], ttau[i].t[:], ttau[i], ttau[i].t[:], AF.Exp)
                    for h in range(8):
                        act(E1n[i], E1n[i].t[:, h, :], sc[i], sc[i].t[:, (h * 2) * 128:(h * 2 + 1) * 128], AF.Exp,
                            reads=[ncc[i]], bias=ncc[i].t[:, h:h + 1])
                    act(E2[i], E2[i].t[:], sc[i],
                        sc[i].t[:].rearrange("p (h t k) -> p h t k", h=8, t=2)[:, :, 1, :], AF.Exp)
                pu = pU[0]
                pAccs = [pM, pU[1]]
                wts = {}
                cnt = dict(tn=0)

                def stA(n):
                    eb, i = divmod(n, 2)
                    if i == 0:
                        wts[eb] = (wload(dn_s, dn_s.t[:, :, eb * 512:(eb + 1) * 512], v8),
                                   wload(up_s, up_s.t[:, eb * 4:(eb + 1) * 4, :], v4))
                    dnb = wts[eb][0]
                    dv = v8(dnb.t)
                    ag = Ag[n % 2]
                    for c in range(8):
                        mm(pu, pu.t[:], xn2T, xn2T.t[:, c, i * 128:(i + 1) * 128], dnb, dv[:, c, :],
                           start=(c == 0), stop=(c == 7))
                    act(ag, ag.t[:], pu, pu.t[:], AF.Gelu)

                def stT(n, mid=None):
                    eb, i = divmod(n, 2)
                    for h in range(8):
                        if h == 4 and mid is not None:
                            mid()
                        Tb = Tt[cnt["tn"] % 4]
                        cnt["tn"] += 1
                        if h < 2 or h == 7:
                            for q in range(4):
                                act(Tb, Tb.t[:, q * 128:(q + 1) * 128], E2[i], E2[i].t[:, h, :], AF.Copy,
                                    reads=[E1n[i]], scale=E1n[i].t[:, h, eb * 4 + q:eb * 4 + q + 1])
                        else:
                            tt("pool", Tb, Tb.t[:].rearrange("p (a b) -> p a b", a=4),
                               E1n[i], E1n[i].t[:, h, eb * 4:eb * 4 + 4].unsqueeze(2).to_broadcast([128, 4, 128]),
                               E2[i], E2[i].t[:, h, :].unsqueeze(1).to_broadcast([128, 4, 128]), ALU.mult)
                        gm = gms[n % 2][h]
                        stt("dve", gm, gm.t[:], Tb, Tb.t[:], ttau[i].t[:, h:h + 1], Tb, Tb.t[:],
                            ALU.is_ge, ALU.mult, reads=[ttau[i]])

                def stAcc(n):
                    pAcc = pAccs[n % 2]
                    for h in range(8):
                        gm = gms[n % 2][h]
                        mm(pAcc, pAcc.t[:], ident_b, ident_b.t[:], gm, gm.t[:], start=(h == 0), stop=(h == 7))

                def stG(n):
                    pAcc = pAccs[n % 2]
                    ag = Ag[n % 2]
                    gat = GAT[n % 2]
                    tt("dve", GA, GA.t[:], pAcc, pAcc.t[:], ag, ag.t[:], ALU.mult)
                    half = (n % 2) * 512
                    for q in range(4):
                        tr(pTb, pTb.t[:, half + q * 128:half + (q + 1) * 128], GA, GA.t[:, q * 128:(q + 1) * 128], ident_b)
                    cp("act", gat, gat.t[:], pTb, pTb.t[:, half:half + 512].rearrange("p (a b) -> p a b", a=4))

                def stY(n):
                    eb, i = divmod(n, 2)
                    gat = GAT[n % 2]
                    upb = wts[eb][1]
                    uv = v4(upb.t)
                    for q in range(4):
                        for nn in range(2):
                            mm(pY[i][nn], pY[i][nn].t[:], gat, gat.t[:, q, :], upb, uv[:, q, nn * 512:(nn + 1) * 512],
                               start=(eb == 0 and q == 0), stop=(eb == 31 and q == 3))

                NU = 64
                for k in range(NU + 2):
                    if k < NU:
                        stA(k)
                    if 0 <= k - 1 < NU:
                        stAcc(k - 1)
                    if 0 <= k - 2 < NU:
                        stY(k - 2)
                    gfn = (lambda kk=k: stG(kk - 1)) if 0 <= k - 1 < NU else None
                    if k < NU:
                        stT(k, mid=gfn)
                    elif gfn is not None:
                        gfn()
                for i in range(2):
                    for n in range(2):
                        tt("dve", hf, hf.t[:, n * 512:(n + 1) * 512], pY[i][n], pY[i][n].t[:],
                           xh[i], xh[i].t[:, n * 512:(n + 1) * 512], ALU.add)
                    act(yo_p[i], yo_p[i].t[:], hf, hf.t[:], AF.Square, wr=[ss], accum_out=ss.t[:])
                    rsqrt_col(rstd, ss, 1.0 / 1024, EPS)
                    yo = yo_p[i]
                    stt("dve", yo, yo.t[:], hf, hf.t[:], rstd.t[:, 0:1], fnl, fnl.t[:], ALU.mult, ALU.mult, reads=[rstd])
                    S.dma("pool", y_d[t0 + i * 128:t0 + (i + 1) * 128, :], yo.t[:], reads=[yo], writes=[Tn(None)])

        for st in range(NST):
            if stage >= 1:
                mixer(st)
                S.barrier()
            if stage >= 2:
                peer(st)
                S.barrier(switch=((st + 1) % sw_every == 0 and st + 1 < NST))
        S.finish()
        print("ninst", S.ninst, {k: v["count"] for k, v in S.eng.items()})
    return nc


def host_inputs(inp):
    f = lambda a: np.ascontiguousarray(np.asarray(a, dtype=np.float32))
    w_in = np.asarray(inp["w_in"])[0]
    cols = []
    for h in range(8):
        for base in (512, 1536, 2560, 3584):
            cols.append(np.arange(base + h * 128, base + (h + 1) * 128))
    cols.append(np.arange(0, 512))
    cols.append(np.arange(4624, 6672))
    cols.append(np.arange(4608, 4624))
    cols = np.concatenate(cols)
    assert cols.shape[0] == IN_COLS
    r = np.arange(128)
    d = {}
    d["w_in_p"] = f(w_in[:, cols])
    d["mnw"] = f(np.asarray(inp["mix_norm_w"])[0].reshape(8, 128).T)
    d["fnw"] = f(np.asarray(inp["ffn_norm_w"])[0].reshape(8, 128).T)
    d["pool_w_p"] = f(np.asarray(inp["pool_w"])[0].transpose(1, 0, 2))
    d["pool_scale_p"] = f(np.asarray(inp["pool_scale"])[0].reshape(4, 128).T)
    d["conv_w_p"] = f(np.asarray(inp["conv_w"])[0].T.reshape(24, 128, 4).transpose(1, 0, 2))
    d["a_log_p"] = f(np.broadcast_to(np.asarray(inp["a_log"])[0][None, :], (128, 8)))
    d["dt_bias_p"] = f(np.broadcast_to(np.asarray(inp["dt_bias"])[0][None, :], (128, 8)))
    d["dn_norm_p"] = f(np.asarray(inp["dn_norm_w"])[0].reshape(128, 1))
    d["w_pool_up"] = f(np.asarray(inp["w_pool_up"])[0])
    d["w_dn_up"] = f(np.asarray(inp["w_dn_up"])[0])
    d["w_mix_out"] = f(np.asarray(inp["w_mix_out"])[0])
    d["peer_w_query"] = f(np.asarray(inp["peer_w_query"])[0])
    k1 = np.asarray(inp["peer_keys_1"])[0]
    k2 = np.asarray(inp["peer_keys_2"])[0]
    kt = np.stack([k1, k2], axis=1).reshape(16, 128, 128)
    d["keys_t"] = f(kt.transpose(2, 0, 1))
    d["peer_down_t"] = f(np.asarray(inp["peer_down"])[0].T)
    d["peer_up"] = f(np.asarray(inp["peer_up"])[0])
    d["final_w_p"] = f(np.broadcast_to(np.asarray(inp["final_norm_w"])[None, :], (128, 1024)))
    d["c_ident"] = f(np.eye(128))
    d["c_tri"] = f(r[:, None] <= r[None, :])
    d["c_negu"] = f(np.where(r[None, :] >= r[:, None], 0.0, NEG))
    d["c_negls"] = f(np.where(r[:, None] > r[None, :], 0.0, NEG))
    d["c_ones"] = f(np.ones((128, 128)))
    pf = np.ones((4, 16), np.float32)
    for g in range(4):
        w = 2 << g
        for t in range(16):
            pf[g, t] = w / min(t + 1, w)
    d["c_poolfix"] = f(np.broadcast_to(pf[None], (128, 4, 16)))
    return d


_NC_CACHE = {}


def kernel(**inputs):
    x = np.asarray(inputs["x"], dtype=np.float32)
    B, S_TOK, _ = x.shape
    if S_TOK not in _NC_CACHE:
        _NC_CACHE[S_TOK] = build(S_TOK)
    nc = _NC_CACHE[S_TOK]
    shared = host_inputs(inputs)
    in_maps = []
    for b in range(B):
        m = dict(shared)
        m["x"] = np.ascontiguousarray(x[b])
        in_maps.append(m)
    res = run_bass_kernel_spmd(nc, in_maps, core_ids=list(range(B)))
    return np.stack([np.asarray(r["y"]) for r in res.results], axis=0).astype(np.float32)
```

```python
from contextlib import ExitStack
import numpy as np
import concourse.bass as bass
import concourse.mybir as mybir
from concourse.bass_utils import run_bass_kernel_spmd

F32 = mybir.dt.float32
BF16 = mybir.dt.bfloat16
AF = mybir.ActivationFunctionType
ALU = mybir.AluOpType

NEG = -30000.0
EPS = 1e-6
IN_COLS = 6672
NE = 16384


class Buf:
    def __init__(self):
        self.last_w = None
        self.readers = []


class Tn:
    def __init__(self, t, b=None, psum=False):
        self.t = t
        self.b = b if b is not None else Buf()
        self.psum = psum


class Sched:
    CE = ("pe", "act", "dve", "pool")

    def __init__(self, nc, ctx, ndma=8, nsets=2):
        self.nc = nc
        self.eng = {}
        self.sets = [{} for _ in range(nsets)]
        for nm, obj in (("pe", nc.tensor), ("act", nc.scalar), ("dve", nc.vector),
                        ("pool", nc.gpsimd), ("sp", nc.sync)):
            for k in range(nsets):
                if nm != "sp":
                    self.sets[k][nm] = ctx.enter_context(nc.semaphore(f"s{k}_" + nm))
            self.eng[nm] = dict(name=nm, obj=obj, sem=self.sets[0].get(nm), count=0, waited={})
        self.epoch = 0
        self.dq = {}
        for q in ("sp", "pool"):
            sems = [ctx.enter_context(nc.semaphore(f"d_{q}{i}")) for i in range(ndma)]
            self.dq[q] = dict(sems=sems, n=0)
        self.ndma = ndma
        self.ninst = 0
        self.hook = None
        self.nosw = 0

    def _wait(self, e, tok):
        sem, val, ep = tok
        if ep is not None and ep < self.epoch:
            return
        if e["name"] == "pe" and sem is e["sem"]:
            return
        key = id(sem)
        w = e["waited"]
        if w.get(key, 0) >= val:
            return
        e["obj"].wait_ge(sem, val)
        w[key] = val
        self.ninst += 1

    def _deps(self, e, reads, writes):
        for b in reads:
            if b.last_w is not None:
                self._wait(e, b.last_w)
        for b in writes:
            if b.last_w is not None:
                self._wait(e, b.last_w)
            for r in b.readers:
                self._wait(e, r)

    def _commit(self, tok, reads, writes):
        for b in reads:
            b.readers = [r for r in b.readers if r[0] is not tok[0]] + [tok]
        for b in writes:
            b.last_w = tok
            b.readers = []

    def op(self, en, fn, reads=(), writes=()):
        e = self.eng[en]
        rb = [x.b for x in reads]
        wb = [x.b for x in writes] + [x.b for x in reads if x.psum]
        self._deps(e, rb, wb)
        inst = fn(e["obj"])
        e["count"] += 1
        inst.then_inc(e["sem"], 1)
        tok = (e["sem"], e["count"], self.epoch)
        self._commit(tok, rb, wb)
        self.ninst += 1
        if self.hook is not None:
            self.hook()
        return tok

    def dma(self, q, out, in_, reads=(), writes=()):
        e = self.eng[q]
        d = self.dq[q]
        n = d["n"]
        sem = d["sems"][n % self.ndma]
        if n >= self.ndma:
            self._wait(e, (sem, 16 * (n // self.ndma), None))
        rb = [x.b for x in reads]
        wb = [x.b for x in writes]
        self._deps(e, rb, wb)
        e["obj"].dma_start(out=out, in_=in_).then_inc(sem, 16)
        d["n"] = n + 1
        tok = (sem, 16 * (n // self.ndma + 1), None)
        self._commit(tok, rb, wb)
        self.ninst += 1
        return tok

    def run_interleaved(self, fns):
        import threading
        n = len(fns)
        go = [threading.Semaphore(0) for _ in range(n)]
        main = threading.Semaphore(0)
        done = [False] * n
        errs = []
        cur = [0]

        def nxt(i):
            for j in range(i + 1, i + 1 + n):
                if not done[j % n]:
                    return j % n
            return None

        def hook():
            if self.nosw:
                return
            i = cur[0]
            j = nxt(i)
            if j is None or j == i:
                return
            cur[0] = j
            go[j].release()
            go[i].acquire()

        def worker(i):
            go[i].acquire()
            try:
                fns[i]()
            except BaseException as ex:
                errs.append(ex)
            done[i] = True
            j = nxt(i)
            if j is None:
                main.release()
            else:
                cur[0] = j
                go[j].release()

        ths = [threading.Thread(target=worker, args=(i,)) for i in range(n)]
        for t in ths:
            t.start()
        old = self.hook
        self.hook = hook
        cur[0] = 0
        go[0].release()
        main.acquire()
        for t in ths:
            t.join()
        self.hook = old
        if errs:
            raise errs[0]

    def _sync_all(self):
        for a in self.CE + ("sp",):
            for b in self.CE:
                if a != b and self.eng[b]["count"] > 0:
                    self._wait(self.eng[a], (self.eng[b]["sem"], self.eng[b]["count"], self.epoch))

    def barrier(self, switch=False):
        self._sync_all()
        if not switch:
            return
        self.epoch += 1
        new = self.sets[self.epoch]
        for nm, e in self.eng.items():
            e["sem"] = new.get(nm)
            e["count"] = 0

    def finish(self):
        for q, d in self.dq.items():
            e = self.eng[q]
            for i, sem in enumerate(d["sems"]):
                cnt = (d["n"] - i + self.ndma - 1) // self.ndma
                if cnt > 0:
                    e["obj"].wait_ge(sem, 16 * cnt)


def build(S_TOK, dbg=False, stage=99, sw_every=2):
    NST = S_TOK // 256
    nc = bass.Bass("TRN2", target_bir_lowering=False)

    def din(name, shape):
        return nc.dram_tensor(name, list(shape), F32, kind="ExternalInput").ap()

    x_d = din("x", [S_TOK, 1024])
    win_d = din("w_in_p", [1024, IN_COLS])
    mnw_d = din("mnw", [128, 8])
    fnw_d = din("fnw", [128, 8])
    pw_d = din("pool_w_p", [128, 4, 128])
    psc_d = din("pool_scale_p", [128, 4])
    cw_d = din("conv_w_p", [128, 24, 4])
    alog_d = din("a_log_p", [128, 8])
    dtb_d = din("dt_bias_p", [128, 8])
    dnw_d = din("dn_norm_p", [128, 1])
    wpu_d = din("w_pool_up", [512, 1024])
    wdu_d = din("w_dn_up", [1024, 1024])
    wo_d = din("w_mix_out", [1024, 1024])
    wq_d = din("peer_w_query", [1024, 2048])
    kt_d = din("keys_t", [128, 16, 128])
    dnT_d = din("peer_down_t", [1024, NE])
    up_d = din("peer_up", [NE, 1024])
    fnl_d = din("final_w_p", [128, 1024])
    ident_d = din("c_ident", [128, 128])
    tri_d = din("c_tri", [128, 128])
    negu_d = din("c_negu", [128, 128])
    negls_d = din("c_negls", [128, 128])
    ones_d = din("c_ones", [128, 128])
    pfix_d = din("c_poolfix", [128, 4, 16])
    y_d = nc.dram_tensor("y", [S_TOK, 1024], F32, kind="ExternalOutput").ap()
    if dbg:
        h1_d = nc.dram_tensor("h1dbg", [S_TOK, 1024], F32, kind="ExternalOutput").ap()

    win_s = Tn(nc.dram_tensor("win_s", [128, 8, IN_COLS], BF16).ap())
    wq_s = Tn(nc.dram_tensor("wq_s", [128, 8, 2048], BF16).ap())
    dn_s = Tn(nc.dram_tensor("dn_s", [128, 8, NE], BF16).ap())
    up_s = Tn(nc.dram_tensor("up_s", [128, 128, 1024], BF16).ap())

    with ExitStack() as ctx:
        NEP = (NST + sw_every - 1) // sw_every
        S = Sched(nc, ctx, nsets=NEP + 1)

        uid = [0]

        def un_(name):
            uid[0] += 1
            return f"t{uid[0]}_{name}"

        def sb(cx, name, shape, dt=F32):
            return Tn(cx.enter_context(nc.sbuf_tensor(un_(name), list(shape), dt)))

        def ps(cx, name, shape, dt=F32):
            return Tn(cx.enter_context(nc.psum_tensor(un_(name), list(shape), dt)), psum=True)

        def mm(o, o_ap, l, l_ap, r, r_ap, start=True, stop=True):
            S.op("pe", lambda e: e.matmul(o_ap, lhsT=l_ap, rhs=r_ap, start=start, stop=stop),
                 reads=[l, r], writes=[o])

        def tr(o, o_ap, i, i_ap, idn):
            S.op("pe", lambda e: e.transpose(out=o_ap, in_=i_ap, identity=idn.t[:]),
                 reads=[i, idn], writes=[o])

        def act(o, o_ap, i, i_ap, func, reads=(), wr=(), **kw):
            S.op("act", lambda e: e.activation(out=o_ap, in_=i_ap, func=func, **kw),
                 reads=[i] + list(reads), writes=[o] + list(wr))

        def ts(en, o, o_ap, i, i_ap, s1, s2, op0, op1=None, reads=()):
            if op1 is None:
                S.op(en, lambda e: e.tensor_scalar(out=o_ap, in0=i_ap, scalar1=s1, scalar2=None, op0=op0),
                     reads=[i] + list(reads), writes=[o])
            else:
                S.op(en, lambda e: e.tensor_scalar(out=o_ap, in0=i_ap, scalar1=s1, scalar2=s2, op0=op0, op1=op1),
                     reads=[i] + list(reads), writes=[o])

        def tt(en, o, o_ap, a, a_ap, b, b_ap, op):
            S.op(en, lambda e: e.tensor_tensor(out=o_ap, in0=a_ap, in1=b_ap, op=op),
                 reads=[a, b], writes=[o])

        def stt(en, o, o_ap, a, a_ap, sc, b, b_ap, op0, op1, reads=()):
            S.op(en, lambda e: e.scalar_tensor_tensor(out=o_ap, in0=a_ap, scalar=sc, in1=b_ap, op0=op0, op1=op1),
                 reads=[a, b] + list(reads), writes=[o])

        def cp(en, o, o_ap, i, i_ap):
            if en == "act":
                S.op("act", lambda e: e.copy(out=o_ap, in_=i_ap), reads=[i], writes=[o])
            else:
                S.op(en, lambda e: e.tensor_copy(out=o_ap, in_=i_ap), reads=[i], writes=[o])

        def rsqrt_col(o, i, scale, eps, reads=()):
            ts("dve", o, o.t[:], i, i.t[:], scale, eps, ALU.mult, ALU.add)
            act(o, o.t[:], o, o.t[:], AF.Sqrt)
            S.op("dve", lambda e: e.reciprocal(out=o.t[:], in_=o.t[:]), reads=[o], writes=[o])

        ident_f = sb(ctx, "ident_f", [128, 128])
        ident_b = sb(ctx, "ident_b", [128, 128], BF16)
        tri = sb(ctx, "tri", [128, 128])
        negu = sb(ctx, "negu", [128, 128])
        negls = sb(ctx, "negls", [128, 128])
        ones = sb(ctx, "ones", [128, 128])
        pfix = sb(ctx, "pfix", [128, 4, 16])
        mnw = sb(ctx, "mnw", [128, 8])
        fnw = sb(ctx, "fnw", [128, 8])
        psc = sb(ctx, "psc", [128, 4])
        cw = sb(ctx, "cw", [128, 24, 4])
        nA = sb(ctx, "nA", [128, 8])
        dtb = sb(ctx, "dtb", [128, 8])
        dnw = sb(ctx, "dnw", [128, 1])
        fnl = sb(ctx, "fnl", [128, 1024])
        wpu = sb(ctx, "wpu", [128, 4, 1024], BF16)
        wdu = sb(ctx, "wdu", [128, 8, 1024], BF16)
        wo = sb(ctx, "wo", [128, 8, 1024], BF16)
        pw = sb(ctx, "pw", [128, 4, 128], BF16)
        wsm = sb(ctx, "wsm", [128, 8, 16], BF16)
        KT = sb(ctx, "KT", [128, 16, 128], BF16)
        wpool = [sb(ctx, f"wpool{i}", [128, 4096], BF16) for i in range(5)]
        wp_n = [0]
        Sst = [sb(ctx, f"Sst{h}", [128, 128]) for h in range(8)]
        ccar = [sb(ctx, f"ccar{b}", [128, 3]) for b in range(24)]
        xab = [sb(ctx, f"xab{g}", [128, 272]) for g in range(4)]
        xh = [sb(ctx, f"xh{i}", [128, 1024]) for i in range(2)]
        yo_p = [sb(ctx, f"yo{i}", [128, 1024]) for i in range(2)]

        def wload(src, src_ap, view):
            t = wpool[wp_n[0] % len(wpool)]
            wp_n[0] += 1
            S.dma("sp", view(t.t), src_ap, reads=[src], writes=[t])
            return t

        v8 = lambda t: t[:].rearrange("p (c n) -> p c n", c=8)
        v4 = lambda t: t[:].rearrange("p (c n) -> p c n", c=4)

        dummy = Tn(None)
        for (t, d) in ((ident_f, ident_d), (tri, tri_d), (negu, negu_d), (negls, negls_d), (ones, ones_d),
                       (pfix, pfix_d), (mnw, mnw_d), (fnw, fnw_d), (psc, psc_d), (cw, cw_d), (nA, alog_d),
                       (dtb, dtb_d), (dnw, dnw_d), (fnl, fnl_d)):
            S.dma("sp", t.t[:], d, writes=[t])
        cp("dve", ident_b, ident_b.t[:], ident_f, ident_f.t[:])
        act(nA, nA.t[:], nA, nA.t[:], AF.Exp)
        ts("dve", nA, nA.t[:], nA, nA.t[:], -1.0, None, ALU.mult)
        for h in range(8):
            S.op("dve", lambda e: e.memset(Sst[h].t[:], 0.0), writes=[Sst[h]])
        for b in range(24):
            S.op("pool", lambda e: e.memset(ccar[b].t[:], 0.0), writes=[ccar[b]])
        for g in range(4):
            S.op("pool", lambda e: e.memset(xab[g].t[:], 0.0), writes=[xab[g]])

        with ExitStack() as pc:
            stg = [sb(pc, f"stg{i}", [128, 4096]) for i in range(2)]
            sn = [0]

            def prep(src_ap, shape3, dst, dst_ap, scale_t=None, scale_ap=None):
                a, b = shape3
                st_ = stg[sn[0] % 2]
                sn[0] += 1
                sv = st_.t[:, 0:a * b].rearrange("p (a b) -> p a b", a=a)
                S.dma("sp", sv, src_ap, writes=[st_])
                ob = wpool[wp_n[0] % len(wpool)]
                wp_n[0] += 1
                ov = ob.t[:, 0:a * b].rearrange("p (a b) -> p a b", a=a)
                en = "dve" if sn[0] % 2 == 0 else "pool"
                if scale_t is None:
                    cp("act" if sn[0] % 2 == 0 else "dve", ob, ov, st_, sv)
                else:
                    tt(en, ob, ov, st_, sv, scale_t, scale_ap, ALU.mult)
                if dst is None:
                    return ob, ov
                S.dma("pool", dst_ap, ov, reads=[ob], writes=[dst])
                return ob, ov

            for (wt, d, a, b, rs) in ((wpu, wpu_d, 4, 1024, "(g p) n -> p g n"),
                                      (wo, wo_d, 8, 1024, "(c p) n -> p c n")):
                for half in range(a // 4):
                    ob, ov = prep(d.rearrange(rs, p=128)[:, half * 4:(half + 1) * 4, :], (4, 1024), None, None)
                    cp("dve", wt, wt.t[:, half * 4:(half + 1) * 4, :], ob, ov)
            for half in range(2):
                ob, ov = prep(wdu_d.rearrange("(h p) n -> p h n", p=128)[:, half * 4:(half + 1) * 4, :], (4, 1024),
                              None, None, dnw, dnw.t[:, 0:1].unsqueeze(2).to_broadcast([128, 4, 1024]))
                cp("dve", wdu, wdu.t[:, half * 4:(half + 1) * 4, :], ob, ov)
            ob, ov = prep(pw_d, (4, 128), None, None)
            cp("dve", pw, pw.t[:], ob, ov)
            ob, ov = prep(kt_d, (16, 128), None, None)
            cp("dve", KT, KT.t[:], ob, ov)
            winv = win_d.rearrange("(c p) n -> p c n", p=128)
            for blk in range(14):
                c0 = blk * 512
                n = min(512, IN_COLS - c0)
                ob, ov = prep(winv[:, :, c0:c0 + n], (8, n), win_s, win_s.t[:, :, c0:c0 + n],
                              mnw, mnw.t[:].unsqueeze(2).to_broadcast([128, 8, n]))
                if blk == 13:
                    cp("dve", wsm, wsm.t[:], ob, ov)
            wqv = wq_d.rearrange("(c p) n -> p c n", p=128)
            for blk in range(4):
                prep(wqv[:, :, blk * 512:(blk + 1) * 512], (8, 512), wq_s, wq_s.t[:, :, blk * 512:(blk + 1) * 512],
                     fnw, fnw.t[:].unsqueeze(2).to_broadcast([128, 8, 512]))
            dnv = dnT_d.rearrange("(c p) e -> p c e", p=128)
            upv = up_d.rearrange("(ch p) d -> p ch d", p=128)
            for blk in range(32 if stage >= 2 else 0):
                prep(dnv[:, :, blk * 512:(blk + 1) * 512], (8, 512), dn_s, dn_s.t[:, :, blk * 512:(blk + 1) * 512],
                     fnw, fnw.t[:].unsqueeze(2).to_broadcast([128, 8, 512]))
                prep(upv[:, blk * 4:(blk + 1) * 4, :], (4, 1024), up_s, up_s.t[:, blk * 4:(blk + 1) * 4, :])
        S.barrier(switch=True)

        def mixer(st):
            t0 = st * 256
            with ExitStack() as mx:
                pTb = ps(mx, "pTb", [128, 1024], BF16)
                big = [ps(mx, f"big{k}", [128, 512]) for k in range(4)]
                bn = [0]
                qbank = [mx.enter_context(nc.psum_tensor(un_(f"qb{k}"), [128, 512], F32)) for k in range(3)]
                qsl = [Tn(qbank[k], psum=True) for k in range(3)]
                qn = [0]

                def nbig():
                    b = big[bn[0] % 4]
                    bn[0] += 1
                    return b

                def nq():
                    k = qn[0] % 12
                    qn[0] += 1
                    s = qsl[k % 3]
                    qq = k // 3
                    return s, s.t[:, qq * 128:(qq + 1) * 128]

                sq = sb(mx, "sq", [128, 1024])
                xsb = sb(mx, "xsb", [128, 1024], BF16)
                xT = sb(mx, "xT", [128, 8, 256], BF16)
                ss = sb(mx, "ss", [128, 1])
                rstd = sb(mx, "rstd", [128, 1])
                def mk_slot():
                    d_ = {}
                    d_["raw"] = [sb(mx, f"raw{k}", [128, 259]) for k in range(3)]
                    d_["cacc"] = sb(mx, "cacc", [128, 256])
                    d_["qkvT"] = [sb(mx, f"qkvT{k}", [128, 256]) for k in range(3)]
                    d_["szT"] = sb(mx, "szT", [128, 256])
                    d_["sqn"] = sb(mx, "sqn", [128, 256])
                    d_["rn"] = sb(mx, "rn", [128, 256])
                    d_["kbg"] = sb(mx, "kbg", [128, 128])
                    d_["kdec"] = sb(mx, "kdec", [128, 128])
                    d_["vb"] = sb(mx, "vb", [128, 128])
                    d_["trig"] = sb(mx, "trig", [128, 128])
                    d_["Am"] = sb(mx, "Am", [128, 128])
                    d_["EU"] = sb(mx, "EU", [128, 128])
                    d_["DLs"] = sb(mx, "DLs", [128, 128])
                    d_["egrow"] = sb(mx, "egrow", [128, 128])
                    d_["Mb"] = [sb(mx, f"Mb{k}", [128, 128]) for k in range(2)]
                    d_["MTb"] = [sb(mx, f"MTb{k}", [128, 128]) for k in range(2)]
                    d_["PTb"] = [sb(mx, f"PTb{k}", [128, 128]) for k in range(2)]
                    d_["attnT"] = sb(mx, "attnT", [128, 128])
                    d_["qgT"] = sb(mx, "qgT", [128, 128])
                    d_["nwT"] = sb(mx, "nwT", [128, 128])
                    d_["vn"] = sb(mx, "vn", [128, 128])
                    d_["oss"] = sb(mx, "oss", [128, 1])
                    d_["orst"] = sb(mx, "orst", [128, 1])
                    d_["osq"] = sb(mx, "osq", [128, 128])
                    d_["onb"] = sb(mx, "onb", [128, 128], BF16)
                    return d_
                SL = [mk_slot(), mk_slot()]
                lg = [sb(mx, f"lg{i}", [128, 16]) for i in range(2)]
                beta = [sb(mx, f"beta{i}", [128, 8]) for i in range(2)]
                nbeta = [sb(mx, f"nbeta{i}", [128, 8]) for i in range(2)]
                gg = [sb(mx, f"gg{i}", [128, 8]) for i in range(2)]
                gc = [sb(mx, f"gc{i}", [128, 8]) for i in range(2)]
                egl = [sb(mx, f"egl{i}", [128, 8]) for i in range(2)]
                kds = [sb(mx, f"kds{i}", [128, 8]) for i in range(2)]
                bgs = [sb(mx, f"bgs{i}", [128, 8]) for i in range(2)]
                onT = sb(mx, "onT", [128, 8, 256], BF16)
                pa4 = [[sb(mx, f"pa{g}_{k}", [128, 272]) for k in range(2)] for g in range(4)]
                pooledb4 = [sb(mx, f"pooledb{g}", [128, 256], BF16) for g in range(4)]
                paT = sb(mx, "paT", [128, 4, 256], BF16)
                sga = sb(mx, "sga", [128, 256])
                m1 = sb(mx, "m1", [128, 256])
                sgb = sb(mx, "sgb", [128, 256])
                m2 = sb(mx, "m2", [128, 256])
                mT = sb(mx, "mT", [128, 8, 256], BF16)

                for i in range(2):
                    S.dma("sp", xh[i].t[:], x_d[t0 + i * 128:t0 + (i + 1) * 128, :], writes=[xh[i]])
                    act(sq, sq.t[:], xh[i], xh[i].t[:], AF.Square, wr=[ss], accum_out=ss.t[:])
                    rsqrt_col(rstd, ss, 1.0 / 1024, EPS)
                    ts("dve", xsb, xsb.t[:], xh[i], xh[i].t[:], rstd.t[:, 0:1], None, ALU.mult, reads=[rstd])
                    for c in range(8):
                        tr(pTb, pTb.t[:, c * 128:(c + 1) * 128], xsb, xsb.t[:, c * 128:(c + 1) * 128], ident_b)
                    cp("act", xT, xT.t[:, :, i * 128:(i + 1) * 128], pTb,
                       pTb.t[:].rearrange("p (c n) -> p c n", c=8))

                def gate_body(i):
                    s_, s_ap = nq()
                    S.nosw += 1
                    for c in range(8):
                        if c == 7:
                            S.nosw -= 1
                        mm(s_, s_ap[:, 0:16], xT, xT.t[:, c, i * 128:(i + 1) * 128], wsm, wsm.t[:, c, :],
                           start=(c == 0), stop=(c == 7))
                    cp("dve", lg[i], lg[i].t[:], s_, s_ap[:, 0:16])
                    act(beta[i], beta[i].t[:], lg[i], lg[i].t[:, 0:8], AF.Sigmoid)
                    ts("dve", nbeta[i], nbeta[i].t[:], beta[i], beta[i].t[:], -1.0, None, ALU.mult)
                    tt("dve", gg[i], gg[i].t[:], lg[i], lg[i].t[:, 8:16], dtb, dtb.t[:], ALU.add)
                    act(gg[i], gg[i].t[:], gg[i], gg[i].t[:], AF.Exp)
                    act(gg[i], gg[i].t[:], gg[i], gg[i].t[:], AF.Ln, bias=1.0)
                    tt("dve", gg[i], gg[i].t[:], gg[i], gg[i].t[:], nA, nA.t[:], ALU.mult)
                    s_, s_ap = nq()
                    mm(s_, s_ap[:, 0:8], tri, tri.t[:], gg[i], gg[i].t[:])
                    cp("dve", gc[i], gc[i].t[:], s_, s_ap[:, 0:8])
                    s_, s_ap = nq()
                    mm(s_, s_ap[:, 0:8], ones, ones.t[:], gg[i], gg[i].t[:])
                    tt("dve", kds[i], kds[i].t[:], s_, s_ap[:, 0:8], gc[i], gc[i].t[:], ALU.subtract)
                    act(kds[i], kds[i].t[:], kds[i], kds[i].t[:], AF.Exp)
                    act(egl[i], egl[i].t[:], s_, s_ap[:, 0:8], AF.Exp)
                    act(bgs[i], bgs[i].t[:], gc[i], gc[i].t[:], AF.Exp)
                    tt("dve", bgs[i], bgs[i].t[:], bgs[i], bgs[i].t[:], beta[i], beta[i].t[:], ALU.mult)

                S.run_interleaved([lambda: gate_body(0), lambda: gate_body(1)])

                def head_body(h, slot):
                    d_ = SL[slot]
                    raw = d_["raw"]
                    cacc = d_["cacc"]
                    qkvT = d_["qkvT"]
                    szT = d_["szT"]
                    sqn = d_["sqn"]
                    rn = d_["rn"]
                    kbg = d_["kbg"]
                    kdec = d_["kdec"]
                    vb = d_["vb"]
                    trig = d_["trig"]
                    Am = d_["Am"]
                    EU = d_["EU"]
                    DLs = d_["DLs"]
                    egrow = d_["egrow"]
                    Mb = d_["Mb"]
                    MTb = d_["MTb"]
                    PTb = d_["PTb"]
                    attnT = d_["attnT"]
                    qgT = d_["qgT"]
                    nwT = d_["nwT"]
                    vn = d_["vn"]
                    oss = d_["oss"]
                    orst = d_["orst"]
                    osq = d_["osq"]
                    onb = d_["onb"]
                    wb = wload(win_s, win_s.t[:, :, h * 512:(h + 1) * 512], v8)
                    wv = v8(wb.t)
                    for k in range(4):
                        pb = nbig()
                        for c in range(8):
                            mm(pb, pb.t[:, 0:256], wb, wv[:, c, k * 128:(k + 1) * 128], xT, xT.t[:, c, :],
                               start=(c == 0), stop=(c == 7))
                        if k == 3:
                            act(szT, szT.t[:], pb, pb.t[:, 0:256], AF.Silu)
                            continue
                        blk = k * 8 + h
                        r = raw[k]
                        cp("pool", r, r.t[:, 0:3], ccar[blk], ccar[blk].t[:])
                        cp("act", r, r.t[:, 3:259], pb, pb.t[:, 0:256])
                        cp("pool", ccar[blk], ccar[blk].t[:], r, r.t[:, 256:259])
                        ts("dve", cacc, cacc.t[:], r, r.t[:, 0:256], cw.t[:, blk, 0:1], None, ALU.mult, reads=[cw])
                        for j in range(1, 4):
                            stt("dve", cacc, cacc.t[:], r, r.t[:, j:j + 256], cw.t[:, blk, j:j + 1], cacc, cacc.t[:],
                                ALU.mult, ALU.add, reads=[cw])
                        act(qkvT[k], qkvT[k].t[:], cacc, cacc.t[:], AF.Silu)
                        if k < 2:
                            tt("dve", sqn, sqn.t[:], qkvT[k], qkvT[k].t[:], qkvT[k], qkvT[k].t[:], ALU.mult)
                            pn = nbig()
                            mm(pn, pn.t[:, 0:256], ones, ones.t[:], sqn, sqn.t[:])
                            ts("dve", rn, rn.t[:], pn, pn.t[:, 0:256], EPS, None, ALU.add)
                            act(rn, rn.t[:], rn, rn.t[:], AF.Sqrt)
                            S.op("dve", lambda e: e.reciprocal(out=rn.t[:], in_=rn.t[:]), reads=[rn], writes=[rn])
                            sc = (128.0 ** -0.5) if k == 0 else 1.0
                            stt("dve", qkvT[k], qkvT[k].t[:], qkvT[k], qkvT[k].t[:], sc, rn, rn.t[:],
                                ALU.mult, ALU.mult)
                    qT_, kT_, vT_ = qkvT
                    for i in range(2):
                        sl = slice(i * 128, (i + 1) * 128)
                        hh = slice(h, h + 1)
                        k_s, k_ap = nq()
                        tr(k_s, k_ap, kT_, kT_.t[:, sl], ident_f)
                        v_s, v_ap = nq()
                        tr(v_s, v_ap, vT_, vT_.t[:, sl], ident_f)
                        g_s, g_ap = nq()
                        mm(g_s, g_ap, kT_, kT_.t[:, sl], kT_, kT_.t[:, sl])
                        a_s, a_ap = nq()
                        mm(a_s, a_ap, kT_, kT_.t[:, sl], qT_, qT_.t[:, sl])
                        ts("dve", kbg, kbg.t[:], k_s, k_ap, bgs[i].t[:, hh], None, ALU.mult, reads=[bgs[i]])
                        act(kdec, kdec.t[:], k_s, k_ap, AF.Copy, reads=[kds[i]], scale=kds[i].t[:, hh])
                        act(vb, vb.t[:], v_s, v_ap, AF.Copy, reads=[beta[i]], scale=beta[i].t[:, hh])
                        ts("pool", trig, trig.t[:], tri, tri.t[:], gg[i].t[:, hh], None, ALU.mult, reads=[gg[i]])
                        r_s, r_ap = nq()
                        mm(r_s, r_ap, ones, ones.t[:], trig, trig.t[:])
                        ts("dve", Am, Am.t[:], r_s, r_ap, gc[i].t[:, hh], None, ALU.subtract, reads=[gc[i]])
                        act(egrow, egrow.t[:], r_s, r_ap, AF.Exp)
                        tt("pool", EU, EU.t[:], Am, Am.t[:], negu, negu.t[:], ALU.add)
                        act(EU, EU.t[:], EU, EU.t[:], AF.Exp)
                        tt("pool", DLs, DLs.t[:], negls, negls.t[:], Am, Am.t[:], ALU.subtract)
                        act(DLs, DLs.t[:], DLs, DLs.t[:], AF.Exp)
                        M, MT, PT = Mb[0], MTb[0], PTb[0]
                        stt("dve", M, M.t[:], g_s, g_ap, nbeta[i].t[:, hh], DLs, DLs.t[:], ALU.mult, ALU.mult,
                            reads=[nbeta[i]])
                        tt("dve", attnT, attnT.t[:], a_s, a_ap, EU, EU.t[:], ALU.mult)
                        tt("pool", qgT, qgT.t[:], qT_, qT_.t[:, sl], egrow, egrow.t[:], ALU.mult)
                        t_s, t_ap = nq()
                        tr(t_s, t_ap, M, M.t[:], ident_f)
                        cp("act", MT, MT.t[:], t_s, t_ap)
                        tt("dve", PT, PT.t[:], t_s, t_ap, ident_f, ident_f.t[:], ALU.add)
                        for kk in range(6):
                            Mn, MTn, PTn = Mb[(kk + 1) % 2], MTb[(kk + 1) % 2], PTb[(kk + 1) % 2]
                            s1, s1ap = nq()
                            mm(s1, s1ap, MT, MT.t[:], M, M.t[:])
                            cp("act", Mn, Mn.t[:], s1, s1ap)
                            if kk < 5:
                                s2, s2ap = nq()
                                mm(s2, s2ap, M, M.t[:], MT, MT.t[:])
                                cp("dve", MTn, MTn.t[:], s2, s2ap)
                            s3, s3ap = nq()
                            mm(s3, s3ap, Mn, Mn.t[:], PT, PT.t[:])
                            tt("dve", PTn, PTn.t[:], s3, s3ap, PT, PT.t[:], ALU.add)
                            M, MT, PT = Mn, MTn, PTn
                        w_s, w_ap = nq()
                        mm(w_s, w_ap, kbg, kbg.t[:], PT, PT.t[:])
                        S.op("act", lambda e: e.mul(out=nwT.t[:], in_=w_ap, mul=-1.0), reads=[w_s], writes=[nwT])
                        n_s, n_ap = nq()
                        S.nosw += 1
                        mm(n_s, n_ap, PT, PT.t[:], vb, vb.t[:], start=True, stop=False)
                        S.nosw -= 1
                        mm(n_s, n_ap, nwT, nwT.t[:], Sst[h], Sst[h].t[:], start=False, stop=True)
                        cp("dve", vn, vn.t[:], n_s, n_ap)
                        o_s, o_ap = nq()
                        S.nosw += 1
                        mm(o_s, o_ap, qgT, qgT.t[:], Sst[h], Sst[h].t[:], start=True, stop=False)
                        S.nosw -= 1
                        mm(o_s, o_ap, attnT, attnT.t[:], vn, vn.t[:], start=False, stop=True)
                        u_s, u_ap = nq()
                        mm(u_s, u_ap, kdec, kdec.t[:], vn, vn.t[:])
                        stt("dve", Sst[h], Sst[h].t[:], Sst[h], Sst[h].t[:], egl[i].t[:, hh], u_s, u_ap,
                            ALU.mult, ALU.add, reads=[egl[i]])
                        act(osq, osq.t[:], o_s, o_ap, AF.Square, wr=[oss], accum_out=oss.t[:])
                        rsqrt_col(orst, oss, 1.0 / 128, EPS)
                        act(onb, onb.t[:], o_s, o_ap, AF.Copy, reads=[orst], scale=orst.t[:, 0:1])
                        pslot = pTb.t[:, (h % 8) * 128:(h % 8 + 1) * 128]
                        tr(pTb, pslot, onb, onb.t[:], ident_b)
                        tt("dve", onT, onT.t[:, h, sl], pTb, pslot, szT, szT.t[:, sl], ALU.mult)

                for hp in range(0, 8, 2):
                    S.run_interleaved([lambda hp=hp: head_body(hp, 0), lambda hp=hp: head_body(hp + 1, 1)])

                wb = wload(win_s, win_s.t[:, :, 4096:4608], v8)
                wv = v8(wb.t)
                def pool_body(g):
                    pa = pa4[g]
                    pooledb = pooledb4[g]
                    win_ = 2 << g
                    pb = nbig()
                    S.nosw += 1
                    for c in range(8):
                        if c == 7:
                            S.nosw -= 1
                        mm(pb, pb.t[:, 0:256], wb, wv[:, c, g * 128:(g + 1) * 128], xT, xT.t[:, c, :],
                           start=(c == 0), stop=(c == 7))
                    xg = xab[g]
                    cp("pool", xg, xg.t[:, 0:16], xg, xg.t[:, 256:272])
                    cp("act", xg, xg.t[:, 16:272], pb, pb.t[:, 0:256])
                    src = xg
                    sh = 1
                    for stp in range(g + 1):
                        dst = pa[stp % 2]
                        lo = 2 * sh - 1
                        tt("dve", dst, dst.t[:, lo:272], src, src.t[:, lo:272], src, src.t[:, lo - sh:272 - sh], ALU.add)
                        src = dst
                        sh *= 2
                    if st == 0:
                        tt("dve", src, src.t[:, 16:32], src, src.t[:, 16:32], pfix, pfix.t[:, g, :], ALU.mult)
                    stt("dve", pooledb, pooledb.t[:], src, src.t[:, 16:272], 1.0 / win_, xg, xg.t[:, 16:272],
                        ALU.mult, ALU.subtract)
                    pb2 = nbig()
                    mm(pb2, pb2.t[:, 0:256], pw, pw.t[:, g, :], pooledb, pooledb.t[:])
                    act(paT, paT.t[:, g, :], pb2, pb2.t[:, 0:256], AF.Copy, reads=[psc], scale=psc.t[:, g:g + 1])

                S.run_interleaved([lambda g=g: pool_body(g) for g in range(4)])

                wga = [wload(win_s, win_s.t[:, :, 4608 + k * 512:4608 + (k + 1) * 512], v8) for k in range(2)]
                wgb = [wload(win_s, win_s.t[:, :, 5632 + k * 512:5632 + (k + 1) * 512], v8) for k in range(2)]
                for j in range(8):
                    js = slice(j * 128, (j + 1) * 128)
                    jw = slice((j % 4) * 128, (j % 4 + 1) * 128)
                    pA, pB, pC, pD = big[0], big[1], big[2], big[3]
                    for g in range(4):
                        mm(pA, pA.t[:, 0:256], wpu, wpu.t[:, g, js], paT, paT.t[:, g, :], start=(g == 0), stop=(g == 3))
                    wa = wga[j // 4]
                    for c in range(8):
                        mm(pB, pB.t[:, 0:256], wa, v8(wa.t)[:, c, jw], xT, xT.t[:, c, :], start=(c == 0), stop=(c == 7))
                    for hd in range(8):
                        mm(pC, pC.t[:, 0:256], wdu, wdu.t[:, hd, js], onT, onT.t[:, hd, :], start=(hd == 0), stop=(hd == 7))
                    wg = wgb[j // 4]
                    for c in range(8):
                        mm(pD, pD.t[:, 0:256], wg, v8(wg.t)[:, c, jw], xT, xT.t[:, c, :], start=(c == 0), stop=(c == 7))
                    act(sga, sga.t[:], pB, pB.t[:, 0:256], AF.Sigmoid)
                    tt("dve", m1, m1.t[:], pA, pA.t[:, 0:256], sga, sga.t[:], ALU.mult)
                    act(sgb, sgb.t[:], pD, pD.t[:, 0:256], AF.Sigmoid)
                    tt("dve", m2, m2.t[:], pC, pC.t[:, 0:256], sgb, sgb.t[:], ALU.mult)
                    tt("pool", mT, mT.t[:, j, :], m1, m1.t[:], m2, m2.t[:], ALU.add)

                for i in range(2):
                    for n in range(2):
                        pb = nbig()
                        for k in range(8):
                            mm(pb, pb.t[:], mT, mT.t[:, k, i * 128:(i + 1) * 128], wo, wo.t[:, k, n * 512:(n + 1) * 512],
                               start=(k == 0), stop=(k == 7))
                        tt("dve", xh[i], xh[i].t[:, n * 512:(n + 1) * 512], pb, pb.t[:],
                           xh[i], xh[i].t[:, n * 512:(n + 1) * 512], ALU.add)
                    if dbg:
                        S.dma("pool", h1_d[t0 + i * 128:t0 + (i + 1) * 128, :], xh[i].t[:], reads=[xh[i]], writes=[Tn(None)])

        def peer(st):
            t0 = st * 256
            with ExitStack() as px:
                pY = [[ps(px, f"pY{i}{n}", [128, 512]) for n in range(2)] for i in range(2)]
                pU = [ps(px, f"pU{k}", [128, 512]) for k in range(2)]
                pTb = ps(px, "pTbp", [128, 1024], BF16)
                pM = ps(px, "pM", [128, 512])
                hsb = sb(px, "hsb", [128, 1024], BF16)
                xn2T = sb(px, "xn2T", [128, 8, 256], BF16)
                ss = sb(px, "pss", [128, 1])
                rstd = sb(px, "prstd", [128, 1])
                qT = sb(px, "pqT", [128, 16, 256], BF16)
                sc = [sb(px, f"sc{i}", [128, 2048]) for i in range(2)]
                def mk_tk():
                    d_ = {}
                    d_["v16"] = [sb(px, f"v16{k}", [128, 16]) for k in range(2)]
                    d_["tmp128"] = sb(px, "tmp128", [128, 128])
                    d_["cand"] = sb(px, "cand", [128, 256])
                    d_["cand2"] = sb(px, "cand2", [128, 256])
                    d_["c16"] = sb(px, "c16", [128, 16])
                    d_["ex16"] = sb(px, "ex16", [128, 16])
                    d_["negm"] = sb(px, "negm", [128, 1])
                    d_["zz"] = sb(px, "zz", [128, 1])
                    d_["c24"] = sb(px, "c24", [128, 8])
                    d_["tsum"] = sb(px, "tsum", [128, 1])
                    d_["cand3"] = d_["cand"]
                    return d_
                TK = [mk_tk(), mk_tk()]
                taum = [sb(px, f"taum{i}", [128, 8]) for i in range(2)]
                ttau = [sb(px, f"ttau{i}", [128, 8]) for i in range(2)]
                E1n = [sb(px, f"E1n{i}", [128, 8, 128]) for i in range(2)]
                E2 = [sb(px, f"E2{i}", [128, 8, 128]) for i in range(2)]
                gms = [[sb(px, f"gm{u}_{h}", [128, 512], BF16) for h in range(8)] for u in range(2)]
                ncc = [sb(px, f"ncc{i}", [128, 8]) for i in range(2)]
                Ag = [sb(px, f"Ag{k}", [128, 512]) for k in range(2)]
                Tt = [sb(px, f"Tt{k}", [128, 512]) for k in range(4)]
                GA = sb(px, "GA", [128, 512], BF16)
                GAT = [sb(px, f"GAT{k}", [128, 4, 128], BF16) for k in range(2)]
                hf = sb(px, "hf", [128, 1024])

                for i in range(2):
                    act(hf, hf.t[:], xh[i], xh[i].t[:], AF.Square, wr=[ss], accum_out=ss.t[:])
                    rsqrt_col(rstd, ss, 1.0 / 1024, EPS)
                    ts("dve", hsb, hsb.t[:], xh[i], xh[i].t[:], rstd.t[:, 0:1], None, ALU.mult, reads=[rstd])
                    for c in range(8):
                        tr(pTb, pTb.t[:, c * 128:(c + 1) * 128], hsb, hsb.t[:, c * 128:(c + 1) * 128], ident_b)
                    cp("act", xn2T, xn2T.t[:, :, i * 128:(i + 1) * 128], pTb,
                       pTb.t[:].rearrange("p (c n) -> p c n", c=8))
                for blk in range(4):
                    wb = wload(wq_s, wq_s.t[:, :, blk * 512:(blk + 1) * 512], v8)
                    wv = v8(wb.t)
                    for jj in range(4):
                        jb = blk * 4 + jj
                        for c in range(8):
                            mm(pM, pM.t[:, 0:256], wb, wv[:, c, jj * 128:(jj + 1) * 128], xn2T, xn2T.t[:, c, :],
                               start=(c == 0), stop=(c == 7))
                        cp("act" if jb % 2 == 0 else "dve", qT, qT.t[:, jb, :], pM, pM.t[:, 0:256])
                for i in range(2):
                    for b4 in range(4):
                        for jj in range(4):
                            jb = b4 * 4 + jj
                            mm(pM, pM.t[:, jj * 128:(jj + 1) * 128], qT, qT.t[:, jb, i * 128:(i + 1) * 128],
                               KT, KT.t[:, jb, :])
                        cp("act", sc[i], sc[i].t[:, b4 * 512:(b4 + 1) * 512], pM, pM.t[:])
                def topk_body(i, slot):
                    d_ = TK[slot]
                    v16 = d_["v16"]
                    tmp128 = d_["tmp128"]
                    cand = d_["cand"]
                    cand2 = d_["cand2"]
                    c16 = d_["c16"]
                    ex16 = d_["ex16"]
                    negm = d_["negm"]
                    zz = d_["zz"]
                    c24 = d_["c24"]
                    tsum = d_["tsum"]
                    cand3 = d_["cand3"]
                    for h in range(8):
                        for half in range(2):
                            sv = sc[i].t[:, (h * 2 + half) * 128:(h * 2 + half + 1) * 128]
                            vv = v16[half]
                            S.op("dve", lambda e: e.max(out=vv.t[:, 0:8], in_=sv), reads=[sc[i]], writes=[vv])
                            S.op("dve", lambda e: e.match_replace(out=tmp128.t[:], in_to_replace=vv.t[:, 0:8],
                                                                  in_values=sv, imm_value=-1e30),
                                 reads=[sc[i], vv], writes=[tmp128])
                            S.op("dve", lambda e: e.max(out=vv.t[:, 8:16], in_=tmp128.t[:]), reads=[tmp128], writes=[vv])
                        tt("dve", cand, cand.t[:].rearrange("p (a b) -> p a b", a=16),
                           v16[0], v16[0].t[:].unsqueeze(2).to_broadcast([128, 16, 16]),
                           v16[1], v16[1].t[:].unsqueeze(1).to_broadcast([128, 16, 16]), ALU.add)
                        S.op("dve", lambda e: e.max(out=c16.t[:, 0:8], in_=cand.t[:]), reads=[cand], writes=[c16])
                        S.op("dve", lambda e: e.match_replace(out=cand2.t[:], in_to_replace=c16.t[:, 0:8],
                                                              in_values=cand.t[:], imm_value=-1e30),
                             reads=[cand, c16], writes=[cand2])
                        S.op("dve", lambda e: e.max(out=c16.t[:, 8:16], in_=cand2.t[:]), reads=[cand2], writes=[c16])
                        ts("dve", negm, negm.t[:], c16, c16.t[:, 0:1], -1.0, None, ALU.mult)
                        act(ex16, ex16.t[:], c16, c16.t[:], AF.Exp, reads=[negm], wr=[zz], bias=negm.t[:, 0:1],
                            accum_out=zz.t[:])
                        act(zz, zz.t[:], zz, zz.t[:], AF.Ln)
                        tt("dve", ncc[i], ncc[i].t[:, h:h + 1], negm, negm.t[:], zz, zz.t[:], ALU.subtract)
                        S.op("dve", lambda e: e.match_replace(out=cand3.t[:], in_to_replace=c16.t[:, 8:16],
                                                              in_values=cand2.t[:], imm_value=-1e30),
                             reads=[cand2, c16], writes=[cand3])
                        S.op("dve", lambda e: e.max(out=c24.t[:], in_=cand3.t[:]), reads=[cand3], writes=[c24])
                        tt("dve", tsum, tsum.t[:], c16, c16.t[:, 15:16], c24, c24.t[:, 0:1], ALU.add)
                        ts("dve", taum[i], taum[i].t[:, h:h + 1], tsum, tsum.t[:], 0.5, None, ALU.mult)
                S.run_interleaved([lambda: topk_body(0, 0), lambda: topk_body(1, 1)])
                for i in range(2):
                    tt("dve", ttau[i], ttau[i].t[:], taum[i], taum[i].t[:], ncc[i], ncc[i].t[:], ALU.add)
                    act(ttau[i], ttau[i].t[:], ttau[i], ttau[i].t[:], AF.Exp)
                    for h in range(8):
                        act(E1n[i], E1n[i].t[:, h, :], sc[i], sc[i].t[:, (h * 2) * 128:(h * 2 + 1) * 128], AF.Exp,
                            reads=[ncc[i]], bias=ncc[i].t[:, h:h + 1])
                    act(E2[i], E2[i].t[:], sc[i],
                        sc[i].t[:].rearrange("p (h t k) -> p h t k", h=8, t=2)[:, :, 1, :], AF.Exp)
                pu = pU[0]
                pAccs = [pM, pU[1]]
                wts = {}
                cnt = dict(tn=0)

                def stA(n):
                    eb, i = divmod(n, 2)
                    if i == 0:
                        wts[eb] = (wload(dn_s, dn_s.t[:, :, eb * 512:(eb + 1) * 512], v8),
                                   wload(up_s, up_s.t[:, eb * 4:(eb + 1) * 4, :], v4))
                    dnb = wts[eb][0]
                    dv = v8(dnb.t)
                    ag = Ag[n % 2]
                    for c in range(8):
                        mm(pu, pu.t[:], xn2T, xn2T.t[:, c, i * 128:(i + 1) * 128], dnb, dv[:, c, :],
                           start=(c == 0), stop=(c == 7))
                    act(ag, ag.t[:], pu, pu.t[:], AF.Gelu)

                def stT(n, mid=None):
                    eb, i = divmod(n, 2)
                    for h in range(8):
                        if h == 4 and mid is not None:
                            mid()
                        Tb = Tt[cnt["tn"] % 4]
                        cnt["tn"] += 1
                        if h < 2 or h == 7:
                            for q in range(4):
                                act(Tb, Tb.t[:, q * 128:(q + 1) * 128], E2[i], E2[i].t[:, h, :], AF.Copy,
                                    reads=[E1n[i]], scale=E1n[i].t[:, h, eb * 4 + q:eb * 4 + q + 1])
                        else:
                            tt("pool", Tb, Tb.t[:].rearrange("p (a b) -> p a b", a=4),
                               E1n[i], E1n[i].t[:, h, eb * 4:eb * 4 + 4].unsqueeze(2).to_broadcast([128, 4, 128]),
                               E2[i], E2[i].t[:, h, :].unsqueeze(1).to_broadcast([128, 4, 128]), ALU.mult)
                        gm = gms[n % 2][h]
                        stt("dve", gm, gm.t[:], Tb, Tb.t[:], ttau[i].t[:, h:h + 1], Tb, Tb.t[:],
                            ALU.is_ge, ALU.mult, reads=[ttau[i]])

                def stAcc(n):
                    pAcc = pAccs[n % 2]
                    for h in range(8):
                        gm = gms[n % 2][h]
                        mm(pAcc, pAcc.t[:], ident_b, ident_b.t[:], gm, gm.t[:], start=(h == 0), stop=(h == 7))

                def stG(n):
                    pAcc = pAccs[n % 2]
                    ag = Ag[n % 2]
                    gat = GAT[n % 2]
                    tt("dve", GA, GA.t[:], pAcc, pAcc.t[:], ag, ag.t[:], ALU.mult)
                    half = (n % 2) * 512
                    for q in range(4):
                        tr(pTb, pTb.t[:, half + q * 128:half + (q + 1) * 128], GA, GA.t[:, q * 128:(q + 1) * 128], ident_b)
                    cp("act", gat, gat.t[:], pTb, pTb.t[:, half:half + 512].rearrange("p (a b) -> p a b", a=4))

                def stY(n):
                    eb, i = divmod(n, 2)
                    gat = GAT[n % 2]
                    upb = wts[eb][1]
                    uv = v4(upb.t)
                    for q in range(4):
                        for nn in range(2):
                            mm(pY[i][nn], pY[i][nn].t[:], gat, gat.t[:, q, :], upb, uv[:, q, nn * 512:(nn + 1) * 512],
                               start=(eb == 0 and q == 0), stop=(eb == 31 and q == 3))

                NU = 64
                for k in range(NU + 2):
                    if k < NU:
                        stA(k)
                    if 0 <= k - 1 < NU:
                        stAcc(k - 1)
                    if 0 <= k - 2 < NU:
                        stY(k - 2)
                    gfn = (lambda kk=k: stG(kk - 1)) if 0 <= k - 1 < NU else None
                    if k < NU:
                        stT(k, mid=gfn)
                    elif gfn is not None:
                        gfn()
                for i in range(2):
                    for n in range(2):
                        tt("dve", hf, hf.t[:, n * 512:(n + 1) * 512], pY[i][n], pY[i][n].t[:],
                           xh[i], xh[i].t[:, n * 512:(n + 1) * 512], ALU.add)
                    act(yo_p[i], yo_p[i].t[:], hf, hf.t[:], AF.Square, wr=[ss], accum_out=ss.t[:])
                    rsqrt_col(rstd, ss, 1.0 / 1024, EPS)
                    yo = yo_p[i]
                    stt("dve", yo, yo.t[:], hf, hf.t[:], rstd.t[:, 0:1], fnl, fnl.t[:], ALU.mult, ALU.mult, reads=[rstd])
                    S.dma("pool", y_d[t0 + i * 128:t0 + (i + 1) * 128, :], yo.t[:], reads=[yo], writes=[Tn(None)])

        for st in range(NST):
            if stage >= 1:
                mixer(st)
                S.barrier()
            if stage >= 2:
                peer(st)
                S.barrier(switch=((st + 1) % sw_every == 0 and st + 1 < NST))
        S.finish()
        print("ninst", S.ninst, {k: v["count"] for k, v in S.eng.items()})
    return nc


def host_inputs(inp):
    f = lambda a: np.ascontiguousarray(np.asarray(a, dtype=np.float32))
    w_in = np.asarray(inp["w_in"])[0]
    cols = []
    for h in range(8):
        for base in (512, 1536, 2560, 3584):
            cols.append(np.arange(base + h * 128, base + (h + 1) * 128))
    cols.append(np.arange(0, 512))
    cols.append(np.arange(4624, 6672))
    cols.append(np.arange(4608, 4624))
    cols = np.concatenate(cols)
    assert cols.shape[0] == IN_COLS
    r = np.arange(128)
    d = {}
    d["w_in_p"] = f(w_in[:, cols])
    d["mnw"] = f(np.asarray(inp["mix_norm_w"])[0].reshape(8, 128).T)
    d["fnw"] = f(np.asarray(inp["ffn_norm_w"])[0].reshape(8, 128).T)
    d["pool_w_p"] = f(np.asarray(inp["pool_w"])[0].transpose(1, 0, 2))
    d["pool_scale_p"] = f(np.asarray(inp["pool_scale"])[0].reshape(4, 128).T)
    d["conv_w_p"] = f(np.asarray(inp["conv_w"])[0].T.reshape(24, 128, 4).transpose(1, 0, 2))
    d["a_log_p"] = f(np.broadcast_to(np.asarray(inp["a_log"])[0][None, :], (128, 8)))
    d["dt_bias_p"] = f(np.broadcast_to(np.asarray(inp["dt_bias"])[0][None, :], (128, 8)))
    d["dn_norm_p"] = f(np.asarray(inp["dn_norm_w"])[0].reshape(128, 1))
    d["w_pool_up"] = f(np.asarray(inp["w_pool_up"])[0])
    d["w_dn_up"] = f(np.asarray(inp["w_dn_up"])[0])
    d["w_mix_out"] = f(np.asarray(inp["w_mix_out"])[0])
    d["peer_w_query"] = f(np.asarray(inp["peer_w_query"])[0])
    k1 = np.asarray(inp["peer_keys_1"])[0]
    k2 = np.asarray(inp["peer_keys_2"])[0]
    kt = np.stack([k1, k2], axis=1).reshape(16, 128, 128)
    d["keys_t"] = f(kt.transpose(2, 0, 1))
    d["peer_down_t"] = f(np.asarray(inp["peer_down"])[0].T)
    d["peer_up"] = f(np.asarray(inp["peer_up"])[0])
    d["final_w_p"] = f(np.broadcast_to(np.asarray(inp["final_norm_w"])[None, :], (128, 1024)))
    d["c_ident"] = f(np.eye(128))
    d["c_tri"] = f(r[:, None] <= r[None, :])
    d["c_negu"] = f(np.where(r[None, :] >= r[:, None], 0.0, NEG))
    d["c_negls"] = f(np.where(r[:, None] > r[None, :], 0.0, NEG))
    d["c_ones"] = f(np.ones((128, 128)))
    pf = np.ones((4, 16), np.float32)
    for g in range(4):
        w = 2 << g
        for t in range(16):
            pf[g, t] = w / min(t + 1, w)
    d["c_poolfix"] = f(np.broadcast_to(pf[None], (128, 4, 16)))
    return d


_NC_CACHE = {}


def kernel(**inputs):
    x = np.asarray(inputs["x"], dtype=np.float32)
    B, S_TOK, _ = x.shape
    if S_TOK not in _NC_CACHE:
        _NC_CACHE[S_TOK] = build(S_TOK)
    nc = _NC_CACHE[S_TOK]
    shared = host_inputs(inputs)
    in_maps = []
    for b in range(B):
        m = dict(shared)
        m["x"] = np.ascontiguousarray(x[b])
        in_maps.append(m)
    res = run_bass_kernel_spmd(nc, in_maps, core_ids=list(range(B)))
    return np.stack([np.asarray(r["y"]) for r in res.results], axis=0).astype(np.float32)
```
